# Optimizing a Trainium2 kernel written in Bass

```python
import math
import jax, jax.numpy as jnp
from jax import lax
import numpy as np


D_MODEL = 1024
BATCH = 4
SEQ = 8192
DEPTH = 2

GLA_HEADS = 4
GLA_DK = 64
GLA_DV = 128
GLA_RANK = 16
GLA_TAU = 16.0
GLA_CHUNK = 64
DIFF_HEADS = 4
DIFF_DH = 64
DIFF_DV = 2 * DIFF_DH
Q_BLOCK = 128
REL_BUCKETS = 32
REL_MAX_DIST = 128
N_GROUPS = 4
EXPERTS_PER_GROUP = 8
N_EXPERTS = N_GROUPS * EXPERTS_PER_GROUP
TOP_K = 2
D_EXPERT = D_MODEL // 2
RMS_EPS = 1e-6

GLA_QK_W = GLA_HEADS * GLA_DK
GLA_V_W = GLA_HEADS * GLA_DV
DIFF_QK_W = DIFF_HEADS * 2 * DIFF_DH
DIFF_V_W = DIFF_HEADS * DIFF_DV
IN_SPLITS = (GLA_QK_W, GLA_QK_W, GLA_V_W, GLA_V_W, GLA_RANK, GLA_RANK,
             DIFF_QK_W, DIFF_QK_W, DIFF_V_W, D_MODEL, D_MODEL)
D_IN = GLA_QK_W * 2 + GLA_V_W * 2 + GLA_RANK * 2 + DIFF_QK_W * 2 + DIFF_V_W + D_MODEL * 2

kernel_name = 'hybrid_gla_diffattn_hmoe_encoder'


def rms_norm(x, g):
    xf = x.astype(jnp.float32)
    y = xf * lax.rsqrt(jnp.mean(xf * xf, axis=-1, keepdims=True) + RMS_EPS)
    return (y * g.astype(jnp.float32)).astype(x.dtype)


def modulate(h, shift, scale):
    return h * (1.0 + scale[:, None, :]) + shift[:, None, :]


def split_heads(t, n):
    b, s, _ = t.shape
    return t.reshape(b, s, n, -1).transpose(0, 2, 1, 3)


def merge_heads(t):
    b, h, s, d = t.shape
    return t.transpose(0, 2, 1, 3).reshape(b, s, h * d)


def t5_bucket(rel):
    nb = REL_BUCKETS // 2
    max_exact = nb // 2
    ret = jnp.where(rel > 0, nb, 0)
    n = jnp.abs(rel)
    nf = jnp.maximum(n, 1).astype(jnp.float32)
    large = max_exact + (jnp.log(nf / max_exact) / math.log(REL_MAX_DIST / max_exact)
                         * (nb - max_exact)).astype(jnp.int32)
    large = jnp.minimum(large, nb - 1)
    return ret + jnp.where(n < max_exact, n, large)


def gla_chunked(q, k, v, lg, inclusive):
    out_dtype = v.dtype
    b_, h_, s_, dk = q.shape
    dv = v.shape[-1]
    n = s_ // GLA_CHUNK
    f32 = jnp.float32
    q = q.astype(f32).reshape(b_, h_, n, GLA_CHUNK, dk)
    k = k.astype(f32).reshape(b_, h_, n, GLA_CHUNK, dk)
    v = v.astype(f32).reshape(b_, h_, n, GLA_CHUNK, dv)
    cum = jnp.cumsum(lg.astype(f32).reshape(b_, h_, n, GLA_CHUNK, dk), axis=-2)
    cum_last = cum[..., -1:, :]
    q_dec = q * jnp.exp(cum)
    k_inv = k * jnp.exp(-cum)
    k_end = k * jnp.exp(cum_last - cum)
    mask = jnp.tril(jnp.ones((GLA_CHUNK, GLA_CHUNK), bool), 0 if inclusive else -1)
    a = jnp.where(mask, jnp.einsum('bhnid,bhnjd->bhnij', q_dec, k_inv), 0.0)
    o_intra = jnp.einsum('bhnij,bhnjv->bhniv', a, v)
    kv = jnp.einsum('bhncd,bhncv->bhndv', k_end, v)
    chunk_decay = jnp.exp(cum_last[..., 0, :])

    def step(state, inp):
        kv_n, dec_n = inp
        return dec_n[..., None] * state + kv_n, state

    _, states = lax.scan(step, jnp.zeros((b_, h_, dk, dv), f32),
                         (jnp.moveaxis(kv, 2, 0), jnp.moveaxis(chunk_decay, 2, 0)))
    states = jnp.moveaxis(states, 0, 2)
    o_inter = jnp.einsum('bhncd,bhndv->bhncv', q_dec, states)
    return (o_intra + o_inter).reshape(b_, h_, s_, dv).astype(out_dtype)


def diff_attention(q, k, v, lam, rel_bias):
    b_, h_, _, s_, dh = q.shape
    nb = s_ // Q_BLOCK
    qb = q.reshape(b_, h_, 2, nb, Q_BLOCK, dh).transpose(3, 0, 1, 2, 4, 5)
    kpos = jnp.arange(s_, dtype=jnp.int32)
    scale = dh ** -0.5

    def block(args):
        q_blk, start = args
        qpos = start + jnp.arange(Q_BLOCK, dtype=jnp.int32)
        bucket = t5_bucket(kpos[None, :] - qpos[:, None])
        bias = jnp.moveaxis(rel_bias[bucket], -1, 0).astype(jnp.float32)
        logits = (jnp.einsum('bhcqd,bhckd->bhcqk', q_blk, k).astype(jnp.float32) * scale
                  + bias[None, :, None])
        p = jax.nn.softmax(logits, axis=-1)
        w = p[:, :, 0] - lam.astype(jnp.float32) * p[:, :, 1]
        return jnp.einsum('bhqk,bhkv->bhqv', w.astype(v.dtype), v)

    starts = jnp.arange(nb, dtype=jnp.int32) * Q_BLOCK
    out = lax.map(block, (qb, starts))
    return out.transpose(1, 2, 0, 3, 4).reshape(b_, h_, s_, v.shape[-1])


def token_mixer(h, w_in, gla_w_up, gla_b_up, gla_norm_g, qn_g, kn_g, lam_vecs,
                subn_g, rel_bias, w_a, w_b, w_o, lam_init):
    b_, s_, _ = h.shape
    proj = h @ w_in
    idx = np.cumsum(IN_SPLITS)[:-1].tolist()
    (g_q, g_k, g_v, g_g, lr_f, lr_b, d_q, d_k, d_v, gate_a, gate_b) = jnp.split(proj, idx, axis=-1)

    q = split_heads(g_q, GLA_HEADS) * (GLA_DK ** -0.5)
    k = split_heads(g_k, GLA_HEADS)
    v = split_heads(g_v, GLA_HEADS)
    lg_f = split_heads(jax.nn.log_sigmoid((lr_f @ gla_w_up[0] + gla_b_up[0]).astype(jnp.float32)) / GLA_TAU, GLA_HEADS)
    lg_b = split_heads(jax.nn.log_sigmoid((lr_b @ gla_w_up[1] + gla_b_up[1]).astype(jnp.float32)) / GLA_TAU, GLA_HEADS)
    flip = lambda t: jnp.flip(t, axis=2)
    o_f = gla_chunked(q, k, v, lg_f, True)
    o_b = flip(gla_chunked(flip(q), flip(k), flip(v), flip(lg_b), False))
    o_a = merge_heads(rms_norm(o_f + o_b, gla_norm_g)) * jax.nn.silu(g_g)
    y_a = o_a @ w_a

    dq = rms_norm(d_q.reshape(b_, s_, DIFF_HEADS, 2, DIFF_DH).transpose(0, 2, 3, 1, 4), qn_g)
    dk = rms_norm(d_k.reshape(b_, s_, DIFF_HEADS, 2, DIFF_DH).transpose(0, 2, 3, 1, 4), kn_g)
    dv = split_heads(d_v, DIFF_HEADS)
    lam = (jnp.exp(jnp.sum(lam_vecs[0] * lam_vecs[1]).astype(jnp.float32))
           - jnp.exp(jnp.sum(lam_vecs[2] * lam_vecs[3]).astype(jnp.float32)) + lam_init)
    o_b2 = diff_attention(dq, dk, dv, lam, rel_bias)
    o_b2 = merge_heads(rms_norm(o_b2, subn_g) * (1.0 - lam_init))
    y_b = o_b2 @ w_b

    merged = jax.nn.sigmoid(gate_a) * y_a + jax.nn.sigmoid(gate_b) * y_b
    return merged @ w_o


def hier_moe(h, w_rg, b_rg, w_re, b_re, w1, w2):
    b_, s_, d_ = h.shape
    t = h.reshape(-1, d_)
    n_tok = t.shape[0]
    g_logits = (t @ w_rg).astype(jnp.float32) + b_rg.astype(jnp.float32)
    g_prob = jax.nn.softmax(g_logits, axis=-1)
    grp = jnp.argmax(g_logits, axis=-1)
    p_grp = jnp.take_along_axis(g_prob, grp[:, None], axis=-1)
    e_logits = ((t @ w_re).astype(jnp.float32) + b_re.astype(jnp.float32)).reshape(n_tok, N_GROUPS, EXPERTS_PER_GROUP)
    e_logits = jnp.take_along_axis(e_logits, grp[:, None, None], axis=1)[:, 0]
    e_prob = jax.nn.softmax(e_logits, axis=-1)
    top_p, top_e = lax.top_k(e_prob, TOP_K)
    gate = p_grp * (top_p / jnp.sum(top_p, axis=-1, keepdims=True))
    expert = grp[:, None] * EXPERTS_PER_GROUP + top_e
    flat_e = expert.reshape(-1)
    flat_tok = jnp.repeat(jnp.arange(n_tok, dtype=jnp.int32), TOP_K)
    flat_gate = gate.reshape(-1)
    order = jnp.argsort(flat_e)
    tok_s = flat_tok[order]
    xs = t[tok_s]
    sizes = jnp.bincount(flat_e, length=N_EXPERTS).astype(jnp.int32)
    hu = lax.ragged_dot(xs, w1, sizes)
    h_gate, h_up = jnp.split(hu, 2, axis=-1)
    ys = lax.ragged_dot(jax.nn.silu(h_gate) * h_up, w2, sizes)
    ys = ys * flat_gate[order][:, None].astype(ys.dtype)
    out = jnp.zeros_like(t).at[tok_s].add(ys)
    return out.reshape(b_, s_, d_)


def setup_inputs(seed: int = 0) -> dict:
    key = jax.random.key(seed)
    ks = jax.random.split(key, 26)
    nrm = lambda k, shape, s: jax.random.normal(k, shape, jnp.float32) * s
    gain = lambda k, shape: 1.0 + 0.05 * jax.random.normal(k, shape, jnp.float32)
    D = D_MODEL
    return {
        'x': nrm(ks[0], (BATCH, SEQ, D), 1.0),
        'c': nrm(ks[1], (BATCH, D), 1.0),
        'w_ada': nrm(ks[2], (DEPTH, D, 6 * D), 0.5 * D ** -0.5),
        'b_ada': nrm(ks[3], (DEPTH, 6 * D), 0.01),
        'norm1_g': gain(ks[4], (DEPTH, D)),
        'norm2_g': gain(ks[5], (DEPTH, D)),
        'w_in': nrm(ks[6], (DEPTH, D, D_IN), D ** -0.5),
        'gla_w_up': nrm(ks[7], (DEPTH, 2, GLA_RANK, GLA_QK_W), GLA_RANK ** -0.5),
        'gla_b_up': nrm(ks[8], (DEPTH, 2, GLA_QK_W), 0.1),
        'gla_norm_g': gain(ks[9], (DEPTH, GLA_DV)),
        'diff_qnorm_g': gain(ks[10], (DEPTH, DIFF_DH)),
        'diff_knorm_g': gain(ks[11], (DEPTH, DIFF_DH)),
        'diff_lambda': nrm(ks[12], (DEPTH, 4, DIFF_DH), 0.1),
        'diff_subnorm_g': gain(ks[13], (DEPTH, DIFF_DV)),
        'rel_bias': nrm(ks[14], (REL_BUCKETS, DIFF_HEADS), 0.5),
        'w_branch_a': nrm(ks[15], (DEPTH, GLA_V_W, D), GLA_V_W ** -0.5),
        'w_branch_b': nrm(ks[16], (DEPTH, DIFF_V_W, D), DIFF_V_W ** -0.5),
        'w_out': nrm(ks[17], (DEPTH, D, D), D ** -0.5),
        'w_router_group': nrm(ks[18], (DEPTH, D, N_GROUPS), D ** -0.5),
        'b_router_group': nrm(ks[19], (DEPTH, N_GROUPS), 0.01),
        'w_router_expert': nrm(ks[20], (DEPTH, D, N_EXPERTS), D ** -0.5),
        'b_router_expert': nrm(ks[21], (DEPTH, N_EXPERTS), 0.01),
        'w_expert_in': nrm(ks[22], (DEPTH, N_EXPERTS, D, 2 * D_EXPERT), D ** -0.5),
        'w_expert_out': nrm(ks[23], (DEPTH, N_EXPERTS, D_EXPERT, D), D_EXPERT ** -0.5),
    }


def reference(x, c, w_ada, b_ada, norm1_g, norm2_g, w_in, gla_w_up, gla_b_up, gla_norm_g,
              diff_qnorm_g, diff_knorm_g, diff_lambda, diff_subnorm_g, rel_bias,
              w_branch_a, w_branch_b, w_out, w_router_group, b_router_group,
              w_router_expert, b_router_expert, w_expert_in, w_expert_out):
    c_act = jax.nn.silu(c)
    for l in range(DEPTH):
        lam_init = 0.8 - 0.6 * math.exp(-0.3 * l)
        mod = c_act @ w_ada[l] + b_ada[l]
        sh1, sc1, g1, sh2, sc2, g2 = jnp.split(mod, 6, axis=-1)
        h = modulate(rms_norm(x, norm1_g[l]), sh1, sc1)
        y = token_mixer(h, w_in[l], gla_w_up[l], gla_b_up[l], gla_norm_g[l],
                        diff_qnorm_g[l], diff_knorm_g[l], diff_lambda[l], diff_subnorm_g[l],
                        rel_bias, w_branch_a[l], w_branch_b[l], w_out[l], lam_init)
        x = x + g1[:, None, :] * y
        h = modulate(rms_norm(x, norm2_g[l]), sh2, sc2)
        y = hier_moe(h, w_router_group[l], b_router_group[l], w_router_expert[l],
                     b_router_expert[l], w_expert_in[l], w_expert_out[l])
        x = x + g2[:, None, :] * y
    return x
```

```python
import math
from contextlib import ExitStack

import numpy as np
import concourse.bass as bass
import concourse.mybir as mybir
from concourse.bass_utils import run_bass_kernel_spmd

F32 = mybir.dt.float32
BF16 = mybir.dt.bfloat16
AF = mybir.ActivationFunctionType
ALU = mybir.AluOpType
AX = mybir.AxisListType

D = 1024
DEPTH = 2
NE = 32
CAP = 1024
DEXP = 512
RMS_EPS = 1e-6
GW = 1152
GC0 = 512
TVW = 1280
C_GQ, C_GK, C_GV, C_GG, C_LRF, C_LRB, C_DQ, C_DK, C_DV, C_GA, C_GB = (
    0, 256, 512, 1024, 1536, 1552, 1568, 2080, 2592, 3104, 4128)
D_IN = 5152


class Sched:
    ENG_SEM_MAX = 30000

    def __init__(self, nc, stack, n_dma_sems=32):
        self.nc = nc
        self.stack = stack
        self.engs = {"pe": nc.tensor, "act": nc.scalar, "dve": nc.vector,
                     "pool": nc.gpsimd, "sp": nc.sync}
        self.sem = {}
        self.cnt = {}
        self.old = {}
        self.nsem = 0
        for e in self.engs:
            self._new_eng_sem(e)
        self.known = {e: {} for e in self.engs}
        self.dma_sems = [self._mk_sem("dq") for _ in range(n_dma_sems)]
        self.dma_cnt = [0] * n_dma_sems
        self.dma_rr = 0
        self.res = {}
        self.n_instr = 0
        self.stopped = False
        import os
        self.max_instr = int(os.environ.get('BASS_MAXI', '100000000'))

    def _mk_sem(self, name):
        self.nsem += 1
        return self.stack.enter_context(self.nc.semaphore("%s_%d" % (name, self.nsem)))

    def _new_eng_sem(self, e):
        if e in self.sem:
            self.old[e] = (self.sem[e], self.cnt[e], e)
        self.sem[e] = self._mk_sem("s" + e)
        self.cnt[e] = 0

    def _wait(self, eng, tok):
        sem, val, _ = tok
        k = self.known[eng]
        sid = id(sem)
        if k.get(sid, 0) >= val:
            return
        self.engs[eng].wait_ge(sem, val)
        k[sid] = val

    def _deps(self, eng, reads, writes):
        toks = []
        for key in reads:
            r = self.res.get(key)
            if r is not None and r[0] is not None:
                toks.append(r[0])
        for key in writes:
            r = self.res.get(key)
            if r is not None:
                if r[0] is not None and (r[0][2] != eng or eng == "dma"):
                    toks.append(r[0])
                for t in r[1]:
                    if t[2] != eng or eng == "dma":
                        toks.append(t)
        for t in toks:
            if t[2] == "pe" and eng == "pe":
                continue
            self._wait(eng, t)

    def _update(self, tok, reads, writes):
        for key in reads:
            r = self.res.setdefault(key, [None, []])
            r[1].append(tok)
        for key in writes:
            self.res[key] = [tok, []]

    def op(self, eng, fn, reads=(), writes=()):
        if self.n_instr >= self.max_instr:
            self.stopped = True
        if self.stopped:
            return
        self._deps(eng, reads, writes)
        ins = fn()
        if self.cnt[eng] >= self.ENG_SEM_MAX:
            self._new_eng_sem(eng)
        self.cnt[eng] += 1
        ins.then_inc(self.sem[eng], 1)
        self._update((self.sem[eng], self.cnt[eng], eng), reads, writes)
        self.n_instr += 1

    def dma(self, eng, fn, reads=(), writes=()):
        if self.n_instr >= self.max_instr:
            self.stopped = True
        if self.stopped:
            return
        j = self.dma_rr
        self.dma_rr = (self.dma_rr + 1) % len(self.dma_sems)
        sem = self.dma_sems[j]
        if self.dma_cnt[j] > 0:
            self._wait(eng, (sem, 16 * self.dma_cnt[j], "dma"))
        self._deps(eng, reads, writes)
        ins = fn()
        self.dma_cnt[j] += 1
        ins.then_inc(sem, 16)
        self._update((sem, 16 * self.dma_cnt[j], "dma"), reads, writes)
        self.n_instr += 1

    def barrier(self):
        if self.stopped:
            return
        for eng in self.engs:
            for e2 in self.engs:
                if e2 != eng and self.cnt[e2] > 0:
                    self._wait(eng, (self.sem[e2], self.cnt[e2], e2))
                if e2 != eng and e2 in self.old:
                    self._wait(eng, self.old[e2])
            for j, sem in enumerate(self.dma_sems):
                if self.dma_cnt[j] > 0:
                    self._wait(eng, (sem, 16 * self.dma_cnt[j], "dma"))

    def cc(self, fn, reads=(), writes=()):
        if self.stopped:
            return
        if not hasattr(self, "cc_sem"):
            self.cc_sem = self._mk_sem("cc")
            self.cc_cnt = 0
        self._deps("pool", reads, writes)
        ins = fn()
        self.cc_cnt += 1
        ins.then_inc(self.cc_sem)
        self._update((self.cc_sem, self.cc_cnt, "dma"), reads, writes)
        self.n_instr += 1

    def finish(self, eng="sp"):
        if hasattr(self, "cc_sem") and self.cc_cnt > 0:
            self._wait(eng, (self.cc_sem, self.cc_cnt, "dma"))
        for j, sem in enumerate(self.dma_sems):
            if self.dma_cnt[j] > 0:
                self._wait(eng, (sem, 16 * self.dma_cnt[j], "dma"))


class Ring:
    def __init__(self, tiles, name):
        self.tiles = tiles
        self.name = name
        self.i = 0

    def next(self):
        j = self.i % len(self.tiles)
        self.i += 1
        return self.tiles[j], "%s%d" % (self.name, j)


class _Stop(Exception):
    pass


def build_program(SEQ, layers, dbg=False, phase_limit=99, NCORES=8):
    NLOC = SEQ // 2
    NB = NLOC // 512
    NT = NLOC // 128
    NTA = 2 * NT
    SA = 2 * NLOC
    nc = bass.Bass("TRN2", target_bir_lowering=False)

    def din(name, shape, dt=F32):
        return nc.dram_tensor(name, list(shape), dt, kind="ExternalInput").ap()

    def dscr(name, shape, dt):
        return nc.dram_tensor(name, list(shape), dt, kind="ExternalOutput" if dbg else "Internal").ap()

    x_loc_in = din("x_loc", [NLOC, D])
    x_oth_in = din("x_oth", [NLOC, D])
    c_col = din("c_col", [128, 8])
    ident_in = din("ident", [128, 128])
    jmat_in = din("jmat", [128, 128])
    bones_in = din("bones", [128, 128])
    ones_in = din("ones", [128, 128])
    tril_in = din("tril", [128, 256])
    triu_in = din("triu", [128, 256])
    oh_in = din("oh", [32, 3 * TVW])
    ohc_in = din("ohc", [32, 3 * 128])
    flags_in = din("flags", [128, 2])
    hmask_in = din("hmask", [128, 2])
    ecap_in = din("ecap", [128, NE])
    trash_in = din("trash", [128, 1])
    rel_bias_in = din("rel_bias", [32, 4])
    W = {}
    for l in layers:
        W[l] = dict(
            w_ada=din("w_ada%d" % l, [D, 6 * D]), b_ada=din("b_ada%d" % l, [1, 6 * D]),
            n1g=din("n1g%d" % l, [128, 8]), n2g=din("n2g%d" % l, [128, 8]),
            w_in=din("w_in%d" % l, [D, D_IN]),
            w_up=din("w_up%d" % l, [2, 16, 256]), b_up=din("b_up%d" % l, [128, 4]),
            glag=din("glag%d" % l, [128, 1]), qng=din("qng%d" % l, [128, 1]),
            kng=din("kng%d" % l, [128, 1]), lamv=din("lamv%d" % l, [1, 256]),
            subg=din("subg%d" % l, [128, 1]),
            w_a=din("w_a%d" % l, [512, D]), w_b=din("w_b%d" % l, [512, D]),
            w_o=din("w_o%d" % l, [D, D]),
            w_r=din("w_r%d" % l, [D, 36]), b_r=din("b_r%d" % l, [1, 36]),
            n2grow=din("n2grow%d" % l, [1, D]),
            w1=din("w1_%d" % l, [NE, D, 2 * DEXP]), w2=din("w2_%d" % l, [NE, DEXP, D]),
        )
    x_out = nc.dram_tensor("x_out", [NLOC, D], F32, kind="ExternalOutput").ap()

    tv_d = dscr("tv_d", [12, TVW], F32)
    gq_d = dscr("gq_d", [2, 128, NLOC], BF16)
    gk_d = dscr("gk_d", [2, 128, SA], BF16)
    lg_d = dscr("lg_d", [2, 2, 128, SA], F32)
    gv_d = dscr("gv_d", [SA, 512], BF16)
    gg_d = dscr("gg_d", [4, 128, NLOC], BF16)
    dq_d = dscr("dq_d", [4, 128, NLOC], BF16)
    dk_d = dscr("dk_d", [4, 128, SA], BF16)
    dv_d = dscr("dv_d", [SA, 512], BF16)
    ga_d = dscr("ga_d", [8, 128, NLOC], BF16)
    gb_d = dscr("gb_d", [8, 128, NLOC], BF16)
    oa_d = dscr("oa_d", [4, 128, NLOC], BF16)
    ob_d = dscr("ob_d", [4, 128, NLOC], BF16)
    x1_d = dscr("x1_d", [NLOC, D], F32)
    h2_d = dscr("h2_d", [8, 128, NLOC], BF16)
    Xg = dscr("Xg", [NE * CAP + 128, D], BF16)
    Yg = dscr("Yg", [NE * CAP + 1, D], F32)
    rows_d = dscr("rows_d", [2, D], F32)
    xmid_c = [nc.dram_tensor("xmid_c%d" % j, [512, D], F32).ap() for j in range(NB)]
    xg_c = [nc.dram_tensor("xg_c%d" % j, [1024, D], F32).ap() for j in range(NB)]
    dbg_d = dscr("dbg_d", [128, 256], F32)
    dbg2_d = dscr("dbg2_d", [128, 8, 512], BF16)
    dbg3_d = dscr("dbg3_d", [128, 12], F32)
    dbg4_d = dscr("dbg4_d", [128, 4, D], F32)
    dbg_out = {}

    with ExitStack() as top:
        S = Sched(nc, top)

        _uid = [0]

        def sbuf(st, name, shape, dt):
            _uid[0] += 1
            return st.enter_context(nc.sbuf_tensor("s%d_%s" % (_uid[0], name), list(shape), dt))

        pbanks = [top.enter_context(nc.psum_tensor("pb%d" % i, [128, 512], F32)) for i in range(8)]
        PS = Ring(pbanks, "pb")

        def mm(out, lhsT, rhs, start, stop, reads, writes):
            S.op("pe", lambda: nc.tensor.matmul(out, lhsT, rhs, start=start, stop=stop, skip_group_check=True), reads, writes)

        def tr(out, in_, ident, reads, writes):
            S.op("pe", lambda: nc.tensor.transpose(out, in_, ident), reads, writes)

        def act(out, in_, func, reads, writes, bias=None, scale=None, accum=None):
            kw = {}
            if bias is not None:
                kw["bias"] = bias
            if scale is not None:
                kw["scale"] = scale
            if accum is not None:
                kw["accum_out"] = accum
            S.op("act", lambda: nc.scalar.activation(out=out, in_=in_, func=func, **kw), reads, writes)

        def ts(eng, out, in0, s1, s2, op0, op1, reads, writes):
            e = nc.vector if eng == "dve" else nc.gpsimd
            if op1 is None:
                S.op(eng, lambda: e.tensor_scalar(out, in0, s1, None, op0=op0), reads, writes)
            else:
                S.op(eng, lambda: e.tensor_scalar(out, in0, s1, s2, op0=op0, op1=op1), reads, writes)

        def tt(eng, out, in0, in1, op, reads, writes):
            e = nc.vector if eng == "dve" else nc.gpsimd
            S.op(eng, lambda: e.tensor_tensor(out, in0, in1, op), reads, writes)

        def stt(out, in0, scalar, in1, op0, op1, reads, writes):
            S.op("dve", lambda: nc.vector.scalar_tensor_tensor(out, in0, scalar, in1, op0=op0, op1=op1), reads, writes)

        def cp(eng, out, in_, reads, writes):
            if eng == "act":
                S.op("act", lambda: nc.scalar.copy(out, in_), reads, writes)
            else:
                e = nc.vector if eng == "dve" else nc.gpsimd
                S.op(eng, lambda: e.tensor_copy(out, in_), reads, writes)

        def ld(out, in_, reads, writes, q="sp"):
            e = {"sp": nc.sync, "pool": nc.gpsimd, "act": nc.scalar}[q]
            S.dma(q, lambda: e.dma_start(out=out, in_=in_), reads, writes)

        ident_f = sbuf(top, "ident_f", [128, 128], F32)
        ident_b = sbuf(top, "ident_b", [128, 128], BF16)
        jmat_f = sbuf(top, "jmat_f", [128, 128], F32)
        bones_b = sbuf(top, "bones_b", [128, 128], BF16)
        ones_f = sbuf(top, "ones_f", [128, 128], F32)
        tril_f = sbuf(top, "tril_f", [128, 256], F32)
        triu_f = sbuf(top, "triu_f", [128, 256], F32)
        flags = sbuf(top, "flags", [128, 2], F32)
        hm = sbuf(top, "hm", [128, 2], F32)
        eps_c = sbuf(top, "eps_c", [128, 1], F32)
        one_c = sbuf(top, "one_c", [128, 1], F32)
        zero_c = sbuf(top, "zero_c", [128, 1], F32)
        cact = sbuf(top, "cact", [128, 8], F32)
        relb = sbuf(top, "relb", [32, 4], F32)
        cb = sbuf(top, "cb", [128, 12], F32)
        gat = sbuf(top, "gat", [128, NT, 2], F32)
        gix = sbuf(top, "gix", [128, NT, 2], mybir.dt.int32)
        ecap = sbuf(top, "ecap", [128, NE], F32)
        trash = sbuf(top, "trash", [128, 1], F32)
        zrow = sbuf(top, "zrow", [1, D], F32)
        ld(ident_f[:], ident_in, [], ["ident_f"])
        ld(ident_b[:], ident_in, [], ["ident_b"], q="pool")
        ld(jmat_f[:], jmat_in, [], ["jmat_f"])
        ld(bones_b[:], bones_in, [], ["bones_b"], q="pool")
        ld(ones_f[:], ones_in, [], ["ones_f"])
        ld(tril_f[:], tril_in, [], ["tril_f"])
        ld(triu_f[:], triu_in, [], ["triu_f"])
        ld(flags[:], flags_in, [], ["flags"])
        ld(hm[:], hmask_in, [], ["hm"])
        ld(ecap[:], ecap_in, [], ["ecap"])
        ld(trash[:], trash_in, [], ["trash"])
        S.op("dve", lambda: nc.vector.memset(zrow[:], 0.0), [], ["zrow"])
        ld(Yg[NE * CAP:NE * CAP + 1, :], zrow[:], ["zrow"], ["Yg"])
        ld(cact[:], c_col, [], ["cact"])
        ld(relb[:], rel_bias_in, [], ["relb"])
        S.op("dve", lambda: nc.vector.memset(eps_c[:], RMS_EPS), [], ["eps_c"])
        S.op("dve", lambda: nc.vector.memset(one_c[:], 1.0), [], ["one_c"])
        S.op("dve", lambda: nc.vector.memset(zero_c[:], 0.0), [], ["zero_c"])
        act(cact[:], cact[:], AF.Silu, ["cact"], ["cact"])

        def build_gtab(st_outer):
            Gtab = sbuf(st_outer, "Gtab", [128, 12, GW], BF16)
            with ExitStack() as st:
                oh = sbuf(st, "oh", [32, 3 * TVW], F32)
                ohc = sbuf(st, "ohc", [32, 3 * 128], F32)
                tvs = sbuf(st, "tvs", [4, 3 * TVW], F32)
                Hk = sbuf(st, "Hk", [128, GW], F32)
                ld(oh[:], oh_in, [], ["oh"])
                ld(ohc[:], ohc_in, [], ["ohc"])
                for tab in range(3):
                    for c0 in range(0, TVW, 512):
                        w = min(512, TVW - c0)
                        p, pk = PS.next()
                        mm(p[0:4, 0:w], relb[:, :], oh[:, tab * TVW + c0: tab * TVW + c0 + w], True, True,
                           ["relb", "oh"], [pk])
                        cp("dve", tvs[:, tab * TVW + c0: tab * TVW + c0 + w], p[0:4, 0:w], [pk], ["tvs"])
                    ld(tv_d[tab * 4:(tab + 1) * 4, :], tvs[:, tab * TVW:(tab + 1) * TVW], ["tvs"], ["tv_d"])
                for k in range(3):
                    p, pk = PS.next()
                    mm(p[:, 0:4], ohc[:, k * 128:(k + 1) * 128], relb[:, :], True, True, ["ohc", "relb"], [pk])
                    cp("dve", cb[:, k * 4:(k + 1) * 4], p[:, 0:4], [pk], ["cb"])
                for th in range(12):
                    hank = bass.AP(tv_d.tensor, th * TVW, [[1, 128], [1, GW]])
                    ld(Hk[:], hank, ["tv_d"], ["Hk"])
                    for c0 in range(0, GW, 512):
                        w = min(512, GW - c0)
                        p, pk = PS.next()
                        mm(p[:, 0:w], jmat_f[:], Hk[:, c0:c0 + w], True, True, ["jmat_f", "Hk"], [pk])
                        cp("dve", Gtab[:, th, c0:c0 + w], p[:, 0:w], [pk], [("Gtab", th)])
            S.barrier()
            return Gtab

        try:
          for li, l in enumerate(layers):
            Wl = W[l]
            lam_init = 0.8 - 0.6 * math.exp(-0.3 * l)
            def x_loc_block(i, li=li):
                return x_loc_in[i * 512:(i + 1) * 512, :] if li == 0 else xmid_c[i]
            x_src_oth = x_oth_in
            fused_oth = li > 0
            is_last = li == len(layers) - 1
            with ExitStack() as lst:
                modcol = sbuf(lst, "modcol", [128, 48], F32)
                a1c = sbuf(lst, "a1c", [128, 8], F32)
                a2c = sbuf(lst, "a2c", [128, 8], F32)
                g1b = sbuf(lst, "g1b", [128, D], F32)
                g2b = sbuf(lst, "g2b", [128, D], F32)
                n1g = sbuf(lst, "n1g", [128, 8], F32)
                n2g = sbuf(lst, "n2g", [128, 8], F32)
                b_up = sbuf(lst, "b_up", [128, 4], F32)
                glag = sbuf(lst, "glag", [128, 1], F32)
                qng = sbuf(lst, "qng", [128, 1], F32)
                kng = sbuf(lst, "kng", [128, 1], F32)
                subg = sbuf(lst, "subg", [128, 1], F32)
                lamv = sbuf(lst, "lamv", [1, 256], F32)
                lamw = sbuf(lst, "lamw", [1, 8], F32)
                nlam2 = sbuf(lst, "nlam", [128, 2], F32)
                nlam = nlam2[:, 0:1]
                brb = sbuf(lst, "brb", [128, 36], F32)
                brow = sbuf(lst, "brow", [1, 36], F32)
                for nm, t_, src in (("n1g", n1g, Wl["n1g"]), ("n2g", n2g, Wl["n2g"]), ("b_up", b_up, Wl["b_up"]),
                                    ("glag", glag, Wl["glag"]), ("qng", qng, Wl["qng"]), ("kng", kng, Wl["kng"]),
                                    ("subg", subg, Wl["subg"]), ("lamv", lamv, Wl["lamv"]), ("brow", brow, Wl["b_r"])):
                    ld(t_[:], src, [], [nm])
                with ExitStack() as st:
                    modrow = sbuf(st, "modrow", [1, 6 * D], F32)
                    wada = [sbuf(st, "wada%d" % i, [128, 3 * D], F32) for i in range(2)]
                    bada = sbuf(st, "bada", [1, 6 * D], F32)
                    ld(bada[:], Wl["b_ada"], [], ["bada"])
                    macc = [PS.next() for _ in range(6)]
                    nld = 0
                    for half in range(2):
                        for k in range(8):
                            wt = wada[nld % 2]
                            wk = "wada%d" % (nld % 2)
                            nld += 1
                            ld(wt[:], Wl["w_ada"][k * 128:(k + 1) * 128, half * 3 * D:(half + 1) * 3 * D], [], [wk])
                            for j in range(6):
                                p, pk = macc[j]
                                mm(p[0:1, :], cact[:, k:k + 1], wt[:, j * 512:(j + 1) * 512], k == 0, k == 7,
                                   ["cact", wk], [pk])
                        for j in range(6):
                            p, pk = macc[j]
                            c0 = (half * 6 + j) * 512
                            tt("dve", modrow[0:1, c0:c0 + 512], p[0:1, :], bada[0:1, c0:c0 + 512], ALU.add,
                               [pk, "bada"], ["modrow"])
                    n2r = sbuf(st, "n2r", [1, D], F32)
                    arow = sbuf(st, "arow", [1, D], F32)
                    ld(n2r[:], Wl["n2grow"], [], ["n2r"])
                    stt(arow[:], modrow[0:1, 32 * 128:40 * 128], 1.0, n2r[:], ALU.add, ALU.mult, ["modrow", "n2r"], ["arow"])
                    ld(rows_d[0:1, :], arow[:], ["arow"], ["rows_d"])
                    ld(rows_d[1:2, :], modrow[0:1, 24 * 128:32 * 128], ["modrow"], ["rows_d"])
                    p, pk = PS.next()
                    for j in range(48):
                        mm(p[:, j:j + 1], modrow[0:1, j * 128:(j + 1) * 128], ones_f[0:1, 0:1], True, True,
                           ["modrow", "ones_f"], [pk])
                    cp("dve", modcol[:], p[:, 0:48], [pk], ["modcol"])
                    for gi, (gt_, gk_) in enumerate(((g1b, "g1b"), (g2b, "g2b"))):
                        base = (16 if gi == 0 else 40) * 128
                        for n in range(2):
                            p, pk = PS.next()
                            mm(p[:], ones_f[0:1, :], modrow[0:1, base + n * 512: base + (n + 1) * 512], True, True,
                               ["ones_f", "modrow"], [pk])
                            cp("dve", gt_[:, n * 512:(n + 1) * 512], p[:], [pk], [gk_])
                S.barrier()
                stt(a1c[:], modcol[:, 8:16], 1.0, n1g[:], ALU.add, ALU.mult, ["modcol", "n1g"], ["a1c"])
                stt(a2c[:], modcol[:, 32:40], 1.0, n2g[:], ALU.add, ALU.mult, ["modcol", "n2g"], ["a2c"])
                p, pk = PS.next()
                mm(p[:, 0:36], ones_f[0:1, :], brow[0:1, :], True, True, ["ones_f", "brow"], [pk])
                cp("dve", brb[:], p[:, 0:36], [pk], ["brb"])
                tt("dve", lamv[0:1, 0:64], lamv[0:1, 0:64], lamv[0:1, 64:128], ALU.mult, ["lamv"], ["lamv"])
                tt("dve", lamv[0:1, 128:192], lamv[0:1, 128:192], lamv[0:1, 192:256], ALU.mult, ["lamv"], ["lamv"])
                S.op("dve", lambda: nc.vector.reduce_sum(lamw[0:1, 0:1], lamv[0:1, 0:64], axis=AX.X), ["lamv"], ["lamw"])
                S.op("dve", lambda: nc.vector.reduce_sum(lamw[0:1, 1:2], lamv[0:1, 128:192], axis=AX.X), ["lamv"], ["lamw"])
                act(lamw[0:1, 2:4], lamw[0:1, 0:2], AF.Exp, ["lamw"], ["lamw"])
                stt(lamw[0:1, 4:5], lamw[0:1, 3:4], -lam_init, lamw[0:1, 2:3], ALU.add, ALU.subtract, ["lamw"], ["lamw"])
                p, pk = PS.next()
                mm(p[:, 0:1], ones_f[0:1, :], lamw[0:1, 4:5], True, True, ["ones_f", "lamw"], [pk])
                S.op("dve", lambda: nc.vector.memset(nlam2[:], 0.0), [], ["nlam"])
                cp("dve", nlam, p[:, 0:1], [pk], ["nlam"])
                qngs = sbuf(lst, "qngs", [128, 1], F32)
                subgs = sbuf(lst, "subgs", [128, 1], F32)
                ts("dve", qngs[:], qng[:], 0.125, None, ALU.mult, None, ["qng"], ["qngs"])
                ts("dve", subgs[:], subg[:], 1.0 - lam_init, None, ALU.mult, None, ["subg"], ["subgs"])

                def norm_block(xt, xk, ac, bc0, hT, hk, scr, hT32=None):
                    norm_pre(xt, xk, scr)
                    norm_post(xt, xk, ac, bc0, hT, hk, scr, hT32)

                def norm_pre(xt, xk, scr):
                    ss = scr["ss"]
                    for t in range(4):
                        act(scr["junk"][:], xt[:, t, :], AF.Square, [xk], ["junk"], accum=ss[:, t:t + 1])
                    act(ss[:, 4:8], ss[:, 0:4], AF.Ln, ["junk"], ["ssb"], bias=eps_c[:], scale=1.0 / D)
                    act(ss[:, 8:12], ss[:, 4:8], AF.Exp, ["ssb"], ["ssc"], scale=-0.5)
                    for t in range(4):
                        ts("pool" if t % 2 else "dve", xt[:, t, :], xt[:, t, :], ss[:, 8 + t:9 + t], None,
                           ALU.mult, None, [xk, "ssc"], [xk])

                def norm_post(xt, xk, ac, bc0, hT, hk, scr, hT32=None):
                    for k in range(8):
                        p, pk = PS.next()
                        for t in range(4):
                            tr(p[:, t * 128:(t + 1) * 128], xt[:, t, k * 128:(k + 1) * 128], ident_f[:],
                               [xk, "ident_f"], [pk])
                        dst = hT if hT32 is None else hT32
                        dkey = hk if hT32 is None else hk + "32"
                        if k % 2 == 0:
                            act(dst[:, k, :], p[:], AF.Identity, [pk, ac, "modcol"], [(dkey, k)],
                                bias=modcol[:, bc0 + k:bc0 + k + 1], scale=scr["ac"][:, k:k + 1])
                        else:
                            ts("dve", dst[:, k, :], p[:], scr["ac"][:, k:k + 1], modcol[:, bc0 + k:bc0 + k + 1],
                               ALU.mult, ALU.add, [pk, ac, "modcol"], [(dkey, k)])
                        if hT32 is not None and hT is not None:
                            cp("pool", hT[:, k, :], hT32[:, k, :], [(dkey, k)], [(hk, k)])

                print('n_instr before N1', S.n_instr)
                if dbg:
                    ld(dbg_d[:, 0:48], modcol[:], ["modcol"], ["dbg_d"])
                    ld(dbg_d[:, 48:56], a1c[:], ["a1c"], ["dbg_d"])
                    ld(dbg_d[:, 56:64], a2c[:], ["a2c"], ["dbg_d"])
                    ld(dbg_d[:, 64:66], nlam2[:, 0:2], ["nlam"], ["dbg_d"])
                    ld(dbg_d[:, 65:73], cact[:], ["cact"], ["dbg_d"])
                    ld(dbg_d[:, 80:144], g1b[:, 0:64], ["g1b"], ["dbg_d"])
                    ld(dbg_d[:, 144:208], g2b[:, 960:1024], ["g2b"], ["dbg_d"])
                    ld(dbg_d[:, 208:244], brb[:], ["brb"], ["dbg_d"])
                S.barrier()
                if phase_limit < 2:
                    S.stopped = True
                with ExitStack() as st:
                    w_in = sbuf(st, "w_in", [128, 8, D_IN], BF16)
                    for k in range(8):
                        ld(w_in[:, k, :], Wl["w_in"][k * 128:(k + 1) * 128, :], [], [("w_in", k)], q="pool")
                    wup = sbuf(st, "wup", [16, 2, 256], F32)
                    ld(wup[:], Wl["w_up"].rearrange("d r c -> r d c"), [], ["wup"])
                    nbup = sbuf(st, "nbup", [128, 4], F32)
                    ts("dve", nbup[:], b_up[:], -1.0, None, ALU.mult, None, ["b_up"], ["nbup"])
                    xts = [sbuf(st, "xt%d" % i, [128, 4, D], F32) for i in range(2)]
                    hTs = [sbuf(st, "hT%d" % i, [128, 8, 512], BF16) for i in range(2)]
                    scr = dict(ss=sbuf(st, "n_ss", [128, 12], F32), junk=sbuf(st, "n_junk", [128, D], BF16),
                               ac=a1c)
                    stg = Ring([sbuf(st, "stg%d" % i, [128, 512], BF16) for i in range(6)], "stg")
                    stf = Ring([sbuf(st, "stf%d" % i, [128, 512], F32) for i in range(4)], "stf")
                    lrT = [sbuf(st, "lrT%d" % i, [16, 512], F32) for i in range(2)]
                    blocks = [(0, i) for i in range(NB)] + [(1, i) for i in range(NB)]

                    xsel = [sbuf(st, "xsel%d" % i_, [128, D], F32) for i_ in range(2)] if fused_oth else None

                    def load_x(bi):
                        oth, i = blocks[bi]
                        if oth and fused_oth:
                            xt_ = xts[bi % 2]
                            xk_ = "xt%d" % (bi % 2)
                            ld(xt_[:], xg_c[i][0:512, :].rearrange("(t p) d -> p t d", p=128), ["xg"], [xk_])
                            for t in range(4):
                                r0 = 512 + t * 128
                                ld(xsel[t % 2][:], xg_c[i][r0:r0 + 128, :], ["xg"], ["xsel%d" % (t % 2)])
                                ts("pool", xt_[:, t, :], xt_[:, t, :], flags[:, 0:1], None, ALU.mult, None, [xk_, "flags"], [xk_])
                                stt(xt_[:, t, :], xsel[t % 2][:], flags[:, 1:2], xt_[:, t, :], ALU.mult, ALU.add,
                                    ["xsel%d" % (t % 2), "flags", xk_], [xk_])
                            return
                        src = x_src_oth[i * 512:(i + 1) * 512, :] if oth else x_loc_block(i)
                        ld(xts[bi % 2][:], src.rearrange("(t p) d -> p t d", p=128),
                           [("xmid", t_) for t_ in range(NT)] if (li > 0 and not oth) else [], ["xt%d" % (bi % 2)])

                    load_x(0)
                    norm_pre(xts[0], "xt0", scr)
                    norm_post(xts[0], "xt0", "a1c", 0, hTs[0], "hT0", scr)
                    for bi, (oth, i) in enumerate(blocks):
                        if bi + 1 < len(blocks):
                            load_x(bi + 1)
                        xt, xk = xts[bi % 2], "xt%d" % (bi % 2)
                        hT, hk = hTs[bi % 2], "hT%d" % (bi % 2)
                        nxt, nxk = xts[(bi + 1) % 2], "xt%d" % ((bi + 1) % 2)
                        nhT, nhk = hTs[(bi + 1) % 2], "hT%d" % ((bi + 1) % 2)
                        if dbg and bi == 0:
                            ld(dbg2_d, hT[:], [(hk, k) for k in range(8)], ["dbg2"])
                            ld(dbg3_d, scr["ss"][:], ["ssc"], ["dbg3"])
                            ld(dbg4_d, xt[:], [xk], ["dbg4"])
                        tok0 = (NLOC if oth else 0) + i * 512
                        hreads = [(hk, k) for k in range(8)]

                        def fm_tile(c0, m):
                            p, pk = PS.next()
                            for k in range(8):
                                mm(p[0:m, :], w_in[:, k, c0:c0 + m], hT[:, k, :], k == 0, k == 7,
                                   [("w_in", k), (hk, k)], [pk])
                            return p, pk

                        for pr in range(2):
                            p, pk = fm_tile(C_GK + pr * 128, 128)
                            s, sk = stg.next()
                            cp("act", s[:], p[:], [pk], [sk])
                            ld(gk_d[pr, :, tok0:tok0 + 512], s[:], [sk], ["gk_d"])
                            if not oth:
                                p, pk = fm_tile(C_GQ + pr * 128, 128)
                                s, sk = stg.next()
                                ts("dve", s[:], p[:], 0.125, None, ALU.mult, None, [pk], [sk])
                                ld(gq_d[pr, :, tok0:tok0 + 512], s[:], [sk], ["gq_d"])
                        for dr in range(2):
                            p, pk = fm_tile(C_LRF + dr * 16, 16)
                            cp("dve", lrT[dr][:], p[0:16, :], [pk], ["lrT%d" % dr])
                            for pr in range(2):
                                p, pk = PS.next()
                                mm(p[:], wup[:, dr, pr * 128:(pr + 1) * 128], lrT[dr][:], True, True,
                                   ["wup", "lrT%d" % dr], [pk])
                                s, sk = stf.next()
                                act(s[:], p[:], AF.Exp, [pk, "nbup"], [sk], scale=-1.0,
                                    bias=nbup[:, dr * 2 + pr: dr * 2 + pr + 1])
                                act(s[:], s[:], AF.Ln, [sk, "one_c"], [sk], bias=one_c[:])
                                ts("dve", s[:], s[:], -1.0 / 16.0, None, ALU.mult, None, [sk], [sk])
                                ld(lg_d[dr, pr, :, tok0:tok0 + 512], s[:], [sk], ["lg_d"])
                        for (c0, dst, dk_) in ((C_GV, gv_d, "gv_d"), (C_DV, dv_d, "dv_d")):
                            for t in range(4):
                                p, pk = PS.next()
                                for k in range(8):
                                    mm(p[:], hT[:, k, t * 128:(t + 1) * 128], w_in[:, k, c0:c0 + 512], k == 0, k == 7,
                                       [("w_in", k), (hk, k)], [pk])
                                s, sk = stg.next()
                                cp("act" if t % 2 else "dve", s[:], p[:], [pk], [sk])
                                ld(dst[tok0 + t * 128: tok0 + (t + 1) * 128, :], s[:], [sk], [dk_])
                        if bi + 1 < len(blocks):
                            norm_pre(nxt, nxk, scr)
                        qk_list = [("k", C_DK, dk_d, "dk_d", kng, "kng")]
                        if not oth:
                            qk_list.append(("q", C_DQ, dq_d, "dq_d", qngs, "qngs"))
                        for (nm, c0, dst, dkey, gcol, gkey) in qk_list:
                            for h in range(4):
                                p, pk = fm_tile(c0 + h * 128, 128)
                                s, sk = stg.next()
                                act(s[:], p[:], AF.Square, [pk], [sk])
                                p2, pk2 = PS.next()
                                mm(p2[:], bones_b[:], s[:], True, True, ["bones_b", sk], [pk2])
                                f, fk = stf.next()
                                act(f[:], p2[:], AF.Ln, [pk2, "eps_c"], [fk], bias=eps_c[:], scale=1.0 / 64.0)
                                act(f[:], f[:], AF.Exp, [fk], [fk], scale=-0.5)
                                s2, sk2 = stg.next()
                                stt(s2[:], p[:], gcol[:, 0:1], f[:], ALU.mult, ALU.mult, [pk, gkey, fk], [sk2])
                                ld(dst[h, :, tok0:tok0 + 512], s2[:], [sk2], [dkey])
                        if not oth:
                            for j in range(4):
                                p, pk = fm_tile(C_GG + j * 128, 128)
                                s, sk = stg.next()
                                act(s[:], p[:], AF.Silu, [pk], [sk])
                                ld(gg_d[j, :, tok0:tok0 + 512], s[:], [sk], ["gg_d"])
                            for (c0, dst, dkey) in ((C_GA, ga_d, "ga_d"), (C_GB, gb_d, "gb_d")):
                                for j in range(8):
                                    p, pk = fm_tile(c0 + j * 128, 128)
                                    s, sk = stg.next()
                                    act(s[:], p[:], AF.Sigmoid, [pk], [sk])
                                    ld(dst[j, :, tok0:tok0 + 512], s[:], [sk], [dkey])
                        if bi + 1 < len(blocks):
                            norm_post(nxt, nxk, "a1c", 0, nhT, nhk, scr)

                print('n_instr before GLA', S.n_instr)
                S.barrier()
                if phase_limit < 3:
                    S.stopped = True
                with ExitStack() as st:
                    kT = sbuf(st, "g_kT", [128, NLOC], BF16)
                    qT = sbuf(st, "g_qT", [128, NLOC], BF16)
                    lg = sbuf(st, "g_lg", [128, NLOC], F32)
                    cum = sbuf(st, "g_cum", [128, NLOC], F32)
                    kinv = sbuf(st, "g_kinv", [128, NLOC], BF16)
                    qdec = sbuf(st, "g_qdec", [128, NLOC], BF16)
                    qdM = [sbuf(st, "g_qdM%d" % i, [128, NLOC], BF16) for i in range(2)]
                    vv = sbuf(st, "g_v", [128, NT, 256], BF16)
                    rmask = sbuf(st, "g_rmask", [128, NLOC], F32)
                    ofw = sbuf(st, "g_of", [128, NT, 256], BF16)
                    ggT = sbuf(st, "g_gg", [128, 2, NLOC], BF16)
                    Sst = sbuf(st, "g_S", [128, 256], F32)
                    Sbf = sbuf(st, "g_Sbf", [128, 256], BF16)
                    tmpSr = Ring([sbuf(st, "g_tmpS%d" % i, [128, 256], F32) for i in range(4)], "g_tmpS")
                    kTt = Ring([sbuf(st, "g_kTt%d" % i, [128, 128], BF16) for i in range(4)], "g_kTt")
                    ATs = Ring([sbuf(st, "g_AT%d" % i, [128, 256], BF16) for i in range(4)], "g_AT")
                    osum = Ring([sbuf(st, "g_os%d" % i, [128, 256], F32) for i in range(4)], "g_os")
                    onr = Ring([sbuf(st, "g_on%d" % i, [128, 256], F32) for i in range(4)], "g_on")
                    gssr = Ring([sbuf(st, "g_ss%d" % i, [128, 8], F32) for i in range(4)], "g_ss")
                    gjunk = sbuf(st, "g_junk", [128, 128], BF16)
                    oaT = [sbuf(st, "g_oaT%d" % i, [128, 2, 512], BF16) for i in range(2)]
                    S.op("dve", lambda: nc.vector.memset(rmask[:], 1.0), [], ["rmask"])
                    rm3 = rmask[:].rearrange("p (c t) -> p c t", t=128)
                    S.op("dve", lambda: nc.vector.memset(rm3[:, :, 0:1], 0.0), ["rmask"], ["rmask"])
                    for pr in range(2):
                        ld(qT[:], gq_d[pr], ["gq_d"], ["g_qT"])
                        ld(ggT[:], gg_d[pr * 2:(pr + 1) * 2].rearrange("j p n -> p j n"), ["gg_d"], ["g_gg"])
                        for dr in range(2):
                            mask = tril_f if dr == 0 else triu_f
                            mkey = "tril_f" if dr == 0 else "triu_f"
                            dcol = 127 if dr == 0 else 0
                            S.op("dve", lambda: nc.vector.memset(Sst[:], 0.0), [], ["g_S"])
                            for is_oth in (True, False):
                                t0 = NLOC if is_oth else 0
                                ld(kT[:], gk_d[pr, :, t0:t0 + NLOC], ["gk_d"], ["g_kT"])
                                ld(vv[:], gv_d[t0:t0 + NLOC, pr * 256:(pr + 1) * 256].rearrange("(t p) c -> p t c", p=128),
                                   ["gv_d"], ["g_v"])
                                ld(lg[:], lg_d[dr, pr, :, t0:t0 + NLOC], ["lg_d"], ["g_lg"])
                                if dr == 0:
                                    S.op("dve", lambda: nc.vector.tensor_tensor_scan(
                                        out=cum[:], data0=rmask[:], data1=lg[:], initial=0.0, op0=ALU.mult, op1=ALU.add),
                                        ["rmask", "g_lg"], ["g_cum"])
                                else:
                                    S.op("dve", lambda: nc.vector.tensor_tensor_scan(
                                        out=cum[:, ::-1], data0=rmask[:], data1=lg[:, ::-1], initial=0.0,
                                        op0=ALU.mult, op1=ALU.add),
                                        ["rmask", "g_lg"], ["g_cum"])
                                act(lg[:], cum[:], AF.Exp, ["g_cum"], ["g_lg"], scale=-1.0)
                                tt("pool", kinv[:], kT[:], lg[:], ALU.mult, ["g_kT", "g_lg"], ["g_kinv"])
                                act(cum[:], cum[:], AF.Exp, ["g_cum"], ["g_cum"])
                                E = cum
                                if not is_oth:
                                    tt("dve", qdec[:], qT[:], E[:], ALU.mult, ["g_qT", "g_cum"], ["g_qdec"])
                                    for hh in range(2):
                                        ts("pool" if hh else "dve", qdM[hh][:], qdec[:], hm[:, hh:hh + 1], None, ALU.mult, None,
                                           ["g_qdec", "hm"], [("g_qdM", hh)])
                                    ts("dve", Sst[:], Sst[:], flags[:, dr:dr + 1], None, ALU.mult, None,
                                       ["g_S", "flags"], ["g_S"])
                                chunks = list(range(NT))
                                if dr == 1:
                                    chunks.reverse()
                                def g_p1(ci, c):
                                    cs = slice(c * 128, (c + 1) * 128)
                                    d = {}
                                    if not is_oth:
                                        pA, pAk = PS.next()
                                        for hh in range(2):
                                            mm(pA[:, hh * 128:(hh + 1) * 128], kinv[:, cs], qdM[hh][:, cs], True, True,
                                               ["g_kinv", ("g_qdM", hh)], [pAk])
                                        AT, ATk = ATs.next()
                                        tt("dve", AT[:], pA[:, 0:256], mask[:], ALU.mult, [pAk, mkey], [ATk])
                                        d["AT"] = (AT, ATk)
                                    last = (not is_oth) and ci == NT - 1
                                    if not last:
                                        pT, pTk = PS.next()
                                        pTb = pT[:].bitcast(BF16)
                                        tr(pTb[:, 0:128], kinv[:, cs], ident_b[:], ["g_kinv", "ident_b"], [pTk])
                                        kt_, ktk = kTt.next()
                                        cp("act", kt_[:], pTb[:, 0:128], [pTk], [ktk])
                                        d["kT"] = (kt_, ktk)
                                    return d

                                def g_p2(ci, c, d):
                                    if "kT" in d:
                                        kt_, ktk = d["kT"]
                                        pK, pKk = PS.next()
                                        mm(pK[:, 0:256], kt_[:], vv[:, c, :], True, True, [ktk, "g_v"], [pKk])
                                        dc = E[:, c * 128 + dcol: c * 128 + dcol + 1]
                                        tmpS, tmpSk = tmpSr.next()
                                        act(tmpS[:], pK[:, 0:256], AF.Identity, [pKk, "g_cum"], [tmpSk], scale=dc)
                                        d["tmpS"] = (tmpS, tmpSk, dc)

                                def g_state(ci, c, d):
                                    cs = slice(c * 128, (c + 1) * 128)
                                    if not is_oth:
                                        AT, ATk = d["AT"]
                                        cp("pool", Sbf[:], Sst[:], ["g_S"], ["g_Sbf"])
                                        pO, pOk = PS.next()
                                        for hh in range(2):
                                            hs = slice(hh * 128, (hh + 1) * 128)
                                            mm(pO[:, hs], AT[:, hs], vv[:, c, hs], True, False, [ATk, "g_v"], [pOk])
                                            mm(pO[:, hs], qdM[hh][:, cs], Sbf[:, hs], False, True, [("g_qdM", hh), "g_Sbf"], [pOk])
                                        d["pO"] = (pO, pOk)
                                    if "tmpS" in d:
                                        tmpS, tmpSk, dc = d["tmpS"]
                                        stt(Sst[:], Sst[:], dc, tmpS[:], ALU.mult, ALU.add, ["g_S", "g_cum", tmpSk], ["g_S"])

                                def g_x1(ci, c, d):
                                    if is_oth:
                                        return
                                    pO, pOk = d["pO"]
                                    if dr == 0:
                                        cp("act", ofw[:, c, :], pO[:, 0:256], [pOk], [("g_of", c)])
                                        return
                                    os_, osk = osum.next()
                                    gss, gsk = gssr.next()
                                    d["os"] = (os_, osk)
                                    d["gss"] = (gss, gsk)
                                    tt("dve", os_[:], pO[:, 0:256], ofw[:, c, :], ALU.add, [pOk, ("g_of", c)], [osk])
                                    for hh in range(2):
                                        act(gjunk[:], os_[:, hh * 128:(hh + 1) * 128], AF.Square, [osk], ["g_junk", (gsk, "a")],
                                            accum=gss[:, hh:hh + 1])
                                    act(gss[:, 2:4], gss[:, 0:2], AF.Ln, [(gsk, "a"), "eps_c"], [(gsk, "b")],
                                        bias=eps_c[:], scale=1.0 / 128.0)

                                def g_x2(ci, c, d):
                                    if is_oth or dr == 0:
                                        return
                                    os_, osk = d["os"]
                                    gss, gsk = d["gss"]
                                    act(gss[:, 4:6], gss[:, 2:4], AF.Exp, [(gsk, "b")], [(gsk, "c")], scale=-0.5)
                                    on_, onk = onr.next()
                                    d["on"] = (on_, onk)
                                    for hh in range(2):
                                        ts("pool", on_[:, hh * 128:(hh + 1) * 128], os_[:, hh * 128:(hh + 1) * 128],
                                           gss[:, 4 + hh:5 + hh], None, ALU.mult, None, [osk, (gsk, "c")], [(onk, hh)])

                                def g_x3(ci, c, d):
                                    if is_oth or dr == 0:
                                        return
                                    on_, onk = d["on"]
                                    blk, cc = c // 4, c % 4
                                    ob_, obk = oaT[blk % 2], "g_oaT%d" % (blk % 2)
                                    pX, pXk = PS.next()
                                    for hh in range(2):
                                        tr(pX[:, hh * 128:(hh + 1) * 128], on_[:, hh * 128:(hh + 1) * 128], ident_f[:],
                                           [(onk, hh), "ident_f"], [pXk])
                                    for hh in range(2):
                                        stt(ob_[:, hh, cc * 128:(cc + 1) * 128], pX[:, hh * 128:(hh + 1) * 128], glag[:, 0:1],
                                            ggT[:, hh, c * 128:(c + 1) * 128], ALU.mult, ALU.mult,
                                            [pXk, "glag", "g_gg"], [(obk, cc)])
                                    if cc == 0:
                                        ld(oa_d[pr * 2:(pr + 1) * 2, :, blk * 512:(blk + 1) * 512].rearrange("j p n -> p j n"),
                                           ob_[:], [(obk, q_) for q_ in range(4)], ["oa_d"])

                                ds = {}
                                ds[0] = g_p1(0, chunks[0])
                                if NT > 1:
                                    ds[1] = g_p1(1, chunks[1])
                                g_p2(0, chunks[0], ds[0])
                                for ci, c in enumerate(chunks):
                                    if ci + 2 < NT:
                                        ds[ci + 2] = g_p1(ci + 2, chunks[ci + 2])
                                    if ci + 1 < NT:
                                        g_p2(ci + 1, chunks[ci + 1], ds[ci + 1])
                                    g_state(ci, c, ds[ci])
                                    g_x1(ci, c, ds[ci])
                                    if ci >= 1:
                                        g_x2(ci - 1, chunks[ci - 1], ds[ci - 1])
                                    if ci >= 2:
                                        g_x3(ci - 2, chunks[ci - 2], ds[ci - 2])
                                        del ds[ci - 2]
                                g_x2(NT - 1, chunks[NT - 1], ds[NT - 1])
                                if NT >= 2:
                                    g_x3(NT - 2, chunks[NT - 2], ds[NT - 2])
                                g_x3(NT - 1, chunks[NT - 1], ds[NT - 1])

                print('n_instr before ATT', S.n_instr)
                S.barrier()
                if phase_limit < 4:
                    S.stopped = True
                with ExitStack() as st:
                    Gtab = build_gtab(st)
                    KT = [sbuf(st, "a_KT%d" % i, [128, SA], BF16) for i in range(2)]
                    QT = [sbuf(st, "a_QT%d" % i, [128, NLOC], BF16) for i in range(2)]
                    QTm = [[sbuf(st, "a_QTm%d_%d" % (i, c_), [128, NLOC], BF16) for c_ in range(2)] for i in range(2)]
                    VV = [sbuf(st, "a_V%d" % i, [128, NTA, 128], BF16) for i in range(2)]
                    PT = Ring([sbuf(st, "a_PT%d" % i, [128, 512], BF16) for i in range(6)], "a_PT")
                    dacc = [[sbuf(st, "a_dacc%d_%d" % (s_, e_), [128, 512], F32) for e_ in range(2)] for s_ in range(2)]
                    Rinv = Ring([sbuf(st, "a_R%d" % i, [128, 512], F32) for i in range(2)], "a_R")
                    o1 = sbuf(st, "a_o1", [128, 512], F32)
                    od = sbuf(st, "a_od", [128, 512], F32)
                    tq = sbuf(st, "a_tq", [128, 512], F32)
                    sq = sbuf(st, "a_sq", [128, 512], F32)
                    rr = sbuf(st, "a_rr", [128, 512], F32)
                    obT = Ring([sbuf(st, "a_obT%d" % i, [128, 512], BF16) for i in range(2)], "a_obT")

                    def load_head(h):
                        i = h % 2
                        ld(KT[i][:], dk_d[h], ["dk_d"], ["a_KT%d" % i])
                        ld(QT[i][:], dq_d[h], ["dq_d"], ["a_QT%d" % i])
                        for c_ in range(2):
                            ts("pool" if c_ else "dve", QTm[i][c_][:], QT[i][:], hm[:, c_:c_ + 1], None, ALU.mult, None,
                               ["a_QT%d" % i, "hm"], [("a_QTm", i, c_)])
                        ld(VV[i][:], dv_d[:, h * 128:(h + 1) * 128].rearrange("(t p) c -> p t c", p=128),
                           ["dv_d"], ["a_V%d" % i])

                    sps_i = [0]

                    def sps_next():
                        j = sps_i[0] % 4
                        sps_i[0] += 1
                        return pbanks[4 + j], "pb%d" % (4 + j)

                    aps_i = [0]

                    ones_b = sbuf(st, "a_ones_b", [128, 128], BF16)
                    cp("dve", ones_b[:], ones_f[:], ["ones_f"], ["a_ones_b"])

                    def stage1(h, i, qb, c, kt):
                        is_oth = kt >= NT
                        ktl = kt - NT if is_oth else kt
                        dlt = 128 * ktl - 512 * qb
                        tab = None
                        cbi = 0
                        if not is_oth:
                            if -128 <= dlt <= 512:
                                tab, dd = 0, dlt
                            else:
                                cbi = 0 if dlt < 0 else 1
                        else:
                            if dlt + NLOC == 512:
                                tab, dd = 1, 512
                            elif dlt - NLOC == -128:
                                tab, dd = 2, -128
                            else:
                                cbi = 2
                        p, pk = sps_next()
                        mm(p[:], KT[i][:, kt * 128:(kt + 1) * 128], QTm[i][c][:, qb * 512:(qb + 1) * 512], True, tab is None,
                           ["a_KT%d" % i, ("a_QTm", i, c)], [pk])
                        pt_, ptk = PT.next()
                        if tab is not None:
                            s0 = GC0 - dd
                            mm(p[:], ident_b[:], Gtab[:, tab * 4 + h, s0:s0 + 512], False, True,
                               ["ident_b", ("Gtab", tab * 4 + h)], [pk])
                            act(pt_[:], p[:], AF.Exp, [pk], [ptk])
                        else:
                            act(pt_[:], p[:], AF.Exp, [pk, "cb"], [ptk], bias=cb[:, cbi * 4 + h: cbi * 4 + h + 1])
                        return pt_, ptk

                    def stage2(h, i, qb, c, kt, pt_, ptk):
                        OT, OTk = pbanks[c], "pb%d" % c
                        mm(OT[:], VV[i][:, kt, :], pt_[:], kt == 0, kt == NTA - 1, ["a_V%d" % i, ptk], [OTk])
                        e_ = kt % 3
                        if e_ == 2:
                            pS, pSk = pbanks[2 + c], "pb%d" % (2 + c)
                            mm(pS[:], ones_b[:], pt_[:], kt == 2, False, ["a_ones_b", ptk], [pSk])
                        else:
                            dst, dk_ = dacc[c][e_], ("a_dacc", c, e_)
                            eng = "dve" if e_ == 0 else "pool"
                            if kt < 2:
                                cp(eng, dst[:], pt_[:], [ptk], [dk_])
                            else:
                                tt(eng, dst[:], dst[:], pt_[:], ALU.add, [dk_, ptk], [dk_])
                        if kt == NTA - 1:
                            finalize(h, qb, c)

                    def finalize(h, qb, c):
                        OT, OTk = pbanks[c], "pb%d" % c
                        pS, pSk = pbanks[2 + c], "pb%d" % (2 + c)
                        mm(pS[:], ones_f[:], dacc[c][0][:], False, False, ["ones_f", ("a_dacc", c, 0)], [pSk])
                        mm(pS[:], ones_f[:], dacc[c][1][:], False, True, ["ones_f", ("a_dacc", c, 1)], [pSk])
                        R, Rk = Rinv.next()
                        S.op("dve", lambda: nc.vector.reciprocal(R[:], pS[:]), [pSk], [Rk])
                        if c == 0:
                            tt("dve", o1[:], OT[:], R[:], ALU.mult, [OTk, Rk], ["a_o1"])
                        else:
                            stt(tq[:], OT[:], nlam[:, 0:1], R[:], ALU.mult, ALU.mult, [OTk, "nlam", Rk], ["a_tq"])
                            tt("pool", od[:], tq[:], o1[:], ALU.add, ["a_tq", "a_o1"], ["a_od"])
                            tt("pool", sq[:], od[:], od[:], ALU.mult, ["a_od"], ["a_sq"])
                            pQ, pQk = sps_next()
                            mm(pQ[:], ones_f[:], sq[:], True, True, ["ones_f", "a_sq"], [pQk])
                            act(rr[:], pQ[:], AF.Ln, [pQk, "eps_c"], ["a_rr"], bias=eps_c[:], scale=1.0 / 128.0)
                            act(rr[:], rr[:], AF.Exp, ["a_rr"], ["a_rr"], scale=-0.5)
                            ob_, obk = obT.next()
                            stt(ob_[:], od[:], subgs[:, 0:1], rr[:], ALU.mult, ALU.mult, ["a_od", "subgs", "a_rr"], [obk])
                            ld(ob_d[h, :, qb * 512:(qb + 1) * 512], ob_[:], [obk], ["ob_d"])

                    LOOK = 2
                    load_head(0)
                    for h in range(4):
                        if h + 1 < 4:
                            load_head(h + 1)
                        i = h % 2
                        pend = []
                        for qb in range(NB):
                            for c in range(2):
                                for kt in range(NTA):
                                    pend.append((qb, c, kt) + stage1(h, i, qb, c, kt))
                                    if len(pend) > LOOK:
                                        qb_, c_, kt_, pt_, ptk = pend.pop(0)
                                        stage2(h, i, qb_, c_, kt_, pt_, ptk)
                        while pend:
                            qb_, c_, kt_, pt_, ptk = pend.pop(0)
                            stage2(h, i, qb_, c_, kt_, pt_, ptk)

                print('n_instr before MERGE', S.n_instr)
                S.barrier()
                if phase_limit < 5:
                    S.stopped = True
                with ExitStack() as st:
                    w_a = sbuf(st, "m_wa", [128, 4, D], BF16)
                    w_b = sbuf(st, "m_wb", [128, 4, D], BF16)
                    w_o = sbuf(st, "m_wo", [128, 8, D], BF16)
                    w_r = sbuf(st, "m_wr", [128, 8, 36], F32)
                    ld(w_a[:], Wl["w_a"].rearrange("(k p) n -> p k n", p=128), [], ["m_wa"], q="pool")
                    ld(w_b[:], Wl["w_b"].rearrange("(k p) n -> p k n", p=128), [], ["m_wb"], q="pool")
                    ld(w_o[:], Wl["w_o"].rearrange("(k p) n -> p k n", p=128), [], ["m_wo"], q="pool")
                    ld(w_r[:], Wl["w_r"].rearrange("(k p) n -> p k n", p=128), [], ["m_wr"])
                    oaB = sbuf(st, "m_oa", [128, 4, 512], BF16)
                    obB = sbuf(st, "m_ob", [128, 4, 512], BF16)
                    gaB = sbuf(st, "m_ga", [128, 8, 512], BF16)
                    gbB = sbuf(st, "m_gb", [128, 8, 512], BF16)
                    xB = sbuf(st, "m_x", [128, 4, D], F32)
                    x1B = sbuf(st, "m_x1", [128, 4, D], F32)
                    mg = sbuf(st, "m_mg", [128, 8, 512], BF16)
                    t1 = Ring([sbuf(st, "m_t1%d" % i, [128, 512], F32) for i in range(2)], "m_t1")
                    t2 = Ring([sbuf(st, "m_t2%d" % i, [128, 512], F32) for i in range(2)], "m_t2")
                    a2b = sbuf(st, "m_a2b", [128, D], F32)
                    sh2b = sbuf(st, "m_sh2b", [128, D], F32)
                    ld(a2b[:], bass.AP(rows_d.tensor, 0, [[0, 128], [1, D]]), ["rows_d"], ["m_a2b"])
                    ld(sh2b[:], bass.AP(rows_d.tensor, D, [[0, 128], [1, D]]), ["rows_d"], ["m_sh2b"])
                    htmp = Ring([sbuf(st, "m_htmp%d" % i_, [128, D], F32) for i_ in range(2)], "m_htmp")
                    h2tokD = [[sbuf(st, "m_h2tok%d_%d" % (b_, i_), [128, D], BF16) for i_ in range(4)] for b_ in range(2)]
                    rbase = sbuf(st, "r_base", [128, NE], F32)
                    S.op("dve", lambda: nc.vector.memset(rbase[:], 0.0), [], ["r_base"])
                    rMT = [sbuf(st, "r_M%d" % t_, [128, 3, NE], F32) for t_ in range(4)]
                    rrkT = [sbuf(st, "r_rk%d" % t_, [128, 5, NE], F32) for t_ in range(4)]
                    rslT = [sbuf(st, "r_sl%d" % t_, [128, 8], F32) for t_ in range(4)]
                    rbt = sbuf(st, "r_bt", [128, 4, NE], F32)
                    sidx = Ring([sbuf(st, "r_sidx%d" % i_, [128, 2], mybir.dt.int32) for i_ in range(4)], "r_sidx")
                    h2T32 = sbuf(st, "m_h2T32", [128, 8, 512], F32)
                    scr2 = dict(ss=sbuf(st, "m_ss", [128, 12], F32), junk=sbuf(st, "m_junk", [128, D], BF16),
                                ac=a2c)
                    rlD = [[sbuf(st, "r_l%d_%d" % (b_, t_), [128, 36], F32) for t_ in range(4)] for b_ in range(2)]
                    rwT = [sbuf(st, "r_w%d" % t_, [128, 64], F32) for t_ in range(4)]
                    rmlT = [sbuf(st, "r_ml%d" % t_, [128, 32], F32) for t_ in range(4)]
                    rm8T = [sbuf(st, "r_m8%d" % t_, [128, 8], F32) for t_ in range(4)]
                    rjunkT = [sbuf(st, "r_junk%d" % t_, [128, 4], F32) for t_ in range(4)]

                    def lockstep(gens):
                        gens = list(gens)
                        while gens:
                            for g_ in list(gens):
                                try:
                                    next(g_)
                                except StopIteration:
                                    gens.remove(g_)
                    def router_block(i):
                        pIs = [None] * 4

                        def rt_A(t):
                            tg = i * 4 + t
                            rl, rw, rml, rm8, rjunk, rM = rlD[i % 2][t], rwT[t], rmlT[t], rm8T[t], rjunkT[t], rMT[t]
                            K = lambda n: (n, t)
                            KL = ("r_l", i % 2, t)
                            S.op("dve", lambda: nc.vector.reduce_max(rw[:, 0:1], rl[:, 0:4], axis=AX.X), [KL], [K("r_w0")])
                            yield
                            ts("dve", rw[:, 1:2], rw[:, 0:1], -1.0, None, ALU.mult, None, [K("r_w0")], [K("r_w1")])
                            yield
                            act(rjunk[:], rl[:, 0:4], AF.Exp, [KL, K("r_w1")], [K("r_junk")], bias=rw[:, 1:2], accum=rw[:, 2:3])
                            yield
                            S.op("dve", lambda: nc.vector.reciprocal(rw[:, 3:4], rw[:, 2:3]), [K("r_junk")], [K("r_w3")])
                            yield
                            ts("dve", rw[:, 4:8], rl[:, 0:4], rw[:, 0:1], None, ALU.is_ge, None, [KL, K("r_w0")], [K("r_w4")])
                            yield
                            ts("dve", rw[:, 8:12], rw[:, 4:8], -1.0, 1e30, ALU.add, ALU.mult, [K("r_w4")], [K("r_w8")])
                            yield
                            for g in range(4):
                                ts("dve", rml[:, g * 8:(g + 1) * 8], rl[:, 4 + g * 8: 4 + (g + 1) * 8], rw[:, 8 + g: 9 + g], None,
                                   ALU.add, None, [KL, K("r_w8")], [K("r_ml")])
                                yield
                            S.op("dve", lambda: nc.vector.max(out=rm8[:], in_=rml[:]), [K("r_ml")], [K("r_m8")])
                            yield
                            tt("dve", rw[:, 12:13], rm8[:, 1:2], rm8[:, 0:1], ALU.subtract, [K("r_m8")], [K("r_w12")])
                            yield
                            act(rw[:, 13:14], rw[:, 12:13], AF.Exp, [K("r_w12")], [K("r_w13")])
                            yield
                            ts("dve", rw[:, 14:15], rw[:, 13:14], 1.0, None, ALU.add, None, [K("r_w13")], [K("r_w14")])
                            yield
                            S.op("dve", lambda: nc.vector.reciprocal(rw[:, 15:16], rw[:, 14:15]), [K("r_w14")], [K("r_w15")])
                            yield
                            tt("dve", rw[:, 16:17], rw[:, 15:16], rw[:, 3:4], ALU.mult, [K("r_w15"), K("r_w3")], [K("r_w16")])
                            yield
                            tt("dve", rw[:, 17:18], rw[:, 16:17], rw[:, 13:14], ALU.mult, [K("r_w16"), K("r_w13")], [K("r_w17")])
                            yield
                            cp("dve", gat[:, tg, 0:2], rw[:, 16:18], [K("r_w16"), K("r_w17")], [("gat", tg)])
                            yield
                            ts("dve", rM[:, 0, :], rml[:], rm8[:, 0:1], None, ALU.is_equal, None, [K("r_ml"), K("r_m8")], [K("r_M0")])
                            yield
                            ts("dve", rM[:, 1, :], rml[:], rm8[:, 1:2], None, ALU.is_equal, None, [K("r_ml"), K("r_m8")], [K("r_M1")])
                            yield
                            tt("dve", rM[:, 2, :], rM[:, 0, :], rM[:, 1, :], ALU.add, [K("r_M0"), K("r_M1")], [K("r_M2")])
                            yield
                            pI, pIk = PS.next()
                            mm(pI[:, 0:NE], tril_f[:, 0:128], rM[:, 2, :], True, True, ["tril_f", K("r_M2")], [pIk])
                            mm(pI[:, NE:2 * NE], ones_f[:], rM[:, 2, :], True, True, ["ones_f", K("r_M2")], [pIk])
                            pIs[t] = (pI, pIk)
                            yield

                        def rt_B(t):
                            tg = i * 4 + t
                            rM, rrk, rsl = rMT[t], rrkT[t], rslT[t]
                            K = lambda n: (n, t)
                            pI, pIk = pIs[t]
                            tt("dve", rrk[:, 0, :], pI[:, 0:NE], rbt[:, t, :], ALU.add, [pIk, ("r_bt", t)], [K("r_rk0")])
                            yield
                            tt("dve", rrk[:, 0, :], rrk[:, 0, :], rM[:, 2, :], ALU.subtract, [K("r_rk0"), K("r_M2")], [K("r_rk0")])
                            yield
                            ts("dve", rrk[:, 1, :], rrk[:, 0, :], float(CAP), None, ALU.is_lt, None, [K("r_rk0")], [K("r_rk1")])
                            yield
                            tt("dve", rrk[:, 2, :], rrk[:, 0, :], ecap[:], ALU.add, [K("r_rk0"), "ecap"], [K("r_rk2")])
                            yield
                            for k_ in range(2):
                                tt("dve", rrk[:, 3, :], rrk[:, 2, :], rM[:, k_, :], ALU.mult, [K("r_rk2"), K("r_M%d" % k_)], [K("r_rk3")])
                                yield
                                S.op("dve", lambda k_=k_: nc.vector.reduce_sum(rsl[:, k_:k_ + 1], rrk[:, 3, :], axis=AX.X),
                                     [K("r_rk3")], [K("r_sl")])
                                yield
                                tt("dve", rrk[:, 4, :], rrk[:, 1, :], rM[:, k_, :], ALU.mult, [K("r_rk1"), K("r_M%d" % k_)], [K("r_rk4")])
                                yield
                                S.op("dve", lambda k_=k_: nc.vector.reduce_sum(rsl[:, 2 + k_:3 + k_], rrk[:, 4, :], axis=AX.X),
                                     [K("r_rk4")], [K("r_sl")])
                                yield
                            ts("dve", rsl[:, 4:6], rsl[:, 0:2], trash[:, 0:1], None, ALU.subtract, None, [K("r_sl"), "trash"], [K("r_sl")])
                            yield
                            tt("dve", rsl[:, 4:6], rsl[:, 4:6], rsl[:, 2:4], ALU.mult, [K("r_sl")], [K("r_sl")])
                            yield
                            ts("dve", rsl[:, 4:6], rsl[:, 4:6], trash[:, 0:1], None, ALU.add, None, [K("r_sl"), "trash"], [K("r_sl")])
                            yield
                            ts("dve", rsl[:, 6:8], rsl[:, 0:2], -float(NE * CAP), None, ALU.add, None, [K("r_sl")], [K("r_sl")])
                            yield
                            tt("dve", rsl[:, 6:8], rsl[:, 6:8], rsl[:, 2:4], ALU.mult, [K("r_sl")], [K("r_sl")])
                            yield
                            ts("dve", rsl[:, 6:8], rsl[:, 6:8], float(NE * CAP), None, ALU.add, None, [K("r_sl")], [K("r_sl")])
                            yield
                            si_, sik = sidx.next()
                            cp("dve", si_[:], rsl[:, 4:6], [K("r_sl")], [sik])
                            yield
                            cp("dve", gix[:, tg, :], rsl[:, 6:8], [K("r_sl")], [("gix", tg)])
                            yield
                            for k_ in range(2):
                                S.dma("pool", (lambda k_=k_, si_=si_: nc.gpsimd.indirect_dma_start(
                                    out=Xg[:, :], out_offset=bass.IndirectOffsetOnAxis(ap=si_[:, k_:k_ + 1], axis=0),
                                    in_=h2tokD[i % 2][t][:, :], in_offset=None)),
                                    [("m_h2tok", i % 2, t), sik], [("Xg", tg, k_)])
                            yield

                        lockstep(rt_A(t) for t in range(4))
                        cp("dve", rbt[:, 0, :], rbase[:], ["r_base"], [("r_bt", 0)])
                        for t in range(1, 4):
                            tt("dve", rbt[:, t, :], rbt[:, t - 1, :], pIs[t - 1][0][:, NE:2 * NE], ALU.add,
                               [("r_bt", t - 1), pIs[t - 1][1]], [("r_bt", t)])
                        tt("dve", rbase[:], rbt[:, 3, :], pIs[3][0][:, NE:2 * NE], ALU.add, [("r_bt", 3), pIs[3][1]], ["r_base"])
                        lockstep(rt_B(t) for t in range(4))

                    for i in range(NB):
                        bs = slice(i * 512, (i + 1) * 512)
                        ld(oaB[:], oa_d[:, :, bs].rearrange("j p n -> p j n"), ["oa_d"], ["m_oa"])
                        ld(obB[:], ob_d[:, :, bs].rearrange("j p n -> p j n"), ["ob_d"], ["m_ob"])
                        ld(gaB[:], ga_d[:, :, bs].rearrange("j p n -> p j n"), ["ga_d"], ["m_ga"])
                        ld(gbB[:], gb_d[:, :, bs].rearrange("j p n -> p j n"), ["gb_d"], ["m_gb"])
                        ld(xB[:], x_loc_block(i).rearrange("(t p) d -> p t d", p=128),
                           [("xmid", t_) for t_ in range(NT)] if li > 0 else [], ["m_x"])
                        for m in range(8):
                            pa, pak = PS.next()
                            for k in range(4):
                                mm(pa[:], w_a[:, k, m * 128:(m + 1) * 128], oaB[:, k, :], k == 0, k == 3, ["m_wa", "m_oa"], [pak])
                            pb_, pbk = PS.next()
                            for k in range(4):
                                mm(pb_[:], w_b[:, k, m * 128:(m + 1) * 128], obB[:, k, :], k == 0, k == 3, ["m_wb", "m_ob"], [pbk])
                            ta, tak = t1.next()
                            tb, tbk = t2.next()
                            tt("dve", ta[:], pa[:], gaB[:, m, :], ALU.mult, [pak, "m_ga"], [tak])
                            tt("dve", tb[:], pb_[:], gbB[:, m, :], ALU.mult, [pbk, "m_gb"], [tbk])
                            tt("pool", mg[:, m, :], ta[:], tb[:], ALU.add, [tak, tbk], [("m_mg", m)])
                        for t in range(4):
                            for n in range(2):
                                p, pk = PS.next()
                                for k in range(8):
                                    mm(p[:], mg[:, k, t * 128:(t + 1) * 128], w_o[:, k, n * 512:(n + 1) * 512], k == 0, k == 7,
                                       [("m_mg", k), "m_wo"], [pk])
                                ta, tak = t1.next()
                                tt("dve", ta[:], p[:], g1b[:, n * 512:(n + 1) * 512], ALU.mult, [pk, "g1b"], [tak])
                                tt("pool", x1B[:, t, n * 512:(n + 1) * 512], ta[:], xB[:, t, n * 512:(n + 1) * 512], ALU.add,
                                   [tak, "m_x"], ["m_x1"])
                        ld(x1_d[bs, :].rearrange("(t p) d -> p t d", p=128), x1B[:], ["m_x1"], ["x1_d"])
                        norm_block(x1B, "m_x1", "a2c", 24, None, "m_h2T", scr2, hT32=h2T32)
                        for t in range(4):
                            hm_, hmk = htmp.next()
                            tt("dve", hm_[:], x1B[:, t, :], a2b[:], ALU.mult, ["m_x1", "m_a2b"], [hmk])
                            tt("pool", h2tokD[i % 2][t][:], hm_[:], sh2b[:], ALU.add, [hmk, "m_sh2b"], [("m_h2tok", i % 2, t)])
                        for t in range(4):
                            p, pk = PS.next()
                            for k in range(8):
                                mm(p[:, 0:36], h2T32[:, k, t * 128:(t + 1) * 128], w_r[:, k, :], k == 0, k == 7,
                                   [("m_h2T32", k), "m_wr"], [pk])
                            tt("dve", rlD[i % 2][t][:], p[:, 0:36], brb[:], ALU.add, [pk, "brb"], [("r_l", i % 2, t)])
                        if i > 0:
                            router_block(i - 1)
                        if i == NB - 1:
                            router_block(i)


                print('n_instr before MOE', S.n_instr)
                S.barrier()
                if phase_limit < 6:
                    S.stopped = True
                with ExitStack() as st:
                    CS = CAP // 128
                    CB = CAP // 512
                    w1s = [sbuf(st, "e_w1%d" % i, [128, 8, 2 * DEXP], BF16) for i in range(2)]
                    w2s = [sbuf(st, "e_w2%d" % i, [128, 4, D], BF16) for i in range(2)]
                    xtok = [sbuf(st, "e_xtok%d" % i, [128, 4, D], BF16) for i in range(2)]
                    xT = [sbuf(st, "e_xT%d" % i, [128, 8, 512], BF16) for i in range(2)]
                    sg = Ring([sbuf(st, "e_sg%d" % i, [128, 512], BF16) for i in range(3)], "e_sg")
                    actT = [sbuf(st, "e_act%d" % i, [128, 4, 512], BF16) for i in range(2)]
                    ybuf = Ring([sbuf(st, "e_y%d" % i, [128, D], F32) for i in range(3)], "e_y")
                    xc = Ring([sbuf(st, "e_xc%d" % i, [128, D], F32) for i in range(4)], "e_xc")
                    y1r = Ring([sbuf(st, "e_y1%d" % i, [128, D], F32) for i in range(4)], "e_y1")
                    y2r = Ring([sbuf(st, "e_y2%d" % i, [128, D], F32) for i in range(4)], "e_y2")
                    xg_keys = [("Xg", tg_, k_) for tg_ in range(NT) for k_ in range(2)]
                    yg_keys = []

                    def load_w(e):
                        i = e % 2
                        ld(w1s[i][:], Wl["w1"][e].rearrange("(k p) n -> p k n", p=128), [], ["e_w1%d" % i], q="pool")
                        ld(w2s[i][:], Wl["w2"][e].rearrange("(k p) n -> p k n", p=128), [], ["e_w2%d" % i], q="pool")
                        for k in range(4):
                            tt("pool", w2s[i][:, k, :], w2s[i][:, k, :], g2b[:], ALU.mult, ["e_w2%d" % i, "g2b"], ["e_w2%d" % i])

                    nblk = 0

                    def load_xtok(e, blk):
                        j = (e * CB + blk) % 2
                        r0 = e * CAP + blk * 512
                        ld(xtok[j][:], Xg[r0:r0 + 512, :].rearrange("(s p) d -> p s d", p=128), xg_keys, ["e_xtok%d" % j])

                    load_w(0)
                    load_xtok(0, 0)
                    for e in range(NE):
                        if e + 1 < NE:
                            load_w(e + 1)
                        i = e % 2
                        w1, w1k, w2, w2k = w1s[i], "e_w1%d" % i, w2s[i], "e_w2%d" % i
                        for blk in range(CB):
                            j_ = nblk % 2
                            nblk += 1
                            if blk + 1 < CB:
                                load_xtok(e, blk + 1)
                            elif e + 1 < NE:
                                load_xtok(e + 1, 0)
                            xt_, xtk = xtok[j_], "e_xtok%d" % j_
                            xT_, xTk = xT[j_], "e_xT%d" % j_
                            for k in range(8):
                                pT, pTk = PS.next()
                                pTb = pT[:].bitcast(BF16)
                                for s_i in range(4):
                                    tr(pTb[:, s_i * 128:(s_i + 1) * 128], xt_[:, s_i, k * 128:(k + 1) * 128], ident_b[:],
                                       [xtk, "ident_b"], [pTk])
                                cp("dve" if k % 2 else "act", xT_[:, k, :], pTb[:, 0:512], [pTk], [(xTk, k)])
                            at, atk = actT[j_], "e_act%d" % j_
                            for j in range(4):
                                pg, pgk = PS.next()
                                for k in range(8):
                                    mm(pg[:], w1[:, k, j * 128:(j + 1) * 128], xT_[:, k, :], k == 0, k == 7, [w1k, (xTk, k)], [pgk])
                                pu, puk = PS.next()
                                for k in range(8):
                                    mm(pu[:], w1[:, k, DEXP + j * 128: DEXP + (j + 1) * 128], xT_[:, k, :], k == 0, k == 7,
                                       [w1k, (xTk, k)], [puk])
                                s_, sk = sg.next()
                                act(s_[:], pg[:], AF.Silu, [pgk], [sk])
                                tt("dve", at[:, j, :], pu[:], s_[:], ALU.mult, [puk, sk], [(atk, j)])
                            for t in range(4):
                                yb, ybk = ybuf.next()
                                for n in range(2):
                                    py, pyk = PS.next()
                                    for j in range(4):
                                        mm(py[:], at[:, j, t * 128:(t + 1) * 128], w2[:, j, n * 512:(n + 1) * 512], j == 0, j == 3,
                                           [(atk, j), w2k], [pyk])
                                    cp("act" if n else "dve", yb[:, n * 512:(n + 1) * 512], py[:], [pyk], [(ybk, n)])
                                r0 = e * CAP + blk * 512 + t * 128
                                yk = ("Yg", e, blk, t)
                                yg_keys.append(yk)
                                ld(Yg[r0:r0 + 128, :], yb[:], [(ybk, 0), (ybk, 1)], [yk])
                    def emit_cc(j):
                        rg = [[2 * g_, 2 * g_ + 1] for g_ in range(NCORES // 2)]
                        S.cc(lambda: nc.gpsimd.collective_compute(
                            "AllGather", ALU.bypass, replica_groups=rg, ins=[xmid_c[j].opt()], outs=[xg_c[j].opt()]),
                            [("xmid", t_) for t_ in range(4 * j, 4 * j + 4)], ["xg"])

                    for tg in range(NT):
                        x_, xk_ = xc.next()
                        ld(x_[:], x1_d[tg * 128:(tg + 1) * 128, :], ["x1_d"], [xk_])
                        ya, yak = y1r.next()
                        yb2, ybk2 = y2r.next()
                        for (yt_, ytk, k_) in ((ya, yak, 0), (yb2, ybk2, 1)):
                            S.dma("pool", (lambda yt_=yt_, k_=k_: nc.gpsimd.indirect_dma_start(
                                out=yt_[:, :], out_offset=None, in_=Yg[:, :],
                                in_offset=bass.IndirectOffsetOnAxis(ap=gix[:, tg, k_:k_ + 1], axis=0))),
                                yg_keys + ["Yg", ("gix", tg)], [ytk])
                        stt(x_[:], ya[:], gat[:, tg, 0:1], x_[:], ALU.mult, ALU.add, [yak, ("gat", tg), xk_], [xk_])
                        stt(x_[:], yb2[:], gat[:, tg, 1:2], x_[:], ALU.mult, ALU.add, [ybk2, ("gat", tg), xk_], [xk_])
                        if is_last:
                            ld(x_out[tg * 128:(tg + 1) * 128, :], x_[:], [xk_], [("xout", tg)])
                        else:
                            ld(xmid_c[tg // 4][(tg % 4) * 128:(tg % 4 + 1) * 128, :], x_[:], [xk_], [("xmid", tg)])
                            if tg % 4 == 3 and tg >= 7:
                                emit_cc(tg // 4 - 1)
                    if not is_last:
                        emit_cc(NB - 1)
        except _Stop:
            pass
        S.finish("sp")
    return nc, S


def _t5_bucket_np(rel):
    import jax
    import jax.numpy as jnp
    cpu = jax.devices("cpu")[0]
    with jax.default_device(cpu):
        rel = jnp.asarray(rel, dtype=jnp.int32)
        nb = 16
        max_exact = 8
        ret = jnp.where(rel > 0, nb, 0)
        n = jnp.abs(rel)
        nf = jnp.maximum(n, 1).astype(jnp.float32)
        large = max_exact + (jnp.log(nf / max_exact) / math.log(128 / max_exact) * (nb - max_exact)).astype(jnp.int32)
        large = jnp.minimum(large, nb - 1)
        out = ret + jnp.where(n < max_exact, n, large)
        return np.asarray(out)


def _onehot(b):
    oh = np.zeros((32, b.shape[0]), np.float32)
    oh[b, np.arange(b.shape[0])] = 1.0
    return oh


def _core_consts(hf, NLOC):
    shift = NLOC if hf == 0 else -NLOC
    n = np.arange(TVW)
    base = GC0 + 127 - n
    tabs = [base, base - NLOC + shift, base + NLOC + shift]
    oh = np.concatenate([_onehot(_t5_bucket_np(t)) for t in tabs], axis=1)
    cvals = _t5_bucket_np(np.array([-100000, 100000, shift]))
    ohc = np.concatenate([np.repeat(_onehot(cvals[k:k + 1]), 128, axis=1) for k in range(3)], axis=1)
    flags = np.zeros((128, 2), np.float32)
    flags[:, 0] = 1.0 if hf == 1 else 0.0
    flags[:, 1] = 1.0 if hf == 0 else 0.0
    return oh, ohc, flags


def _col(v, k):
    return np.ascontiguousarray(np.asarray(v, np.float32).reshape(k, 128).T)


def make_in_maps(inp, SEQ, layers, x_override=None):
    NLOC = SEQ // 2
    B = inp["x"].shape[0]
    f = lambda a: np.ascontiguousarray(np.asarray(a, np.float32))
    ident = np.eye(128, dtype=np.float32)
    jmat = np.ascontiguousarray(ident[::-1])
    bones = np.zeros((128, 128), np.float32)
    bones[:64, :64] = 1.0
    bones[64:, 64:] = 1.0
    jj, ii = np.meshgrid(np.arange(128), np.arange(128), indexing="ij")
    tril = (jj <= ii).astype(np.float32)
    triu = (jj > ii).astype(np.float32)
    hmask = np.zeros((128, 2), np.float32)
    hmask[:64, 0] = 1.0
    hmask[64:, 1] = 1.0
    ecap = np.tile((np.arange(NE, dtype=np.float32) * CAP)[None, :], (128, 1)).astype(np.float32)
    trash = (NE * CAP + np.arange(128, dtype=np.float32)).reshape(128, 1).astype(np.float32)
    shared = dict(trash=trash, ecap=ecap, hmask=hmask, ident=ident, jmat=jmat, bones=bones, ones=np.ones((128, 128), np.float32),
                  tril=np.concatenate([tril, tril], 1), triu=np.concatenate([triu, triu], 1),
                  rel_bias=f(inp["rel_bias"]))
    for l in layers:
        shared.update({
            "w_ada%d" % l: f(inp["w_ada"][l]), "b_ada%d" % l: f(inp["b_ada"][l]).reshape(1, -1),
            "n1g%d" % l: _col(inp["norm1_g"][l], 8), "n2g%d" % l: _col(inp["norm2_g"][l], 8),
            "w_in%d" % l: f(inp["w_in"][l]), "w_up%d" % l: f(inp["gla_w_up"][l]),
            "b_up%d" % l: _col(np.asarray(inp["gla_b_up"][l]).reshape(-1), 4),
            "glag%d" % l: f(inp["gla_norm_g"][l]).reshape(128, 1),
            "qng%d" % l: np.ascontiguousarray(np.tile(f(inp["diff_qnorm_g"][l]), 2).reshape(128, 1)),
            "kng%d" % l: np.ascontiguousarray(np.tile(f(inp["diff_knorm_g"][l]), 2).reshape(128, 1)),
            "lamv%d" % l: f(inp["diff_lambda"][l]).reshape(1, 256),
            "subg%d" % l: f(inp["diff_subnorm_g"][l]).reshape(128, 1),
            "w_a%d" % l: f(inp["w_branch_a"][l]), "w_b%d" % l: f(inp["w_branch_b"][l]), "w_o%d" % l: f(inp["w_out"][l]),
            "w_r%d" % l: np.ascontiguousarray(np.concatenate([f(inp["w_router_group"][l]), f(inp["w_router_expert"][l])], 1)),
            "b_r%d" % l: np.concatenate([f(inp["b_router_group"][l]), f(inp["b_router_expert"][l])]).reshape(1, 36),
            "n2grow%d" % l: f(inp["norm2_g"][l]).reshape(1, D),
            "w1_%d" % l: f(inp["w_expert_in"][l]), "w2_%d" % l: f(inp["w_expert_out"][l]),
        })
    x = f(inp["x"]) if x_override is None else x_override
    maps = []
    consts = [_core_consts(hf, NLOC) for hf in range(2)]
    for core in range(2 * B):
        b, hf = core // 2, core % 2
        oh, ohc, flags = consts[hf]
        m = dict(shared)
        m["x_loc"] = np.ascontiguousarray(x[b, hf * NLOC:(hf + 1) * NLOC])
        m["x_oth"] = np.ascontiguousarray(x[b, (1 - hf) * NLOC:(2 - hf) * NLOC])
        m["c_col"] = _col(inp["c"][b], 8)
        m["oh"], m["ohc"], m["flags"] = oh, ohc, flags
        maps.append(m)
    return maps


_CACHE = {}


def run_layers(inp, SEQ, layer_groups, dbg=False, phase_limit=99):
    B = inp["x"].shape[0]
    NLOC = SEQ // 2
    x = np.ascontiguousarray(np.asarray(inp["x"], np.float32))
    for grp in layer_groups:
        key = (SEQ, tuple(grp))
        if key not in _CACHE:
            _CACHE[key] = build_program(SEQ, grp, dbg=dbg, phase_limit=phase_limit, NCORES=2 * B)[0]
        nc = _CACHE[key]
        maps = make_in_maps(inp, SEQ, grp, x_override=x)
        res = run_bass_kernel_spmd(nc, maps, core_ids=list(range(2 * B)))
        xn = np.empty_like(x)
        if dbg:
            return res
        for core in range(2 * B):
            b, hf = core // 2, core % 2
            xn[b, hf * NLOC:(hf + 1) * NLOC] = res.results[core]["x_out"]
        x = xn
    return x


def kernel(**inputs):
    SEQ = inputs["x"].shape[1]
    return run_layers(inputs, SEQ, [[0, 1]])
```

```python
import math
from contextlib import ExitStack

import numpy as np
import concourse.bass as bass
import concourse.mybir as mybir
from concourse.bass_utils import run_bass_kernel_spmd

F32 = mybir.dt.float32
BF16 = mybir.dt.bfloat16
AF = mybir.ActivationFunctionType
ALU = mybir.AluOpType
AX = mybir.AxisListType

D = 1024
DEPTH = 2
NE = 32
CAP = 1024
DEXP = 512
RMS_EPS = 1e-6
GW = 1152
GC0 = 512
TVW = 1280
C_GQ, C_GK, C_GV, C_GG, C_LRF, C_LRB, C_DQ, C_DK, C_DV, C_GA, C_GB = (
    0, 256, 512, 1024, 1536, 1552, 1568, 2080, 2592, 3104, 4128)
D_IN = 5152


class Sched:
    ENG_SEM_MAX = 30000

    def __init__(self, nc, stack, n_dma_sems=32):
        self.nc = nc
        self.stack = stack
        self.engs = {"pe": nc.tensor, "act": nc.scalar, "dve": nc.vector,
                     "pool": nc.gpsimd, "sp": nc.sync}
        self.sem = {}
        self.cnt = {}
        self.old = {}
        self.nsem = 0
        for e in self.engs:
            self._new_eng_sem(e)
        self.known = {e: {} for e in self.engs}
        self.dma_sems = [self._mk_sem("dq") for _ in range(n_dma_sems)]
        self.dma_cnt = [0] * n_dma_sems
        self.dma_rr = 0
        self.res = {}
        self.n_instr = 0
        self.stopped = False
        import os
        self.max_instr = int(os.environ.get('BASS_MAXI', '100000000'))

    def _mk_sem(self, name):
        self.nsem += 1
        return self.stack.enter_context(self.nc.semaphore("%s_%d" % (name, self.nsem)))

    def _new_eng_sem(self, e):
        if e in self.sem:
            self.old[e] = (self.sem[e], self.cnt[e], e)
        self.sem[e] = self._mk_sem("s" + e)
        self.cnt[e] = 0

    def _wait(self, eng, tok):
        sem, val, _ = tok
        k = self.known[eng]
        sid = id(sem)
        if k.get(sid, 0) >= val:
            return
        self.engs[eng].wait_ge(sem, val)
        k[sid] = val

    def _deps(self, eng, reads, writes):
        toks = []
        for key in reads:
            r = self.res.get(key)
            if r is not None and r[0] is not None:
                toks.append(r[0])
        for key in writes:
            r = self.res.get(key)
            if r is not None:
                if r[0] is not None and (r[0][2] != eng or eng == "dma"):
                    toks.append(r[0])
                for t in r[1]:
                    if t[2] != eng or eng == "dma":
                        toks.append(t)
        for t in toks:
            if t[2] == "pe" and eng == "pe":
                continue
            self._wait(eng, t)

    def _update(self, tok, reads, writes):
        for key in reads:
            r = self.res.setdefault(key, [None, []])
            r[1].append(tok)
        for key in writes:
            self.res[key] = [tok, []]

    def op(self, eng, fn, reads=(), writes=()):
        if self.n_instr >= self.max_instr:
            self.stopped = True
        if self.stopped:
            return
        self._deps(eng, reads, writes)
        ins = fn()
        if self.cnt[eng] >= self.ENG_SEM_MAX:
            self._new_eng_sem(eng)
        self.cnt[eng] += 1
        ins.then_inc(self.sem[eng], 1)
        self._update((self.sem[eng], self.cnt[eng], eng), reads, writes)
        self.n_instr += 1

    def dma(self, eng, fn, reads=(), writes=()):
        if self.n_instr >= self.max_instr:
            self.stopped = True
        if self.stopped:
            return
        j = self.dma_rr
        self.dma_rr = (self.dma_rr + 1) % len(self.dma_sems)
        sem = self.dma_sems[j]
        if self.dma_cnt[j] > 0:
            self._wait(eng, (sem, 16 * self.dma_cnt[j], "dma"))
        self._deps(eng, reads, writes)
        ins = fn()
        self.dma_cnt[j] += 1
        ins.then_inc(sem, 16)
        self._update((sem, 16 * self.dma_cnt[j], "dma"), reads, writes)
        self.n_instr += 1

    def barrier(self):
        if self.stopped:
            return
        for eng in self.engs:
            for e2 in self.engs:
                if e2 != eng and self.cnt[e2] > 0:
                    self._wait(eng, (self.sem[e2], self.cnt[e2], e2))
                if e2 != eng and e2 in self.old:
                    self._wait(eng, self.old[e2])
            for j, sem in enumerate(self.dma_sems):
                if self.dma_cnt[j] > 0:
                    self._wait(eng, (sem, 16 * self.dma_cnt[j], "dma"))

    def cc(self, fn, reads=(), writes=()):
        if self.stopped:
            return
        if not hasattr(self, "cc_sem"):
            self.cc_sem = self._mk_sem("cc")
            self.cc_cnt = 0
        self._deps("pool", reads, writes)
        ins = fn()
        self.cc_cnt += 1
        ins.then_inc(self.cc_sem)
        self._update((self.cc_sem, self.cc_cnt, "dma"), reads, writes)
        self.n_instr += 1

    def finish(self, eng="sp"):
        if hasattr(self, "cc_sem") and self.cc_cnt > 0:
            self._wait(eng, (self.cc_sem, self.cc_cnt, "dma"))
        for j, sem in enumerate(self.dma_sems):
            if self.dma_cnt[j] > 0:
                self._wait(eng, (sem, 16 * self.dma_cnt[j], "dma"))


class Ring:
    def __init__(self, tiles, name):
        self.tiles = tiles
        self.name = name
        self.i = 0

    def next(self):
        j = self.i % len(self.tiles)
        self.i += 1
        return self.tiles[j], "%s%d" % (self.name, j)


class _Stop(Exception):
    pass


def build_program(SEQ, layers, dbg=False, phase_limit=99, NCORES=8):
    NLOC = SEQ // 2
    NB = NLOC // 512
    NT = NLOC // 128
    NTA = 2 * NT
    SA = 2 * NLOC
    nc = bass.Bass("TRN2", target_bir_lowering=False)

    def din(name, shape, dt=F32):
        return nc.dram_tensor(name, list(shape), dt, kind="ExternalInput").ap()

    def dscr(name, shape, dt):
        return nc.dram_tensor(name, list(shape), dt, kind="ExternalOutput" if dbg else "Internal").ap()

    x_loc_in = din("x_loc", [NLOC, D])
    x_oth_in = din("x_oth", [NLOC, D])
    c_col = din("c_col", [128, 8])
    ident_in = din("ident", [128, 128])
    jmat_in = din("jmat", [128, 128])
    bones_in = din("bones", [128, 128])
    ones_in = din("ones", [128, 128])
    tril_in = din("tril", [128, 256])
    triu_in = din("triu", [128, 256])
    oh_in = din("oh", [32, 3 * TVW])
    ohc_in = din("ohc", [32, 3 * 128])
    flags_in = din("flags", [128, 2])
    hmask_in = din("hmask", [128, 2])
    ecap_in = din("ecap", [128, NE])
    trash_in = din("trash", [128, 1])
    rel_bias_in = din("rel_bias", [32, 4])
    W = {}
    for l in layers:
        W[l] = dict(
            w_ada=din("w_ada%d" % l, [D, 6 * D]), b_ada=din("b_ada%d" % l, [1, 6 * D]),
            n1g=din("n1g%d" % l, [128, 8]), n2g=din("n2g%d" % l, [128, 8]),
            w_in=din("w_in%d" % l, [D, D_IN]),
            w_up=din("w_up%d" % l, [2, 16, 256]), b_up=din("b_up%d" % l, [128, 4]),
            glag=din("glag%d" % l, [128, 1]), qng=din("qng%d" % l, [128, 1]),
            kng=din("kng%d" % l, [128, 1]), lamv=din("lamv%d" % l, [1, 256]),
            subg=din("subg%d" % l, [128, 1]),
            w_a=din("w_a%d" % l, [512, D]), w_b=din("w_b%d" % l, [512, D]),
            w_o=din("w_o%d" % l, [D, D]),
            w_r=din("w_r%d" % l, [D, 36]), b_r=din("b_r%d" % l, [1, 36]),
            n2grow=din("n2grow%d" % l, [1, D]),
            w1=din("w1_%d" % l, [NE, D, 2 * DEXP]), w2=din("w2_%d" % l, [NE, DEXP, D]),
        )
    x_out = nc.dram_tensor("x_out", [NLOC, D], F32, kind="ExternalOutput").ap()

    tv_d = dscr("tv_d", [12, TVW], F32)
    gq_d = dscr("gq_d", [2, 128, NLOC], BF16)
    gk_d = dscr("gk_d", [2, 128, SA], BF16)
    lg_d = dscr("lg_d", [2, 2, 128, SA], F32)
    gv_d = dscr("gv_d", [SA, 512], BF16)
    gg_d = dscr("gg_d", [4, 128, NLOC], BF16)
    dq_d = dscr("dq_d", [4, 128, NLOC], BF16)
    dk_d = dscr("dk_d", [4, 128, SA], BF16)
    dv_d = dscr("dv_d", [SA, 512], BF16)
    ga_d = dscr("ga_d", [8, 128, NLOC], BF16)
    gb_d = dscr("gb_d", [8, 128, NLOC], BF16)
    oa_d = dscr("oa_d", [4, 128, NLOC], BF16)
    ob_d = dscr("ob_d", [4, 128, NLOC], BF16)
    x1_d = dscr("x1_d", [NLOC, D], F32)
    h2_d = dscr("h2_d", [8, 128, NLOC], BF16)
    Xg = dscr("Xg", [NE * CAP + 128, D], BF16)
    Yg = dscr("Yg", [NE * CAP + 1, D], F32)
    rows_d = dscr("rows_d", [2, D], F32)
    xmid_c = [nc.dram_tensor("xmid_c%d" % j, [512, D], F32).ap() for j in range(NB)]
    xg_c = [nc.dram_tensor("xg_c%d" % j, [1024, D], F32).ap() for j in range(NB)]
    dbg_d = dscr("dbg_d", [128, 256], F32)
    dbg2_d = dscr("dbg2_d", [128, 8, 512], BF16)
    dbg3_d = dscr("dbg3_d", [128, 12], F32)
    dbg4_d = dscr("dbg4_d", [128, 4, D], F32)
    dbg_out = {}

    with ExitStack() as top:
        S = Sched(nc, top)

        _uid = [0]

        def sbuf(st, name, shape, dt):
            _uid[0] += 1
            return st.enter_context(nc.sbuf_tensor("s%d_%s" % (_uid[0], name), list(shape), dt))

        pbanks = [top.enter_context(nc.psum_tensor("pb%d" % i, [128, 512], F32)) for i in range(8)]
        PS = Ring(pbanks, "pb")

        def mm(out, lhsT, rhs, start, stop, reads, writes):
            S.op("pe", lambda: nc.tensor.matmul(out, lhsT, rhs, start=start, stop=stop, skip_group_check=True), reads, writes)

        def tr(out, in_, ident, reads, writes):
            S.op("pe", lambda: nc.tensor.transpose(out, in_, ident), reads, writes)

        def act(out, in_, func, reads, writes, bias=None, scale=None, accum=None):
            kw = {}
            if bias is not None:
                kw["bias"] = bias
            if scale is not None:
                kw["scale"] = scale
            if accum is not None:
                kw["accum_out"] = accum
            S.op("act", lambda: nc.scalar.activation(out=out, in_=in_, func=func, **kw), reads, writes)

        def ts(eng, out, in0, s1, s2, op0, op1, reads, writes):
            e = nc.vector if eng == "dve" else nc.gpsimd
            if op1 is None:
                S.op(eng, lambda: e.tensor_scalar(out, in0, s1, None, op0=op0), reads, writes)
            else:
                S.op(eng, lambda: e.tensor_scalar(out, in0, s1, s2, op0=op0, op1=op1), reads, writes)

        def tt(eng, out, in0, in1, op, reads, writes):
            e = nc.vector if eng == "dve" else nc.gpsimd
            S.op(eng, lambda: e.tensor_tensor(out, in0, in1, op), reads, writes)

        def stt(out, in0, scalar, in1, op0, op1, reads, writes):
            S.op("dve", lambda: nc.vector.scalar_tensor_tensor(out, in0, scalar, in1, op0=op0, op1=op1), reads, writes)

        def cp(eng, out, in_, reads, writes):
            if eng == "act":
                S.op("act", lambda: nc.scalar.copy(out, in_), reads, writes)
            else:
                e = nc.vector if eng == "dve" else nc.gpsimd
                S.op(eng, lambda: e.tensor_copy(out, in_), reads, writes)

        def ld(out, in_, reads, writes, q="sp"):
            e = {"sp": nc.sync, "pool": nc.gpsimd, "act": nc.scalar}[q]
            S.dma(q, lambda: e.dma_start(out=out, in_=in_), reads, writes)

        ident_f = sbuf(top, "ident_f", [128, 128], F32)
        ident_b = sbuf(top, "ident_b", [128, 128], BF16)
        jmat_f = sbuf(top, "jmat_f", [128, 128], F32)
        bones_b = sbuf(top, "bones_b", [128, 128], BF16)
        ones_f = sbuf(top, "ones_f", [128, 128], F32)
        tril_f = sbuf(top, "tril_f", [128, 256], F32)
        triu_f = sbuf(top, "triu_f", [128, 256], F32)
        flags = sbuf(top, "flags", [128, 2], F32)
        hm = sbuf(top, "hm", [128, 2], F32)
        eps_c = sbuf(top, "eps_c", [128, 1], F32)
        one_c = sbuf(top, "one_c", [128, 1], F32)
        zero_c = sbuf(top, "zero_c", [128, 1], F32)
        cact = sbuf(top, "cact", [128, 8], F32)
        relb = sbuf(top, "relb", [32, 4], F32)
        cb = sbuf(top, "cb", [128, 12], F32)
        gat = sbuf(top, "gat", [128, NT, 2], F32)
        gix = sbuf(top, "gix", [128, NT, 2], mybir.dt.int32)
        ecap = sbuf(top, "ecap", [128, NE], F32)
        trash = sbuf(top, "trash", [128, 1], F32)
        zrow = sbuf(top, "zrow", [1, D], F32)
        ld(ident_f[:], ident_in, [], ["ident_f"])
        ld(ident_b[:], ident_in, [], ["ident_b"], q="pool")
        ld(jmat_f[:], jmat_in, [], ["jmat_f"])
        ld(bones_b[:], bones_in, [], ["bones_b"], q="pool")
        ld(ones_f[:], ones_in, [], ["ones_f"])
        ld(tril_f[:], tril_in, [], ["tril_f"])
        ld(triu_f[:], triu_in, [], ["triu_f"])
        ld(flags[:], flags_in, [], ["flags"])
        ld(hm[:], hmask_in, [], ["hm"])
        ld(ecap[:], ecap_in, [], ["ecap"])
        ld(trash[:], trash_in, [], ["trash"])
        S.op("dve", lambda: nc.vector.memset(zrow[:], 0.0), [], ["zrow"])
        ld(Yg[NE * CAP:NE * CAP + 1, :], zrow[:], ["zrow"], ["Yg"])
        ld(cact[:], c_col, [], ["cact"])
        ld(relb[:], rel_bias_in, [], ["relb"])
        S.op("dve", lambda: nc.vector.memset(eps_c[:], RMS_EPS), [], ["eps_c"])
        S.op("dve", lambda: nc.vector.memset(one_c[:], 1.0), [], ["one_c"])
        S.op("dve", lambda: nc.vector.memset(zero_c[:], 0.0), [], ["zero_c"])
        act(cact[:], cact[:], AF.Silu, ["cact"], ["cact"])

        def build_gtab(st_outer):
            Gtab = sbuf(st_outer, "Gtab", [128, 12, GW], BF16)
            with ExitStack() as st:
                oh = sbuf(st, "oh", [32, 3 * TVW], F32)
                ohc = sbuf(st, "ohc", [32, 3 * 128], F32)
                tvs = sbuf(st, "tvs", [4, 3 * TVW], F32)
                Hk = sbuf(st, "Hk", [128, GW], F32)
                ld(oh[:], oh_in, [], ["oh"])
                ld(ohc[:], ohc_in, [], ["ohc"])
                for tab in range(3):
                    for c0 in range(0, TVW, 512):
                        w = min(512, TVW - c0)
                        p, pk = PS.next()
                        mm(p[0:4, 0:w], relb[:, :], oh[:, tab * TVW + c0: tab * TVW + c0 + w], True, True,
                           ["relb", "oh"], [pk])
                        cp("dve", tvs[:, tab * TVW + c0: tab * TVW + c0 + w], p[0:4, 0:w], [pk], ["tvs"])
                    ld(tv_d[tab * 4:(tab + 1) * 4, :], tvs[:, tab * TVW:(tab + 1) * TVW], ["tvs"], ["tv_d"])
                for k in range(3):
                    p, pk = PS.next()
                    mm(p[:, 0:4], ohc[:, k * 128:(k + 1) * 128], relb[:, :], True, True, ["ohc", "relb"], [pk])
                    cp("dve", cb[:, k * 4:(k + 1) * 4], p[:, 0:4], [pk], ["cb"])
                for th in range(12):
                    hank = bass.AP(tv_d.tensor, th * TVW, [[1, 128], [1, GW]])
                    ld(Hk[:], hank, ["tv_d"], ["Hk"])
                    for c0 in range(0, GW, 512):
                        w = min(512, GW - c0)
                        p, pk = PS.next()
                        mm(p[:, 0:w], jmat_f[:], Hk[:, c0:c0 + w], True, True, ["jmat_f", "Hk"], [pk])
                        cp("dve", Gtab[:, th, c0:c0 + w], p[:, 0:w], [pk], [("Gtab", th)])
            S.barrier()
            return Gtab

        try:
          for li, l in enumerate(layers):
            Wl = W[l]
            lam_init = 0.8 - 0.6 * math.exp(-0.3 * l)
            def x_loc_block(i, li=li):
                return x_loc_in[i * 512:(i + 1) * 512, :] if li == 0 else xmid_c[i]
            x_src_oth = x_oth_in
            fused_oth = li > 0
            is_last = li == len(layers) - 1
            with ExitStack() as lst:
                modcol = sbuf(lst, "modcol", [128, 48], F32)
                a1c = sbuf(lst, "a1c", [128, 8], F32)
                a2c = sbuf(lst, "a2c", [128, 8], F32)
                g1b = sbuf(lst, "g1b", [128, D], F32)
                g2b = sbuf(lst, "g2b", [128, D], F32)
                n1g = sbuf(lst, "n1g", [128, 8], F32)
                n2g = sbuf(lst, "n2g", [128, 8], F32)
                b_up = sbuf(lst, "b_up", [128, 4], F32)
                glag = sbuf(lst, "glag", [128, 1], F32)
                qng = sbuf(lst, "qng", [128, 1], F32)
                kng = sbuf(lst, "kng", [128, 1], F32)
                subg = sbuf(lst, "subg", [128, 1], F32)
                lamv = sbuf(lst, "lamv", [1, 256], F32)
                lamw = sbuf(lst, "lamw", [1, 8], F32)
                nlam2 = sbuf(lst, "nlam", [128, 2], F32)
                nlam = nlam2[:, 0:1]
                brb = sbuf(lst, "brb", [128, 36], F32)
                brow = sbuf(lst, "brow", [1, 36], F32)
                for nm, t_, src in (("n1g", n1g, Wl["n1g"]), ("n2g", n2g, Wl["n2g"]), ("b_up", b_up, Wl["b_up"]),
                                    ("glag", glag, Wl["glag"]), ("qng", qng, Wl["qng"]), ("kng", kng, Wl["kng"]),
                                    ("subg", subg, Wl["subg"]), ("lamv", lamv, Wl["lamv"]), ("brow", brow, Wl["b_r"])):
                    ld(t_[:], src, [], [nm])
                with ExitStack() as st:
                    modrow = sbuf(st, "modrow", [1, 6 * D], F32)
                    wada = [sbuf(st, "wada%d" % i, [128, 3 * D], F32) for i in range(2)]
                    bada = sbuf(st, "bada", [1, 6 * D], F32)
                    ld(bada[:], Wl["b_ada"], [], ["bada"])
                    macc = [PS.next() for _ in range(6)]
                    nld = 0
                    for half in range(2):
                        for k in range(8):
                            wt = wada[nld % 2]
                            wk = "wada%d" % (nld % 2)
                            nld += 1
                            ld(wt[:], Wl["w_ada"][k * 128:(k + 1) * 128, half * 3 * D:(half + 1) * 3 * D], [], [wk])
                            for j in range(6):
                                p, pk = macc[j]
                                mm(p[0:1, :], cact[:, k:k + 1], wt[:, j * 512:(j + 1) * 512], k == 0, k == 7,
                                   ["cact", wk], [pk])
                        for j in range(6):
                            p, pk = macc[j]
                            c0 = (half * 6 + j) * 512
                            tt("dve", modrow[0:1, c0:c0 + 512], p[0:1, :], bada[0:1, c0:c0 + 512], ALU.add,
                               [pk, "bada"], ["modrow"])
                    n2r = sbuf(st, "n2r", [1, D], F32)
                    arow = sbuf(st, "arow", [1, D], F32)
                    ld(n2r[:], Wl["n2grow"], [], ["n2r"])
                    stt(arow[:], modrow[0:1, 32 * 128:40 * 128], 1.0, n2r[:], ALU.add, ALU.mult, ["modrow", "n2r"], ["arow"])
                    ld(rows_d[0:1, :], arow[:], ["arow"], ["rows_d"])
                    ld(rows_d[1:2, :], modrow[0:1, 24 * 128:32 * 128], ["modrow"], ["rows_d"])
                    p, pk = PS.next()
                    for j in range(48):
                        mm(p[:, j:j + 1], modrow[0:1, j * 128:(j + 1) * 128], ones_f[0:1, 0:1], True, True,
                           ["modrow", "ones_f"], [pk])
                    cp("dve", modcol[:], p[:, 0:48], [pk], ["modcol"])
                    for gi, (gt_, gk_) in enumerate(((g1b, "g1b"), (g2b, "g2b"))):
                        base = (16 if gi == 0 else 40) * 128
                        for n in range(2):
                            p, pk = PS.next()
                            mm(p[:], ones_f[0:1, :], modrow[0:1, base + n * 512: base + (n + 1) * 512], True, True,
                               ["ones_f", "modrow"], [pk])
                            cp("dve", gt_[:, n * 512:(n + 1) * 512], p[:], [pk], [gk_])
                S.barrier()
                stt(a1c[:], modcol[:, 8:16], 1.0, n1g[:], ALU.add, ALU.mult, ["modcol", "n1g"], ["a1c"])
                stt(a2c[:], modcol[:, 32:40], 1.0, n2g[:], ALU.add, ALU.mult, ["modcol", "n2g"], ["a2c"])
                p, pk = PS.next()
                mm(p[:, 0:36], ones_f[0:1, :], brow[0:1, :], True, True, ["ones_f", "brow"], [pk])
                cp("dve", brb[:], p[:, 0:36], [pk], ["brb"])
                tt("dve", lamv[0:1, 0:64], lamv[0:1, 0:64], lamv[0:1, 64:128], ALU.mult, ["lamv"], ["lamv"])
                tt("dve", lamv[0:1, 128:192], lamv[0:1, 128:192], lamv[0:1, 192:256], ALU.mult, ["lamv"], ["lamv"])
                S.op("dve", lambda: nc.vector.reduce_sum(lamw[0:1, 0:1], lamv[0:1, 0:64], axis=AX.X), ["lamv"], ["lamw"])
                S.op("dve", lambda: nc.vector.reduce_sum(lamw[0:1, 1:2], lamv[0:1, 128:192], axis=AX.X), ["lamv"], ["lamw"])
                act(lamw[0:1, 2:4], lamw[0:1, 0:2], AF.Exp, ["lamw"], ["lamw"])
                stt(lamw[0:1, 4:5], lamw[0:1, 3:4], -lam_init, lamw[0:1, 2:3], ALU.add, ALU.subtract, ["lamw"], ["lamw"])
                p, pk = PS.next()
                mm(p[:, 0:1], ones_f[0:1, :], lamw[0:1, 4:5], True, True, ["ones_f", "lamw"], [pk])
                S.op("dve", lambda: nc.vector.memset(nlam2[:], 0.0), [], ["nlam"])
                cp("dve", nlam, p[:, 0:1], [pk], ["nlam"])
                qngs = sbuf(lst, "qngs", [128, 1], F32)
                subgs = sbuf(lst, "subgs", [128, 1], F32)
                ts("dve", qngs[:], qng[:], 0.125, None, ALU.mult, None, ["qng"], ["qngs"])
                ts("dve", subgs[:], subg[:], 1.0 - lam_init, None, ALU.mult, None, ["subg"], ["subgs"])

                def norm_block(xt, xk, ac, bc0, hT, hk, scr, hT32=None):
                    norm_pre(xt, xk, scr)
                    norm_post(xt, xk, ac, bc0, hT, hk, scr, hT32)

                def norm_pre(xt, xk, scr):
                    ss = scr["ss"]
                    for t in range(4):
                        act(scr["junk"][:], xt[:, t, :], AF.Square, [xk], ["junk"], accum=ss[:, t:t + 1])
                    act(ss[:, 4:8], ss[:, 0:4], AF.Ln, ["junk"], ["ssb"], bias=eps_c[:], scale=1.0 / D)
                    act(ss[:, 8:12], ss[:, 4:8], AF.Exp, ["ssb"], ["ssc"], scale=-0.5)
                    for t in range(4):
                        ts("pool" if t % 2 else "dve", xt[:, t, :], xt[:, t, :], ss[:, 8 + t:9 + t], None,
                           ALU.mult, None, [xk, "ssc"], [xk])

                def norm_post(xt, xk, ac, bc0, hT, hk, scr, hT32=None):
                    for k in range(8):
                        p, pk = PS.next()
                        for t in range(4):
                            tr(p[:, t * 128:(t + 1) * 128], xt[:, t, k * 128:(k + 1) * 128], ident_f[:],
                               [xk, "ident_f"], [pk])
                        dst = hT if hT32 is None else hT32
                        dkey = hk if hT32 is None else hk + "32"
                        if k % 2 == 0:
                            act(dst[:, k, :], p[:], AF.Identity, [pk, ac, "modcol"], [(dkey, k)],
                                bias=modcol[:, bc0 + k:bc0 + k + 1], scale=scr["ac"][:, k:k + 1])
                        else:
                            ts("dve", dst[:, k, :], p[:], scr["ac"][:, k:k + 1], modcol[:, bc0 + k:bc0 + k + 1],
                               ALU.mult, ALU.add, [pk, ac, "modcol"], [(dkey, k)])
                        if hT32 is not None and hT is not None:
                            cp("pool", hT[:, k, :], hT32[:, k, :], [(dkey, k)], [(hk, k)])

                print('n_instr before N1', S.n_instr)
                if dbg:
                    ld(dbg_d[:, 0:48], modcol[:], ["modcol"], ["dbg_d"])
                    ld(dbg_d[:, 48:56], a1c[:], ["a1c"], ["dbg_d"])
                    ld(dbg_d[:, 56:64], a2c[:], ["a2c"], ["dbg_d"])
                    ld(dbg_d[:, 64:66], nlam2[:, 0:2], ["nlam"], ["dbg_d"])
                    ld(dbg_d[:, 65:73], cact[:], ["cact"], ["dbg_d"])
                    ld(dbg_d[:, 80:144], g1b[:, 0:64], ["g1b"], ["dbg_d"])
                    ld(dbg_d[:, 144:208], g2b[:, 960:1024], ["g2b"], ["dbg_d"])
                    ld(dbg_d[:, 208:244], brb[:], ["brb"], ["dbg_d"])
                S.barrier()
                if phase_limit < 2:
                    S.stopped = True
                with ExitStack() as st:
                    w_in = sbuf(st, "w_in", [128, 8, D_IN], BF16)
                    for k in range(8):
                        ld(w_in[:, k, :], Wl["w_in"][k * 128:(k + 1) * 128, :], [], [("w_in", k)], q="pool")
                    wup = sbuf(st, "wup", [16, 2, 256], F32)
                    ld(wup[:], Wl["w_up"].rearrange("d r c -> r d c"), [], ["wup"])
                    nbup = sbuf(st, "nbup", [128, 4], F32)
                    ts("dve", nbup[:], b_up[:], -1.0, None, ALU.mult, None, ["b_up"], ["nbup"])
                    xts = [sbuf(st, "xt%d" % i, [128, 4, D], F32) for i in range(2)]
                    hTs = [sbuf(st, "hT%d" % i, [128, 8, 512], BF16) for i in range(2)]
                    scr = dict(ss=sbuf(st, "n_ss", [128, 12], F32), junk=sbuf(st, "n_junk", [128, D], BF16),
                               ac=a1c)
                    stg = Ring([sbuf(st, "stg%d" % i, [128, 512], BF16) for i in range(6)], "stg")
                    stf = Ring([sbuf(st, "stf%d" % i, [128, 512], F32) for i in range(4)], "stf")
                    lrT = [sbuf(st, "lrT%d" % i, [16, 512], F32) for i in range(2)]
                    blocks = [(0, i) for i in range(NB)] + [(1, i) for i in range(NB)]

                    xsel = [sbuf(st, "xsel%d" % i_, [128, D], F32) for i_ in range(2)] if fused_oth else None

                    def load_x(bi):
                        oth, i = blocks[bi]
                        if oth and fused_oth:
                            xt_ = xts[bi % 2]
                            xk_ = "xt%d" % (bi % 2)
                            ld(xt_[:], xg_c[i][0:512, :].rearrange("(t p) d -> p t d", p=128), ["xg"], [xk_])
                            for t in range(4):
                                r0 = 512 + t * 128
                                ld(xsel[t % 2][:], xg_c[i][r0:r0 + 128, :], ["xg"], ["xsel%d" % (t % 2)])
                                ts("pool", xt_[:, t, :], xt_[:, t, :], flags[:, 0:1], None, ALU.mult, None, [xk_, "flags"], [xk_])
                                stt(xt_[:, t, :], xsel[t % 2][:], flags[:, 1:2], xt_[:, t, :], ALU.mult, ALU.add,
                                    ["xsel%d" % (t % 2), "flags", xk_], [xk_])
                            return
                        src = x_src_oth[i * 512:(i + 1) * 512, :] if oth else x_loc_block(i)
                        ld(xts[bi % 2][:], src.rearrange("(t p) d -> p t d", p=128),
                           [("xmid", t_) for t_ in range(NT)] if (li > 0 and not oth) else [], ["xt%d" % (bi % 2)])

                    load_x(0)
                    norm_pre(xts[0], "xt0", scr)
                    norm_post(xts[0], "xt0", "a1c", 0, hTs[0], "hT0", scr)
                    for bi, (oth, i) in enumerate(blocks):
                        if bi + 1 < len(blocks):
                            load_x(bi + 1)
                        xt, xk = xts[bi % 2], "xt%d" % (bi % 2)
                        hT, hk = hTs[bi % 2], "hT%d" % (bi % 2)
                        nxt, nxk = xts[(bi + 1) % 2], "xt%d" % ((bi + 1) % 2)
                        nhT, nhk = hTs[(bi + 1) % 2], "hT%d" % ((bi + 1) % 2)
                        if dbg and bi == 0:
                            ld(dbg2_d, hT[:], [(hk, k) for k in range(8)], ["dbg2"])
                            ld(dbg3_d, scr["ss"][:], ["ssc"], ["dbg3"])
                            ld(dbg4_d, xt[:], [xk], ["dbg4"])
                        tok0 = (NLOC if oth else 0) + i * 512
                        hreads = [(hk, k) for k in range(8)]

                        def fm_tile(c0, m):
                            p, pk = PS.next()
                            for k in range(8):
                                mm(p[0:m, :], w_in[:, k, c0:c0 + m], hT[:, k, :], k == 0, k == 7,
                                   [("w_in", k), (hk, k)], [pk])
                            return p, pk

                        for pr in range(2):
                            p, pk = fm_tile(C_GK + pr * 128, 128)
                            s, sk = stg.next()
                            cp("act", s[:], p[:], [pk], [sk])
                            ld(gk_d[pr, :, tok0:tok0 + 512], s[:], [sk], ["gk_d"])
                            if not oth:
                                p, pk = fm_tile(C_GQ + pr * 128, 128)
                                s, sk = stg.next()
                                ts("dve", s[:], p[:], 0.125, None, ALU.mult, None, [pk], [sk])
                                ld(gq_d[pr, :, tok0:tok0 + 512], s[:], [sk], ["gq_d"])
                        for dr in range(2):
                            p, pk = fm_tile(C_LRF + dr * 16, 16)
                            cp("dve", lrT[dr][:], p[0:16, :], [pk], ["lrT%d" % dr])
                            for pr in range(2):
                                p, pk = PS.next()
                                mm(p[:], wup[:, dr, pr * 128:(pr + 1) * 128], lrT[dr][:], True, True,
                                   ["wup", "lrT%d" % dr], [pk])
                                s, sk = stf.next()
                                act(s[:], p[:], AF.Exp, [pk, "nbup"], [sk], scale=-1.0,
                                    bias=nbup[:, dr * 2 + pr: dr * 2 + pr + 1])
                                act(s[:], s[:], AF.Ln, [sk, "one_c"], [sk], bias=one_c[:])
                                ts("dve", s[:], s[:], -1.0 / 16.0, None, ALU.mult, None, [sk], [sk])
                                ld(lg_d[dr, pr, :, tok0:tok0 + 512], s[:], [sk], ["lg_d"])
                        for (c0, dst, dk_) in ((C_GV, gv_d, "gv_d"), (C_DV, dv_d, "dv_d")):
                            for t in range(4):
                                p, pk = PS.next()
                                for k in range(8):
                                    mm(p[:], hT[:, k, t * 128:(t + 1) * 128], w_in[:, k, c0:c0 + 512], k == 0, k == 7,
                                       [("w_in", k), (hk, k)], [pk])
                                s, sk = stg.next()
                                cp("act" if t % 2 else "dve", s[:], p[:], [pk], [sk])
                                ld(dst[tok0 + t * 128: tok0 + (t + 1) * 128, :], s[:], [sk], [dk_])
                        if bi + 1 < len(blocks):
                            norm_pre(nxt, nxk, scr)
                        qk_list = [("k", C_DK, dk_d, "dk_d", kng, "kng")]
                        if not oth:
                            qk_list.append(("q", C_DQ, dq_d, "dq_d", qngs, "qngs"))
                        for (nm, c0, dst, dkey, gcol, gkey) in qk_list:
                            for h in range(4):
                                p, pk = fm_tile(c0 + h * 128, 128)
                                s, sk = stg.next()
                                act(s[:], p[:], AF.Square, [pk], [sk])
                                p2, pk2 = PS.next()
                                mm(p2[:], bones_b[:], s[:], True, True, ["bones_b", sk], [pk2])
                                f, fk = stf.next()
                                act(f[:], p2[:], AF.Ln, [pk2, "eps_c"], [fk], bias=eps_c[:], scale=1.0 / 64.0)
                                act(f[:], f[:], AF.Exp, [fk], [fk], scale=-0.5)
                                s2, sk2 = stg.next()
                                stt(s2[:], p[:], gcol[:, 0:1], f[:], ALU.mult, ALU.mult, [pk, gkey, fk], [sk2])
                                ld(dst[h, :, tok0:tok0 + 512], s2[:], [sk2], [dkey])
                        if not oth:
                            for j in range(4):
                                p, pk = fm_tile(C_GG + j * 128, 128)
                                s, sk = stg.next()
                                act(s[:], p[:], AF.Silu, [pk], [sk])
                                ld(gg_d[j, :, tok0:tok0 + 512], s[:], [sk], ["gg_d"])
                            for (c0, dst, dkey) in ((C_GA, ga_d, "ga_d"), (C_GB, gb_d, "gb_d")):
                                for j in range(8):
                                    p, pk = fm_tile(c0 + j * 128, 128)
                                    s, sk = stg.next()
                                    act(s[:], p[:], AF.Sigmoid, [pk], [sk])
                                    ld(dst[j, :, tok0:tok0 + 512], s[:], [sk], [dkey])
                        if bi + 1 < len(blocks):
                            norm_post(nxt, nxk, "a1c", 0, nhT, nhk, scr)

                print('n_instr before GLA', S.n_instr)
                S.barrier()
                if phase_limit < 3:
                    S.stopped = True
                with ExitStack() as st:
                    kT = sbuf(st, "g_kT", [128, NLOC], BF16)
                    qT = sbuf(st, "g_qT", [128, NLOC], BF16)
                    lg = sbuf(st, "g_lg", [128, NLOC], F32)
                    cum = sbuf(st, "g_cum", [128, NLOC], F32)
                    kinv = sbuf(st, "g_kinv", [128, NLOC], BF16)
                    qdec = sbuf(st, "g_qdec", [128, NLOC], BF16)
                    qdM = [sbuf(st, "g_qdM%d" % i, [128, NLOC], BF16) for i in range(2)]
                    vv = sbuf(st, "g_v", [128, NT, 256], BF16)
                    rmask = sbuf(st, "g_rmask", [128, NLOC], F32)
                    ofw = sbuf(st, "g_of", [128, NT, 256], BF16)
                    ggT = sbuf(st, "g_gg", [128, 2, NLOC], BF16)
                    Sst = sbuf(st, "g_S", [128, 256], F32)
                    Sbf = sbuf(st, "g_Sbf", [128, 256], BF16)
                    tmpSr = Ring([sbuf(st, "g_tmpS%d" % i, [128, 256], F32) for i in range(4)], "g_tmpS")
                    kTt = Ring([sbuf(st, "g_kTt%d" % i, [128, 128], BF16) for i in range(4)], "g_kTt")
                    ATs = Ring([sbuf(st, "g_AT%d" % i, [128, 256], BF16) for i in range(4)], "g_AT")
                    osum = Ring([sbuf(st, "g_os%d" % i, [128, 256], F32) for i in range(4)], "g_os")
                    onr = Ring([sbuf(st, "g_on%d" % i, [128, 256], F32) for i in range(4)], "g_on")
                    gssr = Ring([sbuf(st, "g_ss%d" % i, [128, 8], F32) for i in range(4)], "g_ss")
                    gjunk = sbuf(st, "g_junk", [128, 128], BF16)
                    oaT = [sbuf(st, "g_oaT%d" % i, [128, 2, 512], BF16) for i in range(2)]
                    S.op("dve", lambda: nc.vector.memset(rmask[:], 1.0), [], ["rmask"])
                    rm3 = rmask[:].rearrange("p (c t) -> p c t", t=128)
                    S.op("dve", lambda: nc.vector.memset(rm3[:, :, 0:1], 0.0), ["rmask"], ["rmask"])
                    for pr in range(2):
                        ld(qT[:], gq_d[pr], ["gq_d"], ["g_qT"])
                        ld(ggT[:], gg_d[pr * 2:(pr + 1) * 2].rearrange("j p n -> p j n"), ["gg_d"], ["g_gg"])
                        for dr in range(2):
                            mask = tril_f if dr == 0 else triu_f
                            mkey = "tril_f" if dr == 0 else "triu_f"
                            dcol = 127 if dr == 0 else 0
                            S.op("dve", lambda: nc.vector.memset(Sst[:], 0.0), [], ["g_S"])
                            for is_oth in (True, False):
                                t0 = NLOC if is_oth else 0
                                ld(kT[:], gk_d[pr, :, t0:t0 + NLOC], ["gk_d"], ["g_kT"])
                                ld(vv[:], gv_d[t0:t0 + NLOC, pr * 256:(pr + 1) * 256].rearrange("(t p) c -> p t c", p=128),
                                   ["gv_d"], ["g_v"])
                                ld(lg[:], lg_d[dr, pr, :, t0:t0 + NLOC], ["lg_d"], ["g_lg"])
                                if dr == 0:
                                    S.op("dve", lambda: nc.vector.tensor_tensor_scan(
                                        out=cum[:], data0=rmask[:], data1=lg[:], initial=0.0, op0=ALU.mult, op1=ALU.add),
                                        ["rmask", "g_lg"], ["g_cum"])
                                else:
                                    S.op("dve", lambda: nc.vector.tensor_tensor_scan(
                                        out=cum[:, ::-1], data0=rmask[:], data1=lg[:, ::-1], initial=0.0,
                                        op0=ALU.mult, op1=ALU.add),
                                        ["rmask", "g_lg"], ["g_cum"])
                                act(lg[:], cum[:], AF.Exp, ["g_cum"], ["g_lg"], scale=-1.0)
                                tt("pool", kinv[:], kT[:], lg[:], ALU.mult, ["g_kT", "g_lg"], ["g_kinv"])
                                act(cum[:], cum[:], AF.Exp, ["g_cum"], ["g_cum"])
                                E = cum
                                if not is_oth:
                                    tt("dve", qdec[:], qT[:], E[:], ALU.mult, ["g_qT", "g_cum"], ["g_qdec"])
                                    for hh in range(2):
                                        ts("pool" if hh else "dve", qdM[hh][:], qdec[:], hm[:, hh:hh + 1], None, ALU.mult, None,
                                           ["g_qdec", "hm"], [("g_qdM", hh)])
                                    ts("dve", Sst[:], Sst[:], flags[:, dr:dr + 1], None, ALU.mult, None,
                                       ["g_S", "flags"], ["g_S"])
                                chunks = list(range(NT))
                                if dr == 1:
                                    chunks.reverse()
                                def g_p1(ci, c):
                                    cs = slice(c * 128, (c + 1) * 128)
                                    d = {}
                                    if not is_oth:
                                        pA, pAk = PS.next()
                                        for hh in range(2):
                                            mm(pA[:, hh * 128:(hh + 1) * 128], kinv[:, cs], qdM[hh][:, cs], True, True,
                                               ["g_kinv", ("g_qdM", hh)], [pAk])
                                        AT, ATk = ATs.next()
                                        tt("dve", AT[:], pA[:, 0:256], mask[:], ALU.mult, [pAk, mkey], [ATk])
                                        d["AT"] = (AT, ATk)
                                    last = (not is_oth) and ci == NT - 1
                                    if not last:
                                        pT, pTk = PS.next()
                                        pTb = pT[:].bitcast(BF16)
                                        tr(pTb[:, 0:128], kinv[:, cs], ident_b[:], ["g_kinv", "ident_b"], [pTk])
                                        kt_, ktk = kTt.next()
                                        cp("act", kt_[:], pTb[:, 0:128], [pTk], [ktk])
                                        d["kT"] = (kt_, ktk)
                                    return d

                                def g_p2(ci, c, d):
                                    if "kT" in d:
                                        kt_, ktk = d["kT"]
                                        pK, pKk = PS.next()
                                        mm(pK[:, 0:256], kt_[:], vv[:, c, :], True, True, [ktk, "g_v"], [pKk])
                                        dc = E[:, c * 128 + dcol: c * 128 + dcol + 1]
                                        tmpS, tmpSk = tmpSr.next()
                                        act(tmpS[:], pK[:, 0:256], AF.Identity, [pKk, "g_cum"], [tmpSk], scale=dc)
                                        d["tmpS"] = (tmpS, tmpSk, dc)

                                def g_state(ci, c, d):
                                    cs = slice(c * 128, (c + 1) * 128)
                                    if not is_oth:
                                        AT, ATk = d["AT"]
                                        cp("pool", Sbf[:], Sst[:], ["g_S"], ["g_Sbf"])
                                        pO, pOk = PS.next()
                                        for hh in range(2):
                                            hs = slice(hh * 128, (hh + 1) * 128)
                                            mm(pO[:, hs], AT[:, hs], vv[:, c, hs], True, False, [ATk, "g_v"], [pOk])
                                            mm(pO[:, hs], qdM[hh][:, cs], Sbf[:, hs], False, True, [("g_qdM", hh), "g_Sbf"], [pOk])
                                        d["pO"] = (pO, pOk)
                                    if "tmpS" in d:
                                        tmpS, tmpSk, dc = d["tmpS"]
                                        stt(Sst[:], Sst[:], dc, tmpS[:], ALU.mult, ALU.add, ["g_S", "g_cum", tmpSk], ["g_S"])

                                def g_x1(ci, c, d):
                                    if is_oth:
                                        return
                                    pO, pOk = d["pO"]
                                    if dr == 0:
                                        cp("act", ofw[:, c, :], pO[:, 0:256], [pOk], [("g_of", c)])
                                        return
                                    os_, osk = osum.next()
                                    gss, gsk = gssr.next()
                                    d["os"] = (os_, osk)
                                    d["gss"] = (gss, gsk)
                                    tt("dve", os_[:], pO[:, 0:256], ofw[:, c, :], ALU.add, [pOk, ("g_of", c)], [osk])
                                    for hh in range(2):
                                        act(gjunk[:], os_[:, hh * 128:(hh + 1) * 128], AF.Square, [osk], ["g_junk", (gsk, "a")],
                                            accum=gss[:, hh:hh + 1])
                                    act(gss[:, 2:4], gss[:, 0:2], AF.Ln, [(gsk, "a"), "eps_c"], [(gsk, "b")],
                                        bias=eps_c[:], scale=1.0 / 128.0)

                                def g_x2(ci, c, d):
                                    if is_oth or dr == 0:
                                        return
                                    os_, osk = d["os"]
                                    gss, gsk = d["gss"]
                                    act(gss[:, 4:6], gss[:, 2:4], AF.Exp, [(gsk, "b")], [(gsk, "c")], scale=-0.5)
                                    on_, onk = onr.next()
                                    d["on"] = (on_, onk)
                                    for hh in range(2):
                                        ts("pool", on_[:, hh * 128:(hh + 1) * 128], os_[:, hh * 128:(hh + 1) * 128],
                                           gss[:, 4 + hh:5 + hh], None, ALU.mult, None, [osk, (gsk, "c")], [(onk, hh)])

                                def g_x3(ci, c, d):
                                    if is_oth or dr == 0:
                                        return
                                    on_, onk = d["on"]
                                    blk, cc = c // 4, c % 4
                                    ob_, obk = oaT[blk % 2], "g_oaT%d" % (blk % 2)
                                    pX, pXk = PS.next()
                                    for hh in range(2):
                                        tr(pX[:, hh * 128:(hh + 1) * 128], on_[:, hh * 128:(hh + 1) * 128], ident_f[:],
                                           [(onk, hh), "ident_f"], [pXk])
                                    for hh in range(2):
                                        stt(ob_[:, hh, cc * 128:(cc + 1) * 128], pX[:, hh * 128:(hh + 1) * 128], glag[:, 0:1],
                                            ggT[:, hh, c * 128:(c + 1) * 128], ALU.mult, ALU.mult,
                                            [pXk, "glag", "g_gg"], [(obk, cc)])
                                    if cc == 0:
                                        ld(oa_d[pr * 2:(pr + 1) * 2, :, blk * 512:(blk + 1) * 512].rearrange("j p n -> p j n"),
                                           ob_[:], [(obk, q_) for q_ in range(4)], ["oa_d"])

                                ds = {}
                                ds[0] = g_p1(0, chunks[0])
                                if NT > 1:
                                    ds[1] = g_p1(1, chunks[1])
                                g_p2(0, chunks[0], ds[0])
                                for ci, c in enumerate(chunks):
                                    if ci + 2 < NT:
                                        ds[ci + 2] = g_p1(ci + 2, chunks[ci + 2])
                                    if ci + 1 < NT:
                                        g_p2(ci + 1, chunks[ci + 1], ds[ci + 1])
                                    g_state(ci, c, ds[ci])
                                    g_x1(ci, c, ds[ci])
                                    if ci >= 1:
                                        g_x2(ci - 1, chunks[ci - 1], ds[ci - 1])
                                    if ci >= 2:
                                        g_x3(ci - 2, chunks[ci - 2], ds[ci - 2])
                                        del ds[ci - 2]
                                g_x2(NT - 1, chunks[NT - 1], ds[NT - 1])
                                if NT >= 2:
                                    g_x3(NT - 2, chunks[NT - 2], ds[NT - 2])
                                g_x3(NT - 1, chunks[NT - 1], ds[NT - 1])

                print('n_instr before ATT', S.n_instr)
                S.barrier()
                if phase_limit < 4:
                    S.stopped = True
                with ExitStack() as st:
                    Gtab = build_gtab(st)
                    KT = [sbuf(st, "a_KT%d" % i, [128, SA], BF16) for i in range(2)]
                    QT = [sbuf(st, "a_QT%d" % i, [128, NLOC], BF16) for i in range(2)]
                    QTm = [[sbuf(st, "a_QTm%d_%d" % (i, c_), [128, NLOC], BF16) for c_ in range(2)] for i in range(2)]
                    VV = [sbuf(st, "a_V%d" % i, [128, NTA, 128], BF16) for i in range(2)]
                    PT = Ring([sbuf(st, "a_PT%d" % i, [128, 512], BF16) for i in range(10)], "a_PT")
                    dacc = [[sbuf(st, "a_dacc%d_%d" % (s_, e_), [128, 512], F32) for e_ in range(2)] for s_ in range(2)]
                    Rinv = Ring([sbuf(st, "a_R%d" % i, [128, 512], F32) for i in range(2)], "a_R")
                    o1 = sbuf(st, "a_o1", [128, 512], F32)
                    od = sbuf(st, "a_od", [128, 512], F32)
                    tq = sbuf(st, "a_tq", [128, 512], F32)
                    sq = sbuf(st, "a_sq", [128, 512], F32)
                    rr = sbuf(st, "a_rr", [128, 512], F32)
                    obT = Ring([sbuf(st, "a_obT%d" % i, [128, 512], BF16) for i in range(2)], "a_obT")

                    def load_head(h):
                        i = h % 2
                        ld(KT[i][:], dk_d[h], ["dk_d"], ["a_KT%d" % i])
                        ld(QT[i][:], dq_d[h], ["dq_d"], ["a_QT%d" % i])
                        for c_ in range(2):
                            ts("pool" if c_ else "dve", QTm[i][c_][:], QT[i][:], hm[:, c_:c_ + 1], None, ALU.mult, None,
                               ["a_QT%d" % i, "hm"], [("a_QTm", i, c_)])
                        ld(VV[i][:], dv_d[:, h * 128:(h + 1) * 128].rearrange("(t p) c -> p t c", p=128),
                           ["dv_d"], ["a_V%d" % i])

                    sps_i = [0]

                    def sps_next():
                        j = sps_i[0] % 4
                        sps_i[0] += 1
                        return pbanks[4 + j], "pb%d" % (4 + j)

                    aps_i = [0]

                    ones_b = sbuf(st, "a_ones_b", [128, 128], BF16)
                    cp("dve", ones_b[:], ones_f[:], ["ones_f"], ["a_ones_b"])

                    def stage1(h, i, qb, c, kt):
                        is_oth = kt >= NT
                        ktl = kt - NT if is_oth else kt
                        dlt = 128 * ktl - 512 * qb
                        tab = None
                        cbi = 0
                        if not is_oth:
                            if -128 <= dlt <= 512:
                                tab, dd = 0, dlt
                            else:
                                cbi = 0 if dlt < 0 else 1
                        else:
                            if dlt + NLOC == 512:
                                tab, dd = 1, 512
                            elif dlt - NLOC == -128:
                                tab, dd = 2, -128
                            else:
                                cbi = 2
                        p, pk = sps_next()
                        mm(p[:], KT[i][:, kt * 128:(kt + 1) * 128], QTm[i][c][:, qb * 512:(qb + 1) * 512], True, tab is None,
                           ["a_KT%d" % i, ("a_QTm", i, c)], [pk])
                        pt_, ptk = PT.next()
                        if tab is not None:
                            s0 = GC0 - dd
                            mm(p[:], ident_b[:], Gtab[:, tab * 4 + h, s0:s0 + 512], False, True,
                               ["ident_b", ("Gtab", tab * 4 + h)], [pk])
                            act(pt_[:], p[:], AF.Exp, [pk], [ptk])
                        else:
                            act(pt_[:], p[:], AF.Exp, [pk, "cb"], [ptk], bias=cb[:, cbi * 4 + h: cbi * 4 + h + 1])
                        return pt_, ptk

                    def stage2(h, i, qb, c, kt, pt_, ptk):
                        OT, OTk = pbanks[c], "pb%d" % c
                        mm(OT[:], VV[i][:, kt, :], pt_[:], kt == 0, kt == NTA - 1, ["a_V%d" % i, ptk], [OTk])
                        e_ = kt % 3
                        if e_ == 2:
                            pS, pSk = pbanks[2 + c], "pb%d" % (2 + c)
                            mm(pS[:], ones_b[:], pt_[:], kt == 2, False, ["a_ones_b", ptk], [pSk])
                        else:
                            dst, dk_ = dacc[c][e_], ("a_dacc", c, e_)
                            eng = "dve" if e_ == 0 else "pool"
                            if kt < 2:
                                cp(eng, dst[:], pt_[:], [ptk], [dk_])
                            else:
                                tt(eng, dst[:], dst[:], pt_[:], ALU.add, [dk_, ptk], [dk_])
                        if kt == NTA - 1:
                            finalize(h, qb, c)

                    def finalize(h, qb, c):
                        OT, OTk = pbanks[c], "pb%d" % c
                        pS, pSk = pbanks[2 + c], "pb%d" % (2 + c)
                        mm(pS[:], ones_f[:], dacc[c][0][:], False, False, ["ones_f", ("a_dacc", c, 0)], [pSk])
                        mm(pS[:], ones_f[:], dacc[c][1][:], False, True, ["ones_f", ("a_dacc", c, 1)], [pSk])
                        R, Rk = Rinv.next()
                        S.op("dve", lambda: nc.vector.reciprocal(R[:], pS[:]), [pSk], [Rk])
                        if c == 0:
                            tt("dve", o1[:], OT[:], R[:], ALU.mult, [OTk, Rk], ["a_o1"])
                        else:
                            stt(tq[:], OT[:], nlam[:, 0:1], R[:], ALU.mult, ALU.mult, [OTk, "nlam", Rk], ["a_tq"])
                            tt("pool", od[:], tq[:], o1[:], ALU.add, ["a_tq", "a_o1"], ["a_od"])
                            tt("pool", sq[:], od[:], od[:], ALU.mult, ["a_od"], ["a_sq"])
                            pQ, pQk = sps_next()
                            mm(pQ[:], ones_f[:], sq[:], True, True, ["ones_f", "a_sq"], [pQk])
                            act(rr[:], pQ[:], AF.Ln, [pQk, "eps_c"], ["a_rr"], bias=eps_c[:], scale=1.0 / 128.0)
                            act(rr[:], rr[:], AF.Exp, ["a_rr"], ["a_rr"], scale=-0.5)
                            ob_, obk = obT.next()
                            stt(ob_[:], od[:], subgs[:, 0:1], rr[:], ALU.mult, ALU.mult, ["a_od", "subgs", "a_rr"], [obk])
                            ld(ob_d[h, :, qb * 512:(qb + 1) * 512], ob_[:], [obk], ["ob_d"])

                    LOOK = 2
                    load_head(0)
                    for h in range(4):
                        if h + 1 < 4:
                            load_head(h + 1)
                        i = h % 2
                        pend = []
                        for qb in range(NB):
                            for c in range(2):
                                for kt in range(NTA):
                                    pend.append((qb, c, kt) + stage1(h, i, qb, c, kt))
                                    if len(pend) > LOOK:
                                        qb_, c_, kt_, pt_, ptk = pend.pop(0)
                                        stage2(h, i, qb_, c_, kt_, pt_, ptk)
                        while pend:
                            qb_, c_, kt_, pt_, ptk = pend.pop(0)
                            stage2(h, i, qb_, c_, kt_, pt_, ptk)

                print('n_instr before MERGE', S.n_instr)
                S.barrier()
                if phase_limit < 5:
                    S.stopped = True
                with ExitStack() as st:
                    w_a = sbuf(st, "m_wa", [128, 4, D], BF16)
                    w_b = sbuf(st, "m_wb", [128, 4, D], BF16)
                    w_o = sbuf(st, "m_wo", [128, 8, D], BF16)
                    w_r = sbuf(st, "m_wr", [128, 8, 36], F32)
                    ld(w_a[:], Wl["w_a"].rearrange("(k p) n -> p k n", p=128), [], ["m_wa"], q="pool")
                    ld(w_b[:], Wl["w_b"].rearrange("(k p) n -> p k n", p=128), [], ["m_wb"], q="pool")
                    ld(w_o[:], Wl["w_o"].rearrange("(k p) n -> p k n", p=128), [], ["m_wo"], q="pool")
                    ld(w_r[:], Wl["w_r"].rearrange("(k p) n -> p k n", p=128), [], ["m_wr"])
                    oaB = sbuf(st, "m_oa", [128, 4, 512], BF16)
                    obB = sbuf(st, "m_ob", [128, 4, 512], BF16)
                    gaB = sbuf(st, "m_ga", [128, 8, 512], BF16)
                    gbB = sbuf(st, "m_gb", [128, 8, 512], BF16)
                    xB = sbuf(st, "m_x", [128, 4, D], F32)
                    x1B = sbuf(st, "m_x1", [128, 4, D], F32)
                    mg = sbuf(st, "m_mg", [128, 8, 512], BF16)
                    t1 = Ring([sbuf(st, "m_t1%d" % i, [128, 512], F32) for i in range(2)], "m_t1")
                    t2 = Ring([sbuf(st, "m_t2%d" % i, [128, 512], F32) for i in range(2)], "m_t2")
                    a2b = sbuf(st, "m_a2b", [128, D], F32)
                    sh2b = sbuf(st, "m_sh2b", [128, D], F32)
                    ld(a2b[:], bass.AP(rows_d.tensor, 0, [[0, 128], [1, D]]), ["rows_d"], ["m_a2b"])
                    ld(sh2b[:], bass.AP(rows_d.tensor, D, [[0, 128], [1, D]]), ["rows_d"], ["m_sh2b"])
                    htmp = Ring([sbuf(st, "m_htmp%d" % i_, [128, D], F32) for i_ in range(2)], "m_htmp")
                    h2tok = [sbuf(st, "m_h2tok%d" % i_, [128, D], BF16) for i_ in range(4)]
                    rbase = sbuf(st, "r_base", [128, NE], F32)
                    S.op("dve", lambda: nc.vector.memset(rbase[:], 0.0), [], ["r_base"])
                    rMT = [sbuf(st, "r_M%d" % t_, [128, 3, NE], F32) for t_ in range(4)]
                    rrkT = [sbuf(st, "r_rk%d" % t_, [128, 5, NE], F32) for t_ in range(4)]
                    rslT = [sbuf(st, "r_sl%d" % t_, [128, 8], F32) for t_ in range(4)]
                    rbt = sbuf(st, "r_bt", [128, 4, NE], F32)
                    sidx = Ring([sbuf(st, "r_sidx%d" % i_, [128, 2], mybir.dt.int32) for i_ in range(4)], "r_sidx")
                    h2T32 = sbuf(st, "m_h2T32", [128, 8, 512], F32)
                    scr2 = dict(ss=sbuf(st, "m_ss", [128, 12], F32), junk=sbuf(st, "m_junk", [128, D], BF16),
                                ac=a2c)
                    rlT = [sbuf(st, "r_l%d" % t_, [128, 36], F32) for t_ in range(4)]
                    rwT = [sbuf(st, "r_w%d" % t_, [128, 64], F32) for t_ in range(4)]
                    rmlT = [sbuf(st, "r_ml%d" % t_, [128, 32], F32) for t_ in range(4)]
                    rm8T = [sbuf(st, "r_m8%d" % t_, [128, 8], F32) for t_ in range(4)]
                    rjunkT = [sbuf(st, "r_junk%d" % t_, [128, 4], F32) for t_ in range(4)]

                    def lockstep(gens):
                        gens = list(gens)
                        while gens:
                            for g_ in list(gens):
                                try:
                                    next(g_)
                                except StopIteration:
                                    gens.remove(g_)
                    for i in range(NB):
                        bs = slice(i * 512, (i + 1) * 512)
                        ld(oaB[:], oa_d[:, :, bs].rearrange("j p n -> p j n"), ["oa_d"], ["m_oa"])
                        ld(obB[:], ob_d[:, :, bs].rearrange("j p n -> p j n"), ["ob_d"], ["m_ob"])
                        ld(gaB[:], ga_d[:, :, bs].rearrange("j p n -> p j n"), ["ga_d"], ["m_ga"])
                        ld(gbB[:], gb_d[:, :, bs].rearrange("j p n -> p j n"), ["gb_d"], ["m_gb"])
                        ld(xB[:], x_loc_block(i).rearrange("(t p) d -> p t d", p=128),
                           [("xmid", t_) for t_ in range(NT)] if li > 0 else [], ["m_x"])
                        for m in range(8):
                            pa, pak = PS.next()
                            for k in range(4):
                                mm(pa[:], w_a[:, k, m * 128:(m + 1) * 128], oaB[:, k, :], k == 0, k == 3, ["m_wa", "m_oa"], [pak])
                            pb_, pbk = PS.next()
                            for k in range(4):
                                mm(pb_[:], w_b[:, k, m * 128:(m + 1) * 128], obB[:, k, :], k == 0, k == 3, ["m_wb", "m_ob"], [pbk])
                            ta, tak = t1.next()
                            tb, tbk = t2.next()
                            tt("dve", ta[:], pa[:], gaB[:, m, :], ALU.mult, [pak, "m_ga"], [tak])
                            tt("dve", tb[:], pb_[:], gbB[:, m, :], ALU.mult, [pbk, "m_gb"], [tbk])
                            tt("pool", mg[:, m, :], ta[:], tb[:], ALU.add, [tak, tbk], [("m_mg", m)])
                        for t in range(4):
                            for n in range(2):
                                p, pk = PS.next()
                                for k in range(8):
                                    mm(p[:], mg[:, k, t * 128:(t + 1) * 128], w_o[:, k, n * 512:(n + 1) * 512], k == 0, k == 7,
                                       [("m_mg", k), "m_wo"], [pk])
                                ta, tak = t1.next()
                                tt("dve", ta[:], p[:], g1b[:, n * 512:(n + 1) * 512], ALU.mult, [pk, "g1b"], [tak])
                                tt("pool", x1B[:, t, n * 512:(n + 1) * 512], ta[:], xB[:, t, n * 512:(n + 1) * 512], ALU.add,
                                   [tak, "m_x"], ["m_x1"])
                        ld(x1_d[bs, :].rearrange("(t p) d -> p t d", p=128), x1B[:], ["m_x1"], ["x1_d"])
                        norm_block(x1B, "m_x1", "a2c", 24, None, "m_h2T", scr2, hT32=h2T32)
                        for t in range(4):
                            hm_, hmk = htmp.next()
                            tt("dve", hm_[:], x1B[:, t, :], a2b[:], ALU.mult, ["m_x1", "m_a2b"], [hmk])
                            tt("pool", h2tok[t][:], hm_[:], sh2b[:], ALU.add, [hmk, "m_sh2b"], [("m_h2tok", t)])
                        pIs = [None] * 4

                        def rt_A(t):
                            tg = i * 4 + t
                            rl, rw, rml, rm8, rjunk, rM = rlT[t], rwT[t], rmlT[t], rm8T[t], rjunkT[t], rMT[t]
                            K = lambda n: (n, t)
                            p, pk = PS.next()
                            for k in range(8):
                                mm(p[:, 0:36], h2T32[:, k, t * 128:(t + 1) * 128], w_r[:, k, :], k == 0, k == 7,
                                   [("m_h2T32", k), "m_wr"], [pk])
                            yield
                            tt("dve", rl[:], p[:, 0:36], brb[:], ALU.add, [pk, "brb"], [K("r_l")])
                            yield
                            S.op("dve", lambda: nc.vector.reduce_max(rw[:, 0:1], rl[:, 0:4], axis=AX.X), [K("r_l")], [K("r_w0")])
                            yield
                            ts("dve", rw[:, 1:2], rw[:, 0:1], -1.0, None, ALU.mult, None, [K("r_w0")], [K("r_w1")])
                            yield
                            act(rjunk[:], rl[:, 0:4], AF.Exp, [K("r_l"), K("r_w1")], [K("r_junk")], bias=rw[:, 1:2], accum=rw[:, 2:3])
                            yield
                            S.op("dve", lambda: nc.vector.reciprocal(rw[:, 3:4], rw[:, 2:3]), [K("r_junk")], [K("r_w3")])
                            yield
                            ts("dve", rw[:, 4:8], rl[:, 0:4], rw[:, 0:1], None, ALU.is_ge, None, [K("r_l"), K("r_w0")], [K("r_w4")])
                            yield
                            ts("dve", rw[:, 8:12], rw[:, 4:8], -1.0, 1e30, ALU.add, ALU.mult, [K("r_w4")], [K("r_w8")])
                            yield
                            for g in range(4):
                                ts("dve", rml[:, g * 8:(g + 1) * 8], rl[:, 4 + g * 8: 4 + (g + 1) * 8], rw[:, 8 + g: 9 + g], None,
                                   ALU.add, None, [K("r_l"), K("r_w8")], [K("r_ml")])
                                yield
                            S.op("dve", lambda: nc.vector.max(out=rm8[:], in_=rml[:]), [K("r_ml")], [K("r_m8")])
                            yield
                            tt("dve", rw[:, 12:13], rm8[:, 1:2], rm8[:, 0:1], ALU.subtract, [K("r_m8")], [K("r_w12")])
                            yield
                            act(rw[:, 13:14], rw[:, 12:13], AF.Exp, [K("r_w12")], [K("r_w13")])
                            yield
                            ts("dve", rw[:, 14:15], rw[:, 13:14], 1.0, None, ALU.add, None, [K("r_w13")], [K("r_w14")])
                            yield
                            S.op("dve", lambda: nc.vector.reciprocal(rw[:, 15:16], rw[:, 14:15]), [K("r_w14")], [K("r_w15")])
                            yield
                            tt("dve", rw[:, 16:17], rw[:, 15:16], rw[:, 3:4], ALU.mult, [K("r_w15"), K("r_w3")], [K("r_w16")])
                            yield
                            tt("dve", rw[:, 17:18], rw[:, 16:17], rw[:, 13:14], ALU.mult, [K("r_w16"), K("r_w13")], [K("r_w17")])
                            yield
                            cp("dve", gat[:, tg, 0:2], rw[:, 16:18], [K("r_w16"), K("r_w17")], [("gat", tg)])
                            yield
                            ts("dve", rM[:, 0, :], rml[:], rm8[:, 0:1], None, ALU.is_equal, None, [K("r_ml"), K("r_m8")], [K("r_M0")])
                            yield
                            ts("dve", rM[:, 1, :], rml[:], rm8[:, 1:2], None, ALU.is_equal, None, [K("r_ml"), K("r_m8")], [K("r_M1")])
                            yield
                            tt("dve", rM[:, 2, :], rM[:, 0, :], rM[:, 1, :], ALU.add, [K("r_M0"), K("r_M1")], [K("r_M2")])
                            yield
                            pI, pIk = PS.next()
                            mm(pI[:, 0:NE], tril_f[:, 0:128], rM[:, 2, :], True, True, ["tril_f", K("r_M2")], [pIk])
                            mm(pI[:, NE:2 * NE], ones_f[:], rM[:, 2, :], True, True, ["ones_f", K("r_M2")], [pIk])
                            pIs[t] = (pI, pIk)
                            yield

                        def rt_B(t):
                            tg = i * 4 + t
                            rM, rrk, rsl = rMT[t], rrkT[t], rslT[t]
                            K = lambda n: (n, t)
                            pI, pIk = pIs[t]
                            tt("dve", rrk[:, 0, :], pI[:, 0:NE], rbt[:, t, :], ALU.add, [pIk, ("r_bt", t)], [K("r_rk0")])
                            yield
                            tt("dve", rrk[:, 0, :], rrk[:, 0, :], rM[:, 2, :], ALU.subtract, [K("r_rk0"), K("r_M2")], [K("r_rk0")])
                            yield
                            ts("dve", rrk[:, 1, :], rrk[:, 0, :], float(CAP), None, ALU.is_lt, None, [K("r_rk0")], [K("r_rk1")])
                            yield
                            tt("dve", rrk[:, 2, :], rrk[:, 0, :], ecap[:], ALU.add, [K("r_rk0"), "ecap"], [K("r_rk2")])
                            yield
                            for k_ in range(2):
                                tt("dve", rrk[:, 3, :], rrk[:, 2, :], rM[:, k_, :], ALU.mult, [K("r_rk2"), K("r_M%d" % k_)], [K("r_rk3")])
                                yield
                                S.op("dve", lambda k_=k_: nc.vector.reduce_sum(rsl[:, k_:k_ + 1], rrk[:, 3, :], axis=AX.X),
                                     [K("r_rk3")], [K("r_sl")])
                                yield
                                tt("dve", rrk[:, 4, :], rrk[:, 1, :], rM[:, k_, :], ALU.mult, [K("r_rk1"), K("r_M%d" % k_)], [K("r_rk4")])
                                yield
                                S.op("dve", lambda k_=k_: nc.vector.reduce_sum(rsl[:, 2 + k_:3 + k_], rrk[:, 4, :], axis=AX.X),
                                     [K("r_rk4")], [K("r_sl")])
                                yield
                            ts("dve", rsl[:, 4:6], rsl[:, 0:2], trash[:, 0:1], None, ALU.subtract, None, [K("r_sl"), "trash"], [K("r_sl")])
                            yield
                            tt("dve", rsl[:, 4:6], rsl[:, 4:6], rsl[:, 2:4], ALU.mult, [K("r_sl")], [K("r_sl")])
                            yield
                            ts("dve", rsl[:, 4:6], rsl[:, 4:6], trash[:, 0:1], None, ALU.add, None, [K("r_sl"), "trash"], [K("r_sl")])
                            yield
                            ts("dve", rsl[:, 6:8], rsl[:, 0:2], -float(NE * CAP), None, ALU.add, None, [K("r_sl")], [K("r_sl")])
                            yield
                            tt("dve", rsl[:, 6:8], rsl[:, 6:8], rsl[:, 2:4], ALU.mult, [K("r_sl")], [K("r_sl")])
                            yield
                            ts("dve", rsl[:, 6:8], rsl[:, 6:8], float(NE * CAP), None, ALU.add, None, [K("r_sl")], [K("r_sl")])
                            yield
                            si_, sik = sidx.next()
                            cp("dve", si_[:], rsl[:, 4:6], [K("r_sl")], [sik])
                            yield
                            cp("dve", gix[:, tg, :], rsl[:, 6:8], [K("r_sl")], [("gix", tg)])
                            yield
                            for k_ in range(2):
                                S.dma("pool", (lambda k_=k_, si_=si_: nc.gpsimd.indirect_dma_start(
                                    out=Xg[:, :], out_offset=bass.IndirectOffsetOnAxis(ap=si_[:, k_:k_ + 1], axis=0),
                                    in_=h2tok[t][:, :], in_offset=None)),
                                    [("m_h2tok", t), sik], [("Xg", tg, k_)])
                            yield

                        lockstep(rt_A(t) for t in range(4))
                        cp("dve", rbt[:, 0, :], rbase[:], ["r_base"], [("r_bt", 0)])
                        for t in range(1, 4):
                            tt("dve", rbt[:, t, :], rbt[:, t - 1, :], pIs[t - 1][0][:, NE:2 * NE], ALU.add,
                               [("r_bt", t - 1), pIs[t - 1][1]], [("r_bt", t)])
                        tt("dve", rbase[:], rbt[:, 3, :], pIs[3][0][:, NE:2 * NE], ALU.add, [("r_bt", 3), pIs[3][1]], ["r_base"])
                        lockstep(rt_B(t) for t in range(4))

                print('n_instr before MOE', S.n_instr)
                S.barrier()
                if phase_limit < 6:
                    S.stopped = True
                with ExitStack() as st:
                    CS = CAP // 128
                    CB = CAP // 512
                    w1s = [sbuf(st, "e_w1%d" % i, [128, 8, 2 * DEXP], BF16) for i in range(2)]
                    w2s = [sbuf(st, "e_w2%d" % i, [128, 4, D], BF16) for i in range(2)]
                    xtok = [sbuf(st, "e_xtok%d" % i, [128, 4, D], BF16) for i in range(2)]
                    xT = [sbuf(st, "e_xT%d" % i, [128, 8, 512], BF16) for i in range(2)]
                    sg = Ring([sbuf(st, "e_sg%d" % i, [128, 512], BF16) for i in range(3)], "e_sg")
                    actT = [sbuf(st, "e_act%d" % i, [128, 4, 512], BF16) for i in range(2)]
                    ybuf = Ring([sbuf(st, "e_y%d" % i, [128, D], F32) for i in range(3)], "e_y")
                    xc = Ring([sbuf(st, "e_xc%d" % i, [128, D], F32) for i in range(4)], "e_xc")
                    y1r = Ring([sbuf(st, "e_y1%d" % i, [128, D], F32) for i in range(4)], "e_y1")
                    y2r = Ring([sbuf(st, "e_y2%d" % i, [128, D], F32) for i in range(4)], "e_y2")
                    xg_keys = [("Xg", tg_, k_) for tg_ in range(NT) for k_ in range(2)]
                    yg_keys = []

                    def load_w(e):
                        i = e % 2
                        ld(w1s[i][:], Wl["w1"][e].rearrange("(k p) n -> p k n", p=128), [], ["e_w1%d" % i], q="pool")
                        ld(w2s[i][:], Wl["w2"][e].rearrange("(k p) n -> p k n", p=128), [], ["e_w2%d" % i], q="pool")
                        for k in range(4):
                            tt("pool", w2s[i][:, k, :], w2s[i][:, k, :], g2b[:], ALU.mult, ["e_w2%d" % i, "g2b"], ["e_w2%d" % i])

                    nblk = 0

                    def load_xtok(e, blk):
                        j = (e * CB + blk) % 2
                        r0 = e * CAP + blk * 512
                        ld(xtok[j][:], Xg[r0:r0 + 512, :].rearrange("(s p) d -> p s d", p=128), xg_keys, ["e_xtok%d" % j])

                    load_w(0)
                    load_xtok(0, 0)
                    for e in range(NE):
                        if e + 1 < NE:
                            load_w(e + 1)
                        i = e % 2
                        w1, w1k, w2, w2k = w1s[i], "e_w1%d" % i, w2s[i], "e_w2%d" % i
                        for blk in range(CB):
                            j_ = nblk % 2
                            nblk += 1
                            if blk + 1 < CB:
                                load_xtok(e, blk + 1)
                            elif e + 1 < NE:
                                load_xtok(e + 1, 0)
                            xt_, xtk = xtok[j_], "e_xtok%d" % j_
                            xT_, xTk = xT[j_], "e_xT%d" % j_
                            for k in range(8):
                                pT, pTk = PS.next()
                                pTb = pT[:].bitcast(BF16)
                                for s_i in range(4):
                                    tr(pTb[:, s_i * 128:(s_i + 1) * 128], xt_[:, s_i, k * 128:(k + 1) * 128], ident_b[:],
                                       [xtk, "ident_b"], [pTk])
                                cp("dve" if k % 2 else "act", xT_[:, k, :], pTb[:, 0:512], [pTk], [(xTk, k)])
                            at, atk = actT[j_], "e_act%d" % j_
                            for j in range(4):
                                pg, pgk = PS.next()
                                for k in range(8):
                                    mm(pg[:], w1[:, k, j * 128:(j + 1) * 128], xT_[:, k, :], k == 0, k == 7, [w1k, (xTk, k)], [pgk])
                                pu, puk = PS.next()
                                for k in range(8):
                                    mm(pu[:], w1[:, k, DEXP + j * 128: DEXP + (j + 1) * 128], xT_[:, k, :], k == 0, k == 7,
                                       [w1k, (xTk, k)], [puk])
                                s_, sk = sg.next()
                                act(s_[:], pg[:], AF.Silu, [pgk], [sk])
                                tt("dve", at[:, j, :], pu[:], s_[:], ALU.mult, [puk, sk], [(atk, j)])
                            for t in range(4):
                                yb, ybk = ybuf.next()
                                for n in range(2):
                                    py, pyk = PS.next()
                                    for j in range(4):
                                        mm(py[:], at[:, j, t * 128:(t + 1) * 128], w2[:, j, n * 512:(n + 1) * 512], j == 0, j == 3,
                                           [(atk, j), w2k], [pyk])
                                    cp("act" if n else "dve", yb[:, n * 512:(n + 1) * 512], py[:], [pyk], [(ybk, n)])
                                r0 = e * CAP + blk * 512 + t * 128
                                yk = ("Yg", e, blk, t)
                                yg_keys.append(yk)
                                ld(Yg[r0:r0 + 128, :], yb[:], [(ybk, 0), (ybk, 1)], [yk])
                    def emit_cc(j):
                        rg = [[2 * g_, 2 * g_ + 1] for g_ in range(NCORES // 2)]
                        S.cc(lambda: nc.gpsimd.collective_compute(
                            "AllGather", ALU.bypass, replica_groups=rg, ins=[xmid_c[j].opt()], outs=[xg_c[j].opt()]),
                            [("xmid", t_) for t_ in range(4 * j, 4 * j + 4)], ["xg"])

                    for tg in range(NT):
                        x_, xk_ = xc.next()
                        ld(x_[:], x1_d[tg * 128:(tg + 1) * 128, :], ["x1_d"], [xk_])
                        ya, yak = y1r.next()
                        yb2, ybk2 = y2r.next()
                        for (yt_, ytk, k_) in ((ya, yak, 0), (yb2, ybk2, 1)):
                            S.dma("pool", (lambda yt_=yt_, k_=k_: nc.gpsimd.indirect_dma_start(
                                out=yt_[:, :], out_offset=None, in_=Yg[:, :],
                                in_offset=bass.IndirectOffsetOnAxis(ap=gix[:, tg, k_:k_ + 1], axis=0))),
                                yg_keys + ["Yg", ("gix", tg)], [ytk])
                        stt(x_[:], ya[:], gat[:, tg, 0:1], x_[:], ALU.mult, ALU.add, [yak, ("gat", tg), xk_], [xk_])
                        stt(x_[:], yb2[:], gat[:, tg, 1:2], x_[:], ALU.mult, ALU.add, [ybk2, ("gat", tg), xk_], [xk_])
                        if is_last:
                            ld(x_out[tg * 128:(tg + 1) * 128, :], x_[:], [xk_], [("xout", tg)])
                        else:
                            ld(xmid_c[tg // 4][(tg % 4) * 128:(tg % 4 + 1) * 128, :], x_[:], [xk_], [("xmid", tg)])
                            if tg % 4 == 3 and tg >= 7:
                                emit_cc(tg // 4 - 1)
                    if not is_last:
                        emit_cc(NB - 1)
        except _Stop:
            pass
        S.finish("sp")
    return nc, S


def _t5_bucket_np(rel):
    import jax
    import jax.numpy as jnp
    cpu = jax.devices("cpu")[0]
    with jax.default_device(cpu):
        rel = jnp.asarray(rel, dtype=jnp.int32)
        nb = 16
        max_exact = 8
        ret = jnp.where(rel > 0, nb, 0)
        n = jnp.abs(rel)
        nf = jnp.maximum(n, 1).astype(jnp.float32)
        large = max_exact + (jnp.log(nf / max_exact) / math.log(128 / max_exact) * (nb - max_exact)).astype(jnp.int32)
        large = jnp.minimum(large, nb - 1)
        out = ret + jnp.where(n < max_exact, n, large)
        return np.asarray(out)


def _onehot(b):
    oh = np.zeros((32, b.shape[0]), np.float32)
    oh[b, np.arange(b.shape[0])] = 1.0
    return oh


def _core_consts(hf, NLOC):
    shift = NLOC if hf == 0 else -NLOC
    n = np.arange(TVW)
    base = GC0 + 127 - n
    tabs = [base, base - NLOC + shift, base + NLOC + shift]
    oh = np.concatenate([_onehot(_t5_bucket_np(t)) for t in tabs], axis=1)
    cvals = _t5_bucket_np(np.array([-100000, 100000, shift]))
    ohc = np.concatenate([np.repeat(_onehot(cvals[k:k + 1]), 128, axis=1) for k in range(3)], axis=1)
    flags = np.zeros((128, 2), np.float32)
    flags[:, 0] = 1.0 if hf == 1 else 0.0
    flags[:, 1] = 1.0 if hf == 0 else 0.0
    return oh, ohc, flags


def _col(v, k):
    return np.ascontiguousarray(np.asarray(v, np.float32).reshape(k, 128).T)


def make_in_maps(inp, SEQ, layers, x_override=None):
    NLOC = SEQ // 2
    B = inp["x"].shape[0]
    f = lambda a: np.ascontiguousarray(np.asarray(a, np.float32))
    ident = np.eye(128, dtype=np.float32)
    jmat = np.ascontiguousarray(ident[::-1])
    bones = np.zeros((128, 128), np.float32)
    bones[:64, :64] = 1.0
    bones[64:, 64:] = 1.0
    jj, ii = np.meshgrid(np.arange(128), np.arange(128), indexing="ij")
    tril = (jj <= ii).astype(np.float32)
    triu = (jj > ii).astype(np.float32)
    hmask = np.zeros((128, 2), np.float32)
    hmask[:64, 0] = 1.0
    hmask[64:, 1] = 1.0
    ecap = np.tile((np.arange(NE, dtype=np.float32) * CAP)[None, :], (128, 1)).astype(np.float32)
    trash = (NE * CAP + np.arange(128, dtype=np.float32)).reshape(128, 1).astype(np.float32)
    shared = dict(trash=trash, ecap=ecap, hmask=hmask, ident=ident, jmat=jmat, bones=bones, ones=np.ones((128, 128), np.float32),
                  tril=np.concatenate([tril, tril], 1), triu=np.concatenate([triu, triu], 1),
                  rel_bias=f(inp["rel_bias"]))
    for l in layers:
        shared.update({
            "w_ada%d" % l: f(inp["w_ada"][l]), "b_ada%d" % l: f(inp["b_ada"][l]).reshape(1, -1),
            "n1g%d" % l: _col(inp["norm1_g"][l], 8), "n2g%d" % l: _col(inp["norm2_g"][l], 8),
            "w_in%d" % l: f(inp["w_in"][l]), "w_up%d" % l: f(inp["gla_w_up"][l]),
            "b_up%d" % l: _col(np.asarray(inp["gla_b_up"][l]).reshape(-1), 4),
            "glag%d" % l: f(inp["gla_norm_g"][l]).reshape(128, 1),
            "qng%d" % l: np.ascontiguousarray(np.tile(f(inp["diff_qnorm_g"][l]), 2).reshape(128, 1)),
            "kng%d" % l: np.ascontiguousarray(np.tile(f(inp["diff_knorm_g"][l]), 2).reshape(128, 1)),
            "lamv%d" % l: f(inp["diff_lambda"][l]).reshape(1, 256),
            "subg%d" % l: f(inp["diff_subnorm_g"][l]).reshape(128, 1),
            "w_a%d" % l: f(inp["w_branch_a"][l]), "w_b%d" % l: f(inp["w_branch_b"][l]), "w_o%d" % l: f(inp["w_out"][l]),
            "w_r%d" % l: np.ascontiguousarray(np.concatenate([f(inp["w_router_group"][l]), f(inp["w_router_expert"][l])], 1)),
            "b_r%d" % l: np.concatenate([f(inp["b_router_group"][l]), f(inp["b_router_expert"][l])]).reshape(1, 36),
            "n2grow%d" % l: f(inp["norm2_g"][l]).reshape(1, D),
            "w1_%d" % l: f(inp["w_expert_in"][l]), "w2_%d" % l: f(inp["w_expert_out"][l]),
        })
    x = f(inp["x"]) if x_override is None else x_override
    maps = []
    consts = [_core_consts(hf, NLOC) for hf in range(2)]
    for core in range(2 * B):
        b, hf = core // 2, core % 2
        oh, ohc, flags = consts[hf]
        m = dict(shared)
        m["x_loc"] = np.ascontiguousarray(x[b, hf * NLOC:(hf + 1) * NLOC])
        m["x_oth"] = np.ascontiguousarray(x[b, (1 - hf) * NLOC:(2 - hf) * NLOC])
        m["c_col"] = _col(inp["c"][b], 8)
        m["oh"], m["ohc"], m["flags"] = oh, ohc, flags
        maps.append(m)
    return maps


_CACHE = {}


def run_layers(inp, SEQ, layer_groups, dbg=False, phase_limit=99):
    B = inp["x"].shape[0]
    NLOC = SEQ // 2
    x = np.ascontiguousarray(np.asarray(inp["x"], np.float32))
    for grp in layer_groups:
        key = (SEQ, tuple(grp))
        if key not in _CACHE:
            _CACHE[key] = build_program(SEQ, grp, dbg=dbg, phase_limit=phase_limit, NCORES=2 * B)[0]
        nc = _CACHE[key]
        maps = make_in_maps(inp, SEQ, grp, x_override=x)
        res = run_bass_kernel_spmd(nc, maps, core_ids=list(range(2 * B)))
        xn = np.empty_like(x)
        if dbg:
            return res
        for core in range(2 * B):
            b, hf = core // 2, core % 2
            xn[b, hf * NLOC:(hf + 1) * NLOC] = res.results[core]["x_out"]
        x = xn
    return x


def kernel(**inputs):
    SEQ = inputs["x"].shape[1]
    return run_layers(inputs, SEQ, [[0, 1]])
```

```python
import math
from contextlib import ExitStack

import numpy as np
import concourse.bass as bass
import concourse.mybir as mybir
from concourse.bass_utils import run_bass_kernel_spmd

F32 = mybir.dt.float32
BF16 = mybir.dt.bfloat16
AF = mybir.ActivationFunctionType
ALU = mybir.AluOpType
AX = mybir.AxisListType

D = 1024
DEPTH = 2
NE = 32
CAP = 1024
DEXP = 512
RMS_EPS = 1e-6
GW = 1152
GC0 = 512
TVW = 1280
C_GQ, C_GK, C_GV, C_GG, C_LRF, C_LRB, C_DQ, C_DK, C_DV, C_GA, C_GB = (
    0, 256, 512, 1024, 1536, 1552, 1568, 2080, 2592, 3104, 4128)
D_IN = 5152


class Sched:
    ENG_SEM_MAX = 30000

    def __init__(self, nc, stack, n_dma_sems=32):
        self.nc = nc
        self.stack = stack
        self.engs = {"pe": nc.tensor, "act": nc.scalar, "dve": nc.vector,
                     "pool": nc.gpsimd, "sp": nc.sync}
        self.sem = {}
        self.cnt = {}
        self.old = {}
        self.nsem = 0
        for e in self.engs:
            self._new_eng_sem(e)
        self.known = {e: {} for e in self.engs}
        self.dma_sems = [self._mk_sem("dq") for _ in range(n_dma_sems)]
        self.dma_cnt = [0] * n_dma_sems
        self.dma_rr = 0
        self.res = {}
        self.n_instr = 0
        self.stopped = False
        import os
        self.max_instr = int(os.environ.get('BASS_MAXI', '100000000'))

    def _mk_sem(self, name):
        self.nsem += 1
        return self.stack.enter_context(self.nc.semaphore("%s_%d" % (name, self.nsem)))

    def _new_eng_sem(self, e):
        if e in self.sem:
            self.old[e] = (self.sem[e], self.cnt[e], e)
        self.sem[e] = self._mk_sem("s" + e)
        self.cnt[e] = 0

    def _wait(self, eng, tok):
        sem, val, _ = tok
        k = self.known[eng]
        sid = id(sem)
        if k.get(sid, 0) >= val:
            return
        self.engs[eng].wait_ge(sem, val)
        k[sid] = val

    def _deps(self, eng, reads, writes):
        toks = []
        for key in reads:
            r = self.res.get(key)
            if r is not None and r[0] is not None:
                toks.append(r[0])
        for key in writes:
            r = self.res.get(key)
            if r is not None:
                if r[0] is not None and (r[0][2] != eng or eng == "dma"):
                    toks.append(r[0])
                for t in r[1]:
                    if t[2] != eng or eng == "dma":
                        toks.append(t)
        for t in toks:
            if t[2] == "pe" and eng == "pe":
                continue
            self._wait(eng, t)

    def _update(self, tok, reads, writes):
        for key in reads:
            r = self.res.setdefault(key, [None, []])
            r[1].append(tok)
        for key in writes:
            self.res[key] = [tok, []]

    def op(self, eng, fn, reads=(), writes=()):
        if self.n_instr >= self.max_instr:
            self.stopped = True
        if self.stopped:
            return
        self._deps(eng, reads, writes)
        ins = fn()
        if self.cnt[eng] >= self.ENG_SEM_MAX:
            self._new_eng_sem(eng)
        self.cnt[eng] += 1
        ins.then_inc(self.sem[eng], 1)
        self._update((self.sem[eng], self.cnt[eng], eng), reads, writes)
        self.n_instr += 1

    def dma(self, eng, fn, reads=(), writes=()):
        if self.n_instr >= self.max_instr:
            self.stopped = True
        if self.stopped:
            return
        j = self.dma_rr
        self.dma_rr = (self.dma_rr + 1) % len(self.dma_sems)
        sem = self.dma_sems[j]
        if self.dma_cnt[j] > 0:
            self._wait(eng, (sem, 16 * self.dma_cnt[j], "dma"))
        self._deps(eng, reads, writes)
        ins = fn()
        self.dma_cnt[j] += 1
        ins.then_inc(sem, 16)
        self._update((sem, 16 * self.dma_cnt[j], "dma"), reads, writes)
        self.n_instr += 1

    def barrier(self):
        if self.stopped:
            return
        for eng in self.engs:
            for e2 in self.engs:
                if e2 != eng and self.cnt[e2] > 0:
                    self._wait(eng, (self.sem[e2], self.cnt[e2], e2))
                if e2 != eng and e2 in self.old:
                    self._wait(eng, self.old[e2])
            for j, sem in enumerate(self.dma_sems):
                if self.dma_cnt[j] > 0:
                    self._wait(eng, (sem, 16 * self.dma_cnt[j], "dma"))

    def cc(self, fn, reads=(), writes=()):
        if self.stopped:
            return
        if not hasattr(self, "cc_sem"):
            self.cc_sem = self._mk_sem("cc")
            self.cc_cnt = 0
        self._deps("pool", reads, writes)
        ins = fn()
        self.cc_cnt += 1
        ins.then_inc(self.cc_sem)
        self._update((self.cc_sem, self.cc_cnt, "dma"), reads, writes)
        self.n_instr += 1

    def finish(self, eng="sp"):
        if hasattr(self, "cc_sem") and self.cc_cnt > 0:
            self._wait(eng, (self.cc_sem, self.cc_cnt, "dma"))
        for j, sem in enumerate(self.dma_sems):
            if self.dma_cnt[j] > 0:
                self._wait(eng, (sem, 16 * self.dma_cnt[j], "dma"))


class Ring:
    def __init__(self, tiles, name):
        self.tiles = tiles
        self.name = name
        self.i = 0

    def next(self):
        j = self.i % len(self.tiles)
        self.i += 1
        return self.tiles[j], "%s%d" % (self.name, j)


class _Stop(Exception):
    pass


def build_program(SEQ, layers, dbg=False, phase_limit=99, NCORES=8):
    NLOC = SEQ // 2
    NB = NLOC // 512
    NT = NLOC // 128
    NTA = 2 * NT
    SA = 2 * NLOC
    nc = bass.Bass("TRN2", target_bir_lowering=False)

    def din(name, shape, dt=F32):
        return nc.dram_tensor(name, list(shape), dt, kind="ExternalInput").ap()

    def dscr(name, shape, dt):
        return nc.dram_tensor(name, list(shape), dt, kind="ExternalOutput" if dbg else "Internal").ap()

    x_loc_in = din("x_loc", [NLOC, D])
    x_oth_in = din("x_oth", [NLOC, D])
    c_col = din("c_col", [128, 8])
    ident_in = din("ident", [128, 128])
    jmat_in = din("jmat", [128, 128])
    bones_in = din("bones", [128, 128])
    ones_in = din("ones", [128, 128])
    tril_in = din("tril", [128, 256])
    triu_in = din("triu", [128, 256])
    oh_in = din("oh", [32, 3 * TVW])
    ohc_in = din("ohc", [32, 3 * 128])
    flags_in = din("flags", [128, 2])
    hmask_in = din("hmask", [128, 2])
    ecap_in = din("ecap", [128, NE])
    trash_in = din("trash", [128, 1])
    rel_bias_in = din("rel_bias", [32, 4])
    W = {}
    for l in layers:
        W[l] = dict(
            w_ada=din("w_ada%d" % l, [D, 6 * D]), b_ada=din("b_ada%d" % l, [1, 6 * D]),
            n1g=din("n1g%d" % l, [128, 8]), n2g=din("n2g%d" % l, [128, 8]),
            w_in=din("w_in%d" % l, [D, D_IN]),
            w_up=din("w_up%d" % l, [2, 16, 256]), b_up=din("b_up%d" % l, [128, 4]),
            glag=din("glag%d" % l, [128, 1]), qng=din("qng%d" % l, [128, 1]),
            kng=din("kng%d" % l, [128, 1]), lamv=din("lamv%d" % l, [1, 256]),
            subg=din("subg%d" % l, [128, 1]),
            w_a=din("w_a%d" % l, [512, D]), w_b=din("w_b%d" % l, [512, D]),
            w_o=din("w_o%d" % l, [D, D]),
            w_r=din("w_r%d" % l, [D, 36]), b_r=din("b_r%d" % l, [1, 36]),
            n2grow=din("n2grow%d" % l, [1, D]),
            w1=din("w1_%d" % l, [NE, D, 2 * DEXP]), w2=din("w2_%d" % l, [NE, DEXP, D]),
        )
    x_out = nc.dram_tensor("x_out", [NLOC, D], F32, kind="ExternalOutput").ap()

    tv_d = dscr("tv_d", [12, TVW], F32)
    gq_d = dscr("gq_d", [2, 128, NLOC], BF16)
    gk_d = dscr("gk_d", [2, 128, SA], BF16)
    lg_d = dscr("lg_d", [2, 2, 128, SA], F32)
    gv_d = dscr("gv_d", [SA, 512], BF16)
    gg_d = dscr("gg_d", [4, 128, NLOC], BF16)
    dq_d = dscr("dq_d", [4, 128, NLOC], BF16)
    dk_d = dscr("dk_d", [4, 128, SA], BF16)
    dv_d = dscr("dv_d", [SA, 512], BF16)
    ga_d = dscr("ga_d", [8, 128, NLOC], BF16)
    gb_d = dscr("gb_d", [8, 128, NLOC], BF16)
    oa_d = dscr("oa_d", [4, 128, NLOC], BF16)
    ob_d = dscr("ob_d", [4, 128, NLOC], BF16)
    x1_d = dscr("x1_d", [NLOC, D], F32)
    h2_d = dscr("h2_d", [8, 128, NLOC], BF16)
    Xg = dscr("Xg", [NE * CAP + 128, D], BF16)
    Yg = dscr("Yg", [NE * CAP + 1, D], F32)
    rows_d = dscr("rows_d", [2, D], F32)
    xmid_c = [nc.dram_tensor("xmid_c%d" % j, [512, D], F32).ap() for j in range(NB)]
    xg_c = [nc.dram_tensor("xg_c%d" % j, [1024, D], F32).ap() for j in range(NB)]
    dbg_d = dscr("dbg_d", [128, 256], F32)
    dbg2_d = dscr("dbg2_d", [128, 8, 512], BF16)
    dbg3_d = dscr("dbg3_d", [128, 12], F32)
    dbg4_d = dscr("dbg4_d", [128, 4, D], F32)
    dbg_out = {}

    with ExitStack() as top:
        S = Sched(nc, top)

        _uid = [0]

        def sbuf(st, name, shape, dt):
            _uid[0] += 1
            return st.enter_context(nc.sbuf_tensor("s%d_%s" % (_uid[0], name), list(shape), dt))

        pbanks = [top.enter_context(nc.psum_tensor("pb%d" % i, [128, 512], F32)) for i in range(8)]
        PS = Ring(pbanks, "pb")

        def mm(out, lhsT, rhs, start, stop, reads, writes):
            S.op("pe", lambda: nc.tensor.matmul(out, lhsT, rhs, start=start, stop=stop, skip_group_check=True), reads, writes)

        def tr(out, in_, ident, reads, writes):
            S.op("pe", lambda: nc.tensor.transpose(out, in_, ident), reads, writes)

        def act(out, in_, func, reads, writes, bias=None, scale=None, accum=None):
            kw = {}
            if bias is not None:
                kw["bias"] = bias
            if scale is not None:
                kw["scale"] = scale
            if accum is not None:
                kw["accum_out"] = accum
            S.op("act", lambda: nc.scalar.activation(out=out, in_=in_, func=func, **kw), reads, writes)

        def ts(eng, out, in0, s1, s2, op0, op1, reads, writes):
            e = nc.vector if eng == "dve" else nc.gpsimd
            if op1 is None:
                S.op(eng, lambda: e.tensor_scalar(out, in0, s1, None, op0=op0), reads, writes)
            else:
                S.op(eng, lambda: e.tensor_scalar(out, in0, s1, s2, op0=op0, op1=op1), reads, writes)

        def tt(eng, out, in0, in1, op, reads, writes):
            e = nc.vector if eng == "dve" else nc.gpsimd
            S.op(eng, lambda: e.tensor_tensor(out, in0, in1, op), reads, writes)

        def stt(out, in0, scalar, in1, op0, op1, reads, writes):
            S.op("dve", lambda: nc.vector.scalar_tensor_tensor(out, in0, scalar, in1, op0=op0, op1=op1), reads, writes)

        def cp(eng, out, in_, reads, writes):
            if eng == "act":
                S.op("act", lambda: nc.scalar.copy(out, in_), reads, writes)
            else:
                e = nc.vector if eng == "dve" else nc.gpsimd
                S.op(eng, lambda: e.tensor_copy(out, in_), reads, writes)

        def ld(out, in_, reads, writes, q="sp"):
            e = {"sp": nc.sync, "pool": nc.gpsimd, "act": nc.scalar}[q]
            S.dma(q, lambda: e.dma_start(out=out, in_=in_), reads, writes)

        ident_f = sbuf(top, "ident_f", [128, 128], F32)
        ident_b = sbuf(top, "ident_b", [128, 128], BF16)
        jmat_f = sbuf(top, "jmat_f", [128, 128], F32)
        bones_b = sbuf(top, "bones_b", [128, 128], BF16)
        ones_f = sbuf(top, "ones_f", [128, 128], F32)
        tril_f = sbuf(top, "tril_f", [128, 256], F32)
        triu_f = sbuf(top, "triu_f", [128, 256], F32)
        flags = sbuf(top, "flags", [128, 2], F32)
        hm = sbuf(top, "hm", [128, 2], F32)
        eps_c = sbuf(top, "eps_c", [128, 1], F32)
        one_c = sbuf(top, "one_c", [128, 1], F32)
        zero_c = sbuf(top, "zero_c", [128, 1], F32)
        cact = sbuf(top, "cact", [128, 8], F32)
        relb = sbuf(top, "relb", [32, 4], F32)
        cb = sbuf(top, "cb", [128, 12], F32)
        gat = sbuf(top, "gat", [128, NT, 2], F32)
        gix = sbuf(top, "gix", [128, NT, 2], mybir.dt.int32)
        ecap = sbuf(top, "ecap", [128, NE], F32)
        trash = sbuf(top, "trash", [128, 1], F32)
        zrow = sbuf(top, "zrow", [1, D], F32)
        ld(ident_f[:], ident_in, [], ["ident_f"])
        ld(ident_b[:], ident_in, [], ["ident_b"], q="pool")
        ld(jmat_f[:], jmat_in, [], ["jmat_f"])
        ld(bones_b[:], bones_in, [], ["bones_b"], q="pool")
        ld(ones_f[:], ones_in, [], ["ones_f"])
        ld(tril_f[:], tril_in, [], ["tril_f"])
        ld(triu_f[:], triu_in, [], ["triu_f"])
        ld(flags[:], flags_in, [], ["flags"])
        ld(hm[:], hmask_in, [], ["hm"])
        ld(ecap[:], ecap_in, [], ["ecap"])
        ld(trash[:], trash_in, [], ["trash"])
        S.op("dve", lambda: nc.vector.memset(zrow[:], 0.0), [], ["zrow"])
        ld(Yg[NE * CAP:NE * CAP + 1, :], zrow[:], ["zrow"], ["Yg"])
        ld(cact[:], c_col, [], ["cact"])
        ld(relb[:], rel_bias_in, [], ["relb"])
        S.op("dve", lambda: nc.vector.memset(eps_c[:], RMS_EPS), [], ["eps_c"])
        S.op("dve", lambda: nc.vector.memset(one_c[:], 1.0), [], ["one_c"])
        S.op("dve", lambda: nc.vector.memset(zero_c[:], 0.0), [], ["zero_c"])
        act(cact[:], cact[:], AF.Silu, ["cact"], ["cact"])

        def build_gtab(st_outer):
            Gtab = sbuf(st_outer, "Gtab", [128, 12, GW], BF16)
            with ExitStack() as st:
                oh = sbuf(st, "oh", [32, 3 * TVW], F32)
                ohc = sbuf(st, "ohc", [32, 3 * 128], F32)
                tvs = sbuf(st, "tvs", [4, 3 * TVW], F32)
                Hk = sbuf(st, "Hk", [128, GW], F32)
                ld(oh[:], oh_in, [], ["oh"])
                ld(ohc[:], ohc_in, [], ["ohc"])
                for tab in range(3):
                    for c0 in range(0, TVW, 512):
                        w = min(512, TVW - c0)
                        p, pk = PS.next()
                        mm(p[0:4, 0:w], relb[:, :], oh[:, tab * TVW + c0: tab * TVW + c0 + w], True, True,
                           ["relb", "oh"], [pk])
                        cp("dve", tvs[:, tab * TVW + c0: tab * TVW + c0 + w], p[0:4, 0:w], [pk], ["tvs"])
                    ld(tv_d[tab * 4:(tab + 1) * 4, :], tvs[:, tab * TVW:(tab + 1) * TVW], ["tvs"], ["tv_d"])
                for k in range(3):
                    p, pk = PS.next()
                    mm(p[:, 0:4], ohc[:, k * 128:(k + 1) * 128], relb[:, :], True, True, ["ohc", "relb"], [pk])
                    cp("dve", cb[:, k * 4:(k + 1) * 4], p[:, 0:4], [pk], ["cb"])
                for th in range(12):
                    hank = bass.AP(tv_d.tensor, th * TVW, [[1, 128], [1, GW]])
                    ld(Hk[:], hank, ["tv_d"], ["Hk"])
                    for c0 in range(0, GW, 512):
                        w = min(512, GW - c0)
                        p, pk = PS.next()
                        mm(p[:, 0:w], jmat_f[:], Hk[:, c0:c0 + w], True, True, ["jmat_f", "Hk"], [pk])
                        cp("dve", Gtab[:, th, c0:c0 + w], p[:, 0:w], [pk], [("Gtab", th)])
            S.barrier()
            return Gtab

        try:
          for li, l in enumerate(layers):
            Wl = W[l]
            lam_init = 0.8 - 0.6 * math.exp(-0.3 * l)
            def x_loc_block(i, li=li):
                return x_loc_in[i * 512:(i + 1) * 512, :] if li == 0 else xmid_c[i]
            x_src_oth = x_oth_in
            fused_oth = li > 0
            is_last = li == len(layers) - 1
            with ExitStack() as lst:
                modcol = sbuf(lst, "modcol", [128, 48], F32)
                a1c = sbuf(lst, "a1c", [128, 8], F32)
                a2c = sbuf(lst, "a2c", [128, 8], F32)
                g1b = sbuf(lst, "g1b", [128, D], F32)
                g2b = sbuf(lst, "g2b", [128, D], F32)
                n1g = sbuf(lst, "n1g", [128, 8], F32)
                n2g = sbuf(lst, "n2g", [128, 8], F32)
                b_up = sbuf(lst, "b_up", [128, 4], F32)
                glag = sbuf(lst, "glag", [128, 1], F32)
                qng = sbuf(lst, "qng", [128, 1], F32)
                kng = sbuf(lst, "kng", [128, 1], F32)
                subg = sbuf(lst, "subg", [128, 1], F32)
                lamv = sbuf(lst, "lamv", [1, 256], F32)
                lamw = sbuf(lst, "lamw", [1, 8], F32)
                nlam2 = sbuf(lst, "nlam", [128, 2], F32)
                nlam = nlam2[:, 0:1]
                brb = sbuf(lst, "brb", [128, 36], F32)
                brow = sbuf(lst, "brow", [1, 36], F32)
                for nm, t_, src in (("n1g", n1g, Wl["n1g"]), ("n2g", n2g, Wl["n2g"]), ("b_up", b_up, Wl["b_up"]),
                                    ("glag", glag, Wl["glag"]), ("qng", qng, Wl["qng"]), ("kng", kng, Wl["kng"]),
                                    ("subg", subg, Wl["subg"]), ("lamv", lamv, Wl["lamv"]), ("brow", brow, Wl["b_r"])):
                    ld(t_[:], src, [], [nm])
                with ExitStack() as st:
                    modrow = sbuf(st, "modrow", [1, 6 * D], F32)
                    wada = [sbuf(st, "wada%d" % i, [128, 3 * D], F32) for i in range(2)]
                    bada = sbuf(st, "bada", [1, 6 * D], F32)
                    ld(bada[:], Wl["b_ada"], [], ["bada"])
                    macc = [PS.next() for _ in range(6)]
                    nld = 0
                    for half in range(2):
                        for k in range(8):
                            wt = wada[nld % 2]
                            wk = "wada%d" % (nld % 2)
                            nld += 1
                            ld(wt[:], Wl["w_ada"][k * 128:(k + 1) * 128, half * 3 * D:(half + 1) * 3 * D], [], [wk])
                            for j in range(6):
                                p, pk = macc[j]
                                mm(p[0:1, :], cact[:, k:k + 1], wt[:, j * 512:(j + 1) * 512], k == 0, k == 7,
                                   ["cact", wk], [pk])
                        for j in range(6):
                            p, pk = macc[j]
                            c0 = (half * 6 + j) * 512
                            tt("dve", modrow[0:1, c0:c0 + 512], p[0:1, :], bada[0:1, c0:c0 + 512], ALU.add,
                               [pk, "bada"], ["modrow"])
                    n2r = sbuf(st, "n2r", [1, D], F32)
                    arow = sbuf(st, "arow", [1, D], F32)
                    ld(n2r[:], Wl["n2grow"], [], ["n2r"])
                    stt(arow[:], modrow[0:1, 32 * 128:40 * 128], 1.0, n2r[:], ALU.add, ALU.mult, ["modrow", "n2r"], ["arow"])
                    ld(rows_d[0:1, :], arow[:], ["arow"], ["rows_d"])
                    ld(rows_d[1:2, :], modrow[0:1, 24 * 128:32 * 128], ["modrow"], ["rows_d"])
                    p, pk = PS.next()
                    for j in range(48):
                        mm(p[:, j:j + 1], modrow[0:1, j * 128:(j + 1) * 128], ones_f[0:1, 0:1], True, True,
                           ["modrow", "ones_f"], [pk])
                    cp("dve", modcol[:], p[:, 0:48], [pk], ["modcol"])
                    for gi, (gt_, gk_) in enumerate(((g1b, "g1b"), (g2b, "g2b"))):
                        base = (16 if gi == 0 else 40) * 128
                        for n in range(2):
                            p, pk = PS.next()
                            mm(p[:], ones_f[0:1, :], modrow[0:1, base + n * 512: base + (n + 1) * 512], True, True,
                               ["ones_f", "modrow"], [pk])
                            cp("dve", gt_[:, n * 512:(n + 1) * 512], p[:], [pk], [gk_])
                S.barrier()
                stt(a1c[:], modcol[:, 8:16], 1.0, n1g[:], ALU.add, ALU.mult, ["modcol", "n1g"], ["a1c"])
                stt(a2c[:], modcol[:, 32:40], 1.0, n2g[:], ALU.add, ALU.mult, ["modcol", "n2g"], ["a2c"])
                p, pk = PS.next()
                mm(p[:, 0:36], ones_f[0:1, :], brow[0:1, :], True, True, ["ones_f", "brow"], [pk])
                cp("dve", brb[:], p[:, 0:36], [pk], ["brb"])
                tt("dve", lamv[0:1, 0:64], lamv[0:1, 0:64], lamv[0:1, 64:128], ALU.mult, ["lamv"], ["lamv"])
                tt("dve", lamv[0:1, 128:192], lamv[0:1, 128:192], lamv[0:1, 192:256], ALU.mult, ["lamv"], ["lamv"])
                S.op("dve", lambda: nc.vector.reduce_sum(lamw[0:1, 0:1], lamv[0:1, 0:64], axis=AX.X), ["lamv"], ["lamw"])
                S.op("dve", lambda: nc.vector.reduce_sum(lamw[0:1, 1:2], lamv[0:1, 128:192], axis=AX.X), ["lamv"], ["lamw"])
                act(lamw[0:1, 2:4], lamw[0:1, 0:2], AF.Exp, ["lamw"], ["lamw"])
                stt(lamw[0:1, 4:5], lamw[0:1, 3:4], -lam_init, lamw[0:1, 2:3], ALU.add, ALU.subtract, ["lamw"], ["lamw"])
                p, pk = PS.next()
                mm(p[:, 0:1], ones_f[0:1, :], lamw[0:1, 4:5], True, True, ["ones_f", "lamw"], [pk])
                S.op("dve", lambda: nc.vector.memset(nlam2[:], 0.0), [], ["nlam"])
                cp("dve", nlam, p[:, 0:1], [pk], ["nlam"])
                qngs = sbuf(lst, "qngs", [128, 1], F32)
                subgs = sbuf(lst, "subgs", [128, 1], F32)
                ts("dve", qngs[:], qng[:], 0.125, None, ALU.mult, None, ["qng"], ["qngs"])
                ts("dve", subgs[:], subg[:], 1.0 - lam_init, None, ALU.mult, None, ["subg"], ["subgs"])

                def norm_block(xt, xk, ac, bc0, hT, hk, scr, hT32=None):
                    norm_pre(xt, xk, scr)
                    norm_post(xt, xk, ac, bc0, hT, hk, scr, hT32)

                def norm_pre(xt, xk, scr):
                    ss = scr["ss"]
                    for t in range(4):
                        act(scr["junk"][:], xt[:, t, :], AF.Square, [xk], ["junk"], accum=ss[:, t:t + 1])
                    act(ss[:, 4:8], ss[:, 0:4], AF.Ln, ["junk"], ["ssb"], bias=eps_c[:], scale=1.0 / D)
                    act(ss[:, 8:12], ss[:, 4:8], AF.Exp, ["ssb"], ["ssc"], scale=-0.5)
                    for t in range(4):
                        ts("pool" if t % 2 else "dve", xt[:, t, :], xt[:, t, :], ss[:, 8 + t:9 + t], None,
                           ALU.mult, None, [xk, "ssc"], [xk])

                def norm_post(xt, xk, ac, bc0, hT, hk, scr, hT32=None):
                    for k in range(8):
                        p, pk = PS.next()
                        for t in range(4):
                            tr(p[:, t * 128:(t + 1) * 128], xt[:, t, k * 128:(k + 1) * 128], ident_f[:],
                               [xk, "ident_f"], [pk])
                        dst = hT if hT32 is None else hT32
                        dkey = hk if hT32 is None else hk + "32"
                        if k % 2 == 0:
                            act(dst[:, k, :], p[:], AF.Identity, [pk, ac, "modcol"], [(dkey, k)],
                                bias=modcol[:, bc0 + k:bc0 + k + 1], scale=scr["ac"][:, k:k + 1])
                        else:
                            ts("dve", dst[:, k, :], p[:], scr["ac"][:, k:k + 1], modcol[:, bc0 + k:bc0 + k + 1],
                               ALU.mult, ALU.add, [pk, ac, "modcol"], [(dkey, k)])
                        if hT32 is not None and hT is not None:
                            cp("pool", hT[:, k, :], hT32[:, k, :], [(dkey, k)], [(hk, k)])

                print('n_instr before N1', S.n_instr)
                if dbg:
                    ld(dbg_d[:, 0:48], modcol[:], ["modcol"], ["dbg_d"])
                    ld(dbg_d[:, 48:56], a1c[:], ["a1c"], ["dbg_d"])
                    ld(dbg_d[:, 56:64], a2c[:], ["a2c"], ["dbg_d"])
                    ld(dbg_d[:, 64:66], nlam2[:, 0:2], ["nlam"], ["dbg_d"])
                    ld(dbg_d[:, 65:73], cact[:], ["cact"], ["dbg_d"])
                    ld(dbg_d[:, 80:144], g1b[:, 0:64], ["g1b"], ["dbg_d"])
                    ld(dbg_d[:, 144:208], g2b[:, 960:1024], ["g2b"], ["dbg_d"])
                    ld(dbg_d[:, 208:244], brb[:], ["brb"], ["dbg_d"])
                S.barrier()
                if phase_limit < 2:
                    S.stopped = True
                with ExitStack() as st:
                    w_in = sbuf(st, "w_in", [128, 8, D_IN], BF16)
                    for k in range(8):
                        ld(w_in[:, k, :], Wl["w_in"][k * 128:(k + 1) * 128, :], [], [("w_in", k)], q="pool")
                    wup = sbuf(st, "wup", [16, 2, 256], F32)
                    ld(wup[:], Wl["w_up"].rearrange("d r c -> r d c"), [], ["wup"])
                    nbup = sbuf(st, "nbup", [128, 4], F32)
                    ts("dve", nbup[:], b_up[:], -1.0, None, ALU.mult, None, ["b_up"], ["nbup"])
                    xts = [sbuf(st, "xt%d" % i, [128, 4, D], F32) for i in range(2)]
                    hTs = [sbuf(st, "hT%d" % i, [128, 8, 512], BF16) for i in range(2)]
                    scr = dict(ss=sbuf(st, "n_ss", [128, 12], F32), junk=sbuf(st, "n_junk", [128, D], BF16),
                               ac=a1c)
                    stg = Ring([sbuf(st, "stg%d" % i, [128, 512], BF16) for i in range(10)], "stg")
                    stf = Ring([sbuf(st, "stf%d" % i, [128, 512], F32) for i in range(6)], "stf")
                    lrT = [sbuf(st, "lrT%d" % i, [16, 512], F32) for i in range(2)]
                    blocks = [(0, i) for i in range(NB)] + [(1, i) for i in range(NB)]

                    xsel = [sbuf(st, "xsel%d" % i_, [128, D], F32) for i_ in range(2)] if fused_oth else None

                    def load_x(bi):
                        oth, i = blocks[bi]
                        if oth and fused_oth:
                            xt_ = xts[bi % 2]
                            xk_ = "xt%d" % (bi % 2)
                            ld(xt_[:], xg_c[i][0:512, :].rearrange("(t p) d -> p t d", p=128), ["xg"], [xk_])
                            for t in range(4):
                                r0 = 512 + t * 128
                                ld(xsel[t % 2][:], xg_c[i][r0:r0 + 128, :], ["xg"], ["xsel%d" % (t % 2)])
                                ts("pool", xt_[:, t, :], xt_[:, t, :], flags[:, 0:1], None, ALU.mult, None, [xk_, "flags"], [xk_])
                                stt(xt_[:, t, :], xsel[t % 2][:], flags[:, 1:2], xt_[:, t, :], ALU.mult, ALU.add,
                                    ["xsel%d" % (t % 2), "flags", xk_], [xk_])
                            return
                        src = x_src_oth[i * 512:(i + 1) * 512, :] if oth else x_loc_block(i)
                        ld(xts[bi % 2][:], src.rearrange("(t p) d -> p t d", p=128),
                           [("xmid", t_) for t_ in range(NT)] if (li > 0 and not oth) else [], ["xt%d" % (bi % 2)])

                    load_x(0)
                    norm_pre(xts[0], "xt0", scr)
                    norm_post(xts[0], "xt0", "a1c", 0, hTs[0], "hT0", scr)
                    for bi, (oth, i) in enumerate(blocks):
                        if bi + 1 < len(blocks):
                            load_x(bi + 1)
                        xt, xk = xts[bi % 2], "xt%d" % (bi % 2)
                        hT, hk = hTs[bi % 2], "hT%d" % (bi % 2)
                        nxt, nxk = xts[(bi + 1) % 2], "xt%d" % ((bi + 1) % 2)
                        nhT, nhk = hTs[(bi + 1) % 2], "hT%d" % ((bi + 1) % 2)
                        if dbg and bi == 0:
                            ld(dbg2_d, hT[:], [(hk, k) for k in range(8)], ["dbg2"])
                            ld(dbg3_d, scr["ss"][:], ["ssc"], ["dbg3"])
                            ld(dbg4_d, xt[:], [xk], ["dbg4"])
                        tok0 = (NLOC if oth else 0) + i * 512
                        hreads = [(hk, k) for k in range(8)]

                        def fm_tile(c0, m):
                            p, pk = PS.next()
                            for k in range(8):
                                mm(p[0:m, :], w_in[:, k, c0:c0 + m], hT[:, k, :], k == 0, k == 7,
                                   [("w_in", k), (hk, k)], [pk])
                            return p, pk

                        for pr in range(2):
                            p, pk = fm_tile(C_GK + pr * 128, 128)
                            s, sk = stg.next()
                            cp("act", s[:], p[:], [pk], [sk])
                            ld(gk_d[pr, :, tok0:tok0 + 512], s[:], [sk], ["gk_d"])
                            if not oth:
                                p, pk = fm_tile(C_GQ + pr * 128, 128)
                                s, sk = stg.next()
                                ts("dve", s[:], p[:], 0.125, None, ALU.mult, None, [pk], [sk])
                                ld(gq_d[pr, :, tok0:tok0 + 512], s[:], [sk], ["gq_d"])
                        for dr in range(2):
                            p, pk = fm_tile(C_LRF + dr * 16, 16)
                            cp("dve", lrT[dr][:], p[0:16, :], [pk], ["lrT%d" % dr])
                            for pr in range(2):
                                p, pk = PS.next()
                                mm(p[:], wup[:, dr, pr * 128:(pr + 1) * 128], lrT[dr][:], True, True,
                                   ["wup", "lrT%d" % dr], [pk])
                                s, sk = stf.next()
                                act(s[:], p[:], AF.Exp, [pk, "nbup"], [sk], scale=-1.0,
                                    bias=nbup[:, dr * 2 + pr: dr * 2 + pr + 1])
                                act(s[:], s[:], AF.Ln, [sk, "one_c"], [sk], bias=one_c[:])
                                ts("dve", s[:], s[:], -1.0 / 16.0, None, ALU.mult, None, [sk], [sk])
                                ld(lg_d[dr, pr, :, tok0:tok0 + 512], s[:], [sk], ["lg_d"])
                        for (c0, dst, dk_) in ((C_GV, gv_d, "gv_d"), (C_DV, dv_d, "dv_d")):
                            for t in range(4):
                                p, pk = PS.next()
                                for k in range(8):
                                    mm(p[:], hT[:, k, t * 128:(t + 1) * 128], w_in[:, k, c0:c0 + 512], k == 0, k == 7,
                                       [("w_in", k), (hk, k)], [pk])
                                s, sk = stg.next()
                                cp("act" if t % 2 else "dve", s[:], p[:], [pk], [sk])
                                ld(dst[tok0 + t * 128: tok0 + (t + 1) * 128, :], s[:], [sk], [dk_])
                        if bi + 1 < len(blocks):
                            norm_pre(nxt, nxk, scr)
                        qk_list = [("k", C_DK, dk_d, "dk_d", kng, "kng")]
                        if not oth:
                            qk_list.append(("q", C_DQ, dq_d, "dq_d", qngs, "qngs"))
                        for (nm, c0, dst, dkey, gcol, gkey) in qk_list:
                            for h in range(4):
                                p, pk = fm_tile(c0 + h * 128, 128)
                                s, sk = stg.next()
                                act(s[:], p[:], AF.Square, [pk], [sk])
                                p2, pk2 = PS.next()
                                mm(p2[:], bones_b[:], s[:], True, True, ["bones_b", sk], [pk2])
                                f, fk = stf.next()
                                act(f[:], p2[:], AF.Ln, [pk2, "eps_c"], [fk], bias=eps_c[:], scale=1.0 / 64.0)
                                act(f[:], f[:], AF.Exp, [fk], [fk], scale=-0.5)
                                s2, sk2 = stg.next()
                                stt(s2[:], p[:], gcol[:, 0:1], f[:], ALU.mult, ALU.mult, [pk, gkey, fk], [sk2])
                                ld(dst[h, :, tok0:tok0 + 512], s2[:], [sk2], [dkey])
                        if not oth:
                            for j in range(4):
                                p, pk = fm_tile(C_GG + j * 128, 128)
                                s, sk = stg.next()
                                act(s[:], p[:], AF.Silu, [pk], [sk])
                                ld(gg_d[j, :, tok0:tok0 + 512], s[:], [sk], ["gg_d"])
                            for (c0, dst, dkey) in ((C_GA, ga_d, "ga_d"), (C_GB, gb_d, "gb_d")):
                                for j in range(8):
                                    p, pk = fm_tile(c0 + j * 128, 128)
                                    s, sk = stg.next()
                                    act(s[:], p[:], AF.Sigmoid, [pk], [sk])
                                    ld(dst[j, :, tok0:tok0 + 512], s[:], [sk], [dkey])
                        if bi + 1 < len(blocks):
                            norm_post(nxt, nxk, "a1c", 0, nhT, nhk, scr)

                print('n_instr before GLA', S.n_instr)
                S.barrier()
                if phase_limit < 3:
                    S.stopped = True
                with ExitStack() as st:
                    kT = sbuf(st, "g_kT", [128, NLOC], BF16)
                    qT = sbuf(st, "g_qT", [128, NLOC], BF16)
                    lg = sbuf(st, "g_lg", [128, NLOC], F32)
                    cum = sbuf(st, "g_cum", [128, NLOC], F32)
                    kinv = sbuf(st, "g_kinv", [128, NLOC], BF16)
                    qdec = sbuf(st, "g_qdec", [128, NLOC], BF16)
                    qdM = [sbuf(st, "g_qdM%d" % i, [128, NLOC], BF16) for i in range(2)]
                    vv = sbuf(st, "g_v", [128, NT, 256], BF16)
                    rmask = sbuf(st, "g_rmask", [128, NLOC], F32)
                    ofw = sbuf(st, "g_of", [128, NT, 256], BF16)
                    ggT = sbuf(st, "g_gg", [128, 2, NLOC], BF16)
                    Sst = sbuf(st, "g_S", [128, 256], F32)
                    Sbf = sbuf(st, "g_Sbf", [128, 256], BF16)
                    tmpSr = Ring([sbuf(st, "g_tmpS%d" % i, [128, 256], F32) for i in range(4)], "g_tmpS")
                    kTt = Ring([sbuf(st, "g_kTt%d" % i, [128, 128], BF16) for i in range(4)], "g_kTt")
                    ATs = Ring([sbuf(st, "g_AT%d" % i, [128, 256], BF16) for i in range(4)], "g_AT")
                    osum = Ring([sbuf(st, "g_os%d" % i, [128, 256], F32) for i in range(4)], "g_os")
                    onr = Ring([sbuf(st, "g_on%d" % i, [128, 256], F32) for i in range(4)], "g_on")
                    gssr = Ring([sbuf(st, "g_ss%d" % i, [128, 8], F32) for i in range(4)], "g_ss")
                    gjunk = sbuf(st, "g_junk", [128, 128], BF16)
                    oaT = [sbuf(st, "g_oaT%d" % i, [128, 2, 512], BF16) for i in range(2)]
                    S.op("dve", lambda: nc.vector.memset(rmask[:], 1.0), [], ["rmask"])
                    rm3 = rmask[:].rearrange("p (c t) -> p c t", t=128)
                    S.op("dve", lambda: nc.vector.memset(rm3[:, :, 0:1], 0.0), ["rmask"], ["rmask"])
                    for pr in range(2):
                        ld(qT[:], gq_d[pr], ["gq_d"], ["g_qT"])
                        ld(ggT[:], gg_d[pr * 2:(pr + 1) * 2].rearrange("j p n -> p j n"), ["gg_d"], ["g_gg"])
                        for dr in range(2):
                            mask = tril_f if dr == 0 else triu_f
                            mkey = "tril_f" if dr == 0 else "triu_f"
                            dcol = 127 if dr == 0 else 0
                            S.op("dve", lambda: nc.vector.memset(Sst[:], 0.0), [], ["g_S"])
                            for is_oth in (True, False):
                                t0 = NLOC if is_oth else 0
                                ld(kT[:], gk_d[pr, :, t0:t0 + NLOC], ["gk_d"], ["g_kT"])
                                ld(vv[:], gv_d[t0:t0 + NLOC, pr * 256:(pr + 1) * 256].rearrange("(t p) c -> p t c", p=128),
                                   ["gv_d"], ["g_v"])
                                ld(lg[:], lg_d[dr, pr, :, t0:t0 + NLOC], ["lg_d"], ["g_lg"])
                                if dr == 0:
                                    S.op("dve", lambda: nc.vector.tensor_tensor_scan(
                                        out=cum[:], data0=rmask[:], data1=lg[:], initial=0.0, op0=ALU.mult, op1=ALU.add),
                                        ["rmask", "g_lg"], ["g_cum"])
                                else:
                                    S.op("dve", lambda: nc.vector.tensor_tensor_scan(
                                        out=cum[:, ::-1], data0=rmask[:], data1=lg[:, ::-1], initial=0.0,
                                        op0=ALU.mult, op1=ALU.add),
                                        ["rmask", "g_lg"], ["g_cum"])
                                act(lg[:], cum[:], AF.Exp, ["g_cum"], ["g_lg"], scale=-1.0)
                                tt("pool", kinv[:], kT[:], lg[:], ALU.mult, ["g_kT", "g_lg"], ["g_kinv"])
                                act(cum[:], cum[:], AF.Exp, ["g_cum"], ["g_cum"])
                                E = cum
                                if not is_oth:
                                    tt("dve", qdec[:], qT[:], E[:], ALU.mult, ["g_qT", "g_cum"], ["g_qdec"])
                                    for hh in range(2):
                                        ts("pool" if hh else "dve", qdM[hh][:], qdec[:], hm[:, hh:hh + 1], None, ALU.mult, None,
                                           ["g_qdec", "hm"], [("g_qdM", hh)])
                                    ts("dve", Sst[:], Sst[:], flags[:, dr:dr + 1], None, ALU.mult, None,
                                       ["g_S", "flags"], ["g_S"])
                                chunks = list(range(NT))
                                if dr == 1:
                                    chunks.reverse()
                                def g_p1(ci, c):
                                    cs = slice(c * 128, (c + 1) * 128)
                                    d = {}
                                    if not is_oth:
                                        pA, pAk = PS.next()
                                        for hh in range(2):
                                            mm(pA[:, hh * 128:(hh + 1) * 128], kinv[:, cs], qdM[hh][:, cs], True, True,
                                               ["g_kinv", ("g_qdM", hh)], [pAk])
                                        AT, ATk = ATs.next()
                                        tt("dve", AT[:], pA[:, 0:256], mask[:], ALU.mult, [pAk, mkey], [ATk])
                                        d["AT"] = (AT, ATk)
                                    last = (not is_oth) and ci == NT - 1
                                    if not last:
                                        pT, pTk = PS.next()
                                        pTb = pT[:].bitcast(BF16)
                                        tr(pTb[:, 0:128], kinv[:, cs], ident_b[:], ["g_kinv", "ident_b"], [pTk])
                                        kt_, ktk = kTt.next()
                                        cp("act", kt_[:], pTb[:, 0:128], [pTk], [ktk])
                                        d["kT"] = (kt_, ktk)
                                    return d

                                def g_p2(ci, c, d):
                                    if "kT" in d:
                                        kt_, ktk = d["kT"]
                                        pK, pKk = PS.next()
                                        mm(pK[:, 0:256], kt_[:], vv[:, c, :], True, True, [ktk, "g_v"], [pKk])
                                        dc = E[:, c * 128 + dcol: c * 128 + dcol + 1]
                                        tmpS, tmpSk = tmpSr.next()
                                        act(tmpS[:], pK[:, 0:256], AF.Identity, [pKk, "g_cum"], [tmpSk], scale=dc)
                                        d["tmpS"] = (tmpS, tmpSk, dc)

                                def g_state(ci, c, d):
                                    cs = slice(c * 128, (c + 1) * 128)
                                    if not is_oth:
                                        AT, ATk = d["AT"]
                                        cp("pool", Sbf[:], Sst[:], ["g_S"], ["g_Sbf"])
                                        pO, pOk = PS.next()
                                        for hh in range(2):
                                            hs = slice(hh * 128, (hh + 1) * 128)
                                            mm(pO[:, hs], AT[:, hs], vv[:, c, hs], True, False, [ATk, "g_v"], [pOk])
                                            mm(pO[:, hs], qdM[hh][:, cs], Sbf[:, hs], False, True, [("g_qdM", hh), "g_Sbf"], [pOk])
                                        d["pO"] = (pO, pOk)
                                    if "tmpS" in d:
                                        tmpS, tmpSk, dc = d["tmpS"]
                                        stt(Sst[:], Sst[:], dc, tmpS[:], ALU.mult, ALU.add, ["g_S", "g_cum", tmpSk], ["g_S"])

                                def g_x1(ci, c, d):
                                    if is_oth:
                                        return
                                    pO, pOk = d["pO"]
                                    if dr == 0:
                                        cp("act", ofw[:, c, :], pO[:, 0:256], [pOk], [("g_of", c)])
                                        return
                                    os_, osk = osum.next()
                                    gss, gsk = gssr.next()
                                    d["os"] = (os_, osk)
                                    d["gss"] = (gss, gsk)
                                    tt("dve", os_[:], pO[:, 0:256], ofw[:, c, :], ALU.add, [pOk, ("g_of", c)], [osk])
                                    for hh in range(2):
                                        act(gjunk[:], os_[:, hh * 128:(hh + 1) * 128], AF.Square, [osk], ["g_junk", (gsk, "a")],
                                            accum=gss[:, hh:hh + 1])
                                    act(gss[:, 2:4], gss[:, 0:2], AF.Ln, [(gsk, "a"), "eps_c"], [(gsk, "b")],
                                        bias=eps_c[:], scale=1.0 / 128.0)

                                def g_x2(ci, c, d):
                                    if is_oth or dr == 0:
                                        return
                                    os_, osk = d["os"]
                                    gss, gsk = d["gss"]
                                    act(gss[:, 4:6], gss[:, 2:4], AF.Exp, [(gsk, "b")], [(gsk, "c")], scale=-0.5)
                                    on_, onk = onr.next()
                                    d["on"] = (on_, onk)
                                    for hh in range(2):
                                        ts("pool", on_[:, hh * 128:(hh + 1) * 128], os_[:, hh * 128:(hh + 1) * 128],
                                           gss[:, 4 + hh:5 + hh], None, ALU.mult, None, [osk, (gsk, "c")], [(onk, hh)])

                                def g_x3(ci, c, d):
                                    if is_oth or dr == 0:
                                        return
                                    on_, onk = d["on"]
                                    blk, cc = c // 4, c % 4
                                    ob_, obk = oaT[blk % 2], "g_oaT%d" % (blk % 2)
                                    pX, pXk = PS.next()
                                    for hh in range(2):
                                        tr(pX[:, hh * 128:(hh + 1) * 128], on_[:, hh * 128:(hh + 1) * 128], ident_f[:],
                                           [(onk, hh), "ident_f"], [pXk])
                                    for hh in range(2):
                                        stt(ob_[:, hh, cc * 128:(cc + 1) * 128], pX[:, hh * 128:(hh + 1) * 128], glag[:, 0:1],
                                            ggT[:, hh, c * 128:(c + 1) * 128], ALU.mult, ALU.mult,
                                            [pXk, "glag", "g_gg"], [(obk, cc)])
                                    if cc == 0:
                                        ld(oa_d[pr * 2:(pr + 1) * 2, :, blk * 512:(blk + 1) * 512].rearrange("j p n -> p j n"),
                                           ob_[:], [(obk, q_) for q_ in range(4)], ["oa_d"])

                                ds = {}
                                ds[0] = g_p1(0, chunks[0])
                                if NT > 1:
                                    ds[1] = g_p1(1, chunks[1])
                                g_p2(0, chunks[0], ds[0])
                                for ci, c in enumerate(chunks):
                                    if ci + 2 < NT:
                                        ds[ci + 2] = g_p1(ci + 2, chunks[ci + 2])
                                    if ci + 1 < NT:
                                        g_p2(ci + 1, chunks[ci + 1], ds[ci + 1])
                                    g_state(ci, c, ds[ci])
                                    g_x1(ci, c, ds[ci])
                                    if ci >= 1:
                                        g_x2(ci - 1, chunks[ci - 1], ds[ci - 1])
                                    if ci >= 2:
                                        g_x3(ci - 2, chunks[ci - 2], ds[ci - 2])
                                        del ds[ci - 2]
                                g_x2(NT - 1, chunks[NT - 1], ds[NT - 1])
                                if NT >= 2:
                                    g_x3(NT - 2, chunks[NT - 2], ds[NT - 2])
                                g_x3(NT - 1, chunks[NT - 1], ds[NT - 1])

                print('n_instr before ATT', S.n_instr)
                S.barrier()
                if phase_limit < 4:
                    S.stopped = True
                with ExitStack() as st:
                    Gtab = build_gtab(st)
                    KT = [sbuf(st, "a_KT%d" % i, [128, SA], BF16) for i in range(2)]
                    QT = [sbuf(st, "a_QT%d" % i, [128, NLOC], BF16) for i in range(2)]
                    QTm = [[sbuf(st, "a_QTm%d_%d" % (i, c_), [128, NLOC], BF16) for c_ in range(2)] for i in range(2)]
                    VV = [sbuf(st, "a_V%d" % i, [128, NTA, 128], BF16) for i in range(2)]
                    PT = Ring([sbuf(st, "a_PT%d" % i, [128, 512], BF16) for i in range(10)], "a_PT")
                    dacc = [[sbuf(st, "a_dacc%d_%d" % (s_, e_), [128, 512], F32) for e_ in range(2)] for s_ in range(2)]
                    Rinv = Ring([sbuf(st, "a_R%d" % i, [128, 512], F32) for i in range(2)], "a_R")
                    o1 = sbuf(st, "a_o1", [128, 512], F32)
                    od = sbuf(st, "a_od", [128, 512], F32)
                    tq = sbuf(st, "a_tq", [128, 512], F32)
                    sq = sbuf(st, "a_sq", [128, 512], F32)
                    rr = sbuf(st, "a_rr", [128, 512], F32)
                    obT = Ring([sbuf(st, "a_obT%d" % i, [128, 512], BF16) for i in range(2)], "a_obT")

                    def load_head(h):
                        i = h % 2
                        ld(KT[i][:], dk_d[h], ["dk_d"], ["a_KT%d" % i])
                        ld(QT[i][:], dq_d[h], ["dq_d"], ["a_QT%d" % i])
                        for c_ in range(2):
                            ts("pool" if c_ else "dve", QTm[i][c_][:], QT[i][:], hm[:, c_:c_ + 1], None, ALU.mult, None,
                               ["a_QT%d" % i, "hm"], [("a_QTm", i, c_)])
                        ld(VV[i][:], dv_d[:, h * 128:(h + 1) * 128].rearrange("(t p) c -> p t c", p=128),
                           ["dv_d"], ["a_V%d" % i])

                    sps_i = [0]

                    def sps_next():
                        j = sps_i[0] % 4
                        sps_i[0] += 1
                        return pbanks[4 + j], "pb%d" % (4 + j)

                    aps_i = [0]

                    ones_b = sbuf(st, "a_ones_b", [128, 128], BF16)
                    cp("dve", ones_b[:], ones_f[:], ["ones_f"], ["a_ones_b"])

                    def stage1(h, i, qb, c, kt):
                        is_oth = kt >= NT
                        ktl = kt - NT if is_oth else kt
                        dlt = 128 * ktl - 512 * qb
                        tab = None
                        cbi = 0
                        if not is_oth:
                            if -128 <= dlt <= 512:
                                tab, dd = 0, dlt
                            else:
                                cbi = 0 if dlt < 0 else 1
                        else:
                            if dlt + NLOC == 512:
                                tab, dd = 1, 512
                            elif dlt - NLOC == -128:
                                tab, dd = 2, -128
                            else:
                                cbi = 2
                        p, pk = sps_next()
                        mm(p[:], KT[i][:, kt * 128:(kt + 1) * 128], QTm[i][c][:, qb * 512:(qb + 1) * 512], True, tab is None,
                           ["a_KT%d" % i, ("a_QTm", i, c)], [pk])
                        pt_, ptk = PT.next()
                        if tab is not None:
                            s0 = GC0 - dd
                            mm(p[:], ident_b[:], Gtab[:, tab * 4 + h, s0:s0 + 512], False, True,
                               ["ident_b", ("Gtab", tab * 4 + h)], [pk])
                            act(pt_[:], p[:], AF.Exp, [pk], [ptk])
                        else:
                            act(pt_[:], p[:], AF.Exp, [pk, "cb"], [ptk], bias=cb[:, cbi * 4 + h: cbi * 4 + h + 1])
                        return pt_, ptk

                    def stage2(h, i, qb, c, kt, pt_, ptk):
                        OT, OTk = pbanks[c], "pb%d" % c
                        mm(OT[:], VV[i][:, kt, :], pt_[:], kt == 0, kt == NTA - 1, ["a_V%d" % i, ptk], [OTk])
                        e_ = kt % 3
                        if e_ == 2:
                            pS, pSk = pbanks[2 + c], "pb%d" % (2 + c)
                            mm(pS[:], ones_b[:], pt_[:], kt == 2, False, ["a_ones_b", ptk], [pSk])
                        else:
                            dst, dk_ = dacc[c][e_], ("a_dacc", c, e_)
                            eng = "dve" if e_ == 0 else "pool"
                            if kt < 2:
                                cp(eng, dst[:], pt_[:], [ptk], [dk_])
                            else:
                                tt(eng, dst[:], dst[:], pt_[:], ALU.add, [dk_, ptk], [dk_])
                        if kt == NTA - 1:
                            finalize(h, qb, c)

                    def finalize(h, qb, c):
                        OT, OTk = pbanks[c], "pb%d" % c
                        pS, pSk = pbanks[2 + c], "pb%d" % (2 + c)
                        mm(pS[:], ones_f[:], dacc[c][0][:], False, False, ["ones_f", ("a_dacc", c, 0)], [pSk])
                        mm(pS[:], ones_f[:], dacc[c][1][:], False, True, ["ones_f", ("a_dacc", c, 1)], [pSk])
                        R, Rk = Rinv.next()
                        S.op("dve", lambda: nc.vector.reciprocal(R[:], pS[:]), [pSk], [Rk])
                        if c == 0:
                            tt("dve", o1[:], OT[:], R[:], ALU.mult, [OTk, Rk], ["a_o1"])
                        else:
                            stt(tq[:], OT[:], nlam[:, 0:1], R[:], ALU.mult, ALU.mult, [OTk, "nlam", Rk], ["a_tq"])
                            tt("pool", od[:], tq[:], o1[:], ALU.add, ["a_tq", "a_o1"], ["a_od"])
                            tt("pool", sq[:], od[:], od[:], ALU.mult, ["a_od"], ["a_sq"])
                            pQ, pQk = sps_next()
                            mm(pQ[:], ones_f[:], sq[:], True, True, ["ones_f", "a_sq"], [pQk])
                            act(rr[:], pQ[:], AF.Ln, [pQk, "eps_c"], ["a_rr"], bias=eps_c[:], scale=1.0 / 128.0)
                            act(rr[:], rr[:], AF.Exp, ["a_rr"], ["a_rr"], scale=-0.5)
                            ob_, obk = obT.next()
                            stt(ob_[:], od[:], subgs[:, 0:1], rr[:], ALU.mult, ALU.mult, ["a_od", "subgs", "a_rr"], [obk])
                            ld(ob_d[h, :, qb * 512:(qb + 1) * 512], ob_[:], [obk], ["ob_d"])

                    LOOK = 2
                    load_head(0)
                    for h in range(4):
                        if h + 1 < 4:
                            load_head(h + 1)
                        i = h % 2
                        pend = []
                        for qb in range(NB):
                            for c in range(2):
                                for kt in range(NTA):
                                    pend.append((qb, c, kt) + stage1(h, i, qb, c, kt))
                                    if len(pend) > LOOK:
                                        qb_, c_, kt_, pt_, ptk = pend.pop(0)
                                        stage2(h, i, qb_, c_, kt_, pt_, ptk)
                        while pend:
                            qb_, c_, kt_, pt_, ptk = pend.pop(0)
                            stage2(h, i, qb_, c_, kt_, pt_, ptk)

                print('n_instr before MERGE', S.n_instr)
                S.barrier()
                if phase_limit < 5:
                    S.stopped = True
                with ExitStack() as st:
                    w_a = sbuf(st, "m_wa", [128, 4, D], BF16)
                    w_b = sbuf(st, "m_wb", [128, 4, D], BF16)
                    w_o = sbuf(st, "m_wo", [128, 8, D], BF16)
                    w_r = sbuf(st, "m_wr", [128, 8, 36], F32)
                    ld(w_a[:], Wl["w_a"].rearrange("(k p) n -> p k n", p=128), [], ["m_wa"], q="pool")
                    ld(w_b[:], Wl["w_b"].rearrange("(k p) n -> p k n", p=128), [], ["m_wb"], q="pool")
                    ld(w_o[:], Wl["w_o"].rearrange("(k p) n -> p k n", p=128), [], ["m_wo"], q="pool")
                    ld(w_r[:], Wl["w_r"].rearrange("(k p) n -> p k n", p=128), [], ["m_wr"])
                    oaB = sbuf(st, "m_oa", [128, 4, 512], BF16)
                    obB = sbuf(st, "m_ob", [128, 4, 512], BF16)
                    gaB = sbuf(st, "m_ga", [128, 8, 512], BF16)
                    gbB = sbuf(st, "m_gb", [128, 8, 512], BF16)
                    xB = sbuf(st, "m_x", [128, 4, D], F32)
                    x1B = sbuf(st, "m_x1", [128, 4, D], F32)
                    mg = sbuf(st, "m_mg", [128, 8, 512], BF16)
                    t1 = Ring([sbuf(st, "m_t1%d" % i, [128, 512], F32) for i in range(2)], "m_t1")
                    t2 = Ring([sbuf(st, "m_t2%d" % i, [128, 512], F32) for i in range(2)], "m_t2")
                    a2b = sbuf(st, "m_a2b", [128, D], F32)
                    sh2b = sbuf(st, "m_sh2b", [128, D], F32)
                    ld(a2b[:], bass.AP(rows_d.tensor, 0, [[0, 128], [1, D]]), ["rows_d"], ["m_a2b"])
                    ld(sh2b[:], bass.AP(rows_d.tensor, D, [[0, 128], [1, D]]), ["rows_d"], ["m_sh2b"])
                    htmp = Ring([sbuf(st, "m_htmp%d" % i_, [128, D], F32) for i_ in range(2)], "m_htmp")
                    h2tok = [sbuf(st, "m_h2tok%d" % i_, [128, D], BF16) for i_ in range(4)]
                    rbase = sbuf(st, "r_base", [128, NE], F32)
                    S.op("dve", lambda: nc.vector.memset(rbase[:], 0.0), [], ["r_base"])
                    rMT = [sbuf(st, "r_M%d" % t_, [128, 3, NE], F32) for t_ in range(4)]
                    rrkT = [sbuf(st, "r_rk%d" % t_, [128, 5, NE], F32) for t_ in range(4)]
                    rslT = [sbuf(st, "r_sl%d" % t_, [128, 8], F32) for t_ in range(4)]
                    rbt = sbuf(st, "r_bt", [128, 4, NE], F32)
                    sidx = Ring([sbuf(st, "r_sidx%d" % i_, [128, 2], mybir.dt.int32) for i_ in range(4)], "r_sidx")
                    h2T32 = sbuf(st, "m_h2T32", [128, 8, 512], F32)
                    scr2 = dict(ss=sbuf(st, "m_ss", [128, 12], F32), junk=sbuf(st, "m_junk", [128, D], BF16),
                                ac=a2c)
                    rlT = [sbuf(st, "r_l%d" % t_, [128, 36], F32) for t_ in range(4)]
                    rwT = [sbuf(st, "r_w%d" % t_, [128, 64], F32) for t_ in range(4)]
                    rmlT = [sbuf(st, "r_ml%d" % t_, [128, 32], F32) for t_ in range(4)]
                    rm8T = [sbuf(st, "r_m8%d" % t_, [128, 8], F32) for t_ in range(4)]
                    rjunkT = [sbuf(st, "r_junk%d" % t_, [128, 4], F32) for t_ in range(4)]

                    def lockstep(gens):
                        gens = list(gens)
                        while gens:
                            for g_ in list(gens):
                                try:
                                    next(g_)
                                except StopIteration:
                                    gens.remove(g_)
                    for i in range(NB):
                        bs = slice(i * 512, (i + 1) * 512)
                        ld(oaB[:], oa_d[:, :, bs].rearrange("j p n -> p j n"), ["oa_d"], ["m_oa"])
                        ld(obB[:], ob_d[:, :, bs].rearrange("j p n -> p j n"), ["ob_d"], ["m_ob"])
                        ld(gaB[:], ga_d[:, :, bs].rearrange("j p n -> p j n"), ["ga_d"], ["m_ga"])
                        ld(gbB[:], gb_d[:, :, bs].rearrange("j p n -> p j n"), ["gb_d"], ["m_gb"])
                        ld(xB[:], x_loc_block(i).rearrange("(t p) d -> p t d", p=128),
                           [("xmid", t_) for t_ in range(NT)] if li > 0 else [], ["m_x"])
                        for m in range(8):
                            pa, pak = PS.next()
                            for k in range(4):
                                mm(pa[:], w_a[:, k, m * 128:(m + 1) * 128], oaB[:, k, :], k == 0, k == 3, ["m_wa", "m_oa"], [pak])
                            pb_, pbk = PS.next()
                            for k in range(4):
                                mm(pb_[:], w_b[:, k, m * 128:(m + 1) * 128], obB[:, k, :], k == 0, k == 3, ["m_wb", "m_ob"], [pbk])
                            ta, tak = t1.next()
                            tb, tbk = t2.next()
                            tt("dve", ta[:], pa[:], gaB[:, m, :], ALU.mult, [pak, "m_ga"], [tak])
                            tt("dve", tb[:], pb_[:], gbB[:, m, :], ALU.mult, [pbk, "m_gb"], [tbk])
                            tt("pool", mg[:, m, :], ta[:], tb[:], ALU.add, [tak, tbk], [("m_mg", m)])
                        for t in range(4):
                            for n in range(2):
                                p, pk = PS.next()
                                for k in range(8):
                                    mm(p[:], mg[:, k, t * 128:(t + 1) * 128], w_o[:, k, n * 512:(n + 1) * 512], k == 0, k == 7,
                                       [("m_mg", k), "m_wo"], [pk])
                                ta, tak = t1.next()
                                tt("dve", ta[:], p[:], g1b[:, n * 512:(n + 1) * 512], ALU.mult, [pk, "g1b"], [tak])
                                tt("pool", x1B[:, t, n * 512:(n + 1) * 512], ta[:], xB[:, t, n * 512:(n + 1) * 512], ALU.add,
                                   [tak, "m_x"], ["m_x1"])
                        ld(x1_d[bs, :].rearrange("(t p) d -> p t d", p=128), x1B[:], ["m_x1"], ["x1_d"])
                        norm_block(x1B, "m_x1", "a2c", 24, None, "m_h2T", scr2, hT32=h2T32)
                        for t in range(4):
                            hm_, hmk = htmp.next()
                            tt("dve", hm_[:], x1B[:, t, :], a2b[:], ALU.mult, ["m_x1", "m_a2b"], [hmk])
                            tt("pool", h2tok[t][:], hm_[:], sh2b[:], ALU.add, [hmk, "m_sh2b"], [("m_h2tok", t)])
                        pIs = [None] * 4

                        def rt_A(t):
                            tg = i * 4 + t
                            rl, rw, rml, rm8, rjunk, rM = rlT[t], rwT[t], rmlT[t], rm8T[t], rjunkT[t], rMT[t]
                            K = lambda n: (n, t)
                            p, pk = PS.next()
                            for k in range(8):
                                mm(p[:, 0:36], h2T32[:, k, t * 128:(t + 1) * 128], w_r[:, k, :], k == 0, k == 7,
                                   [("m_h2T32", k), "m_wr"], [pk])
                            yield
                            tt("dve", rl[:], p[:, 0:36], brb[:], ALU.add, [pk, "brb"], [K("r_l")])
                            yield
                            S.op("dve", lambda: nc.vector.reduce_max(rw[:, 0:1], rl[:, 0:4], axis=AX.X), [K("r_l")], [K("r_w0")])
                            yield
                            ts("dve", rw[:, 1:2], rw[:, 0:1], -1.0, None, ALU.mult, None, [K("r_w0")], [K("r_w1")])
                            yield
                            act(rjunk[:], rl[:, 0:4], AF.Exp, [K("r_l"), K("r_w1")], [K("r_junk")], bias=rw[:, 1:2], accum=rw[:, 2:3])
                            yield
                            S.op("dve", lambda: nc.vector.reciprocal(rw[:, 3:4], rw[:, 2:3]), [K("r_junk")], [K("r_w3")])
                            yield
                            ts("dve", rw[:, 4:8], rl[:, 0:4], rw[:, 0:1], None, ALU.is_ge, None, [K("r_l"), K("r_w0")], [K("r_w4")])
                            yield
                            ts("dve", rw[:, 8:12], rw[:, 4:8], -1.0, 1e30, ALU.add, ALU.mult, [K("r_w4")], [K("r_w8")])
                            yield
                            for g in range(4):
                                ts("dve", rml[:, g * 8:(g + 1) * 8], rl[:, 4 + g * 8: 4 + (g + 1) * 8], rw[:, 8 + g: 9 + g], None,
                                   ALU.add, None, [K("r_l"), K("r_w8")], [K("r_ml")])
                                yield
                            S.op("dve", lambda: nc.vector.max(out=rm8[:], in_=rml[:]), [K("r_ml")], [K("r_m8")])
                            yield
                            tt("dve", rw[:, 12:13], rm8[:, 1:2], rm8[:, 0:1], ALU.subtract, [K("r_m8")], [K("r_w12")])
                            yield
                            act(rw[:, 13:14], rw[:, 12:13], AF.Exp, [K("r_w12")], [K("r_w13")])
                            yield
                            ts("dve", rw[:, 14:15], rw[:, 13:14], 1.0, None, ALU.add, None, [K("r_w13")], [K("r_w14")])
                            yield
                            S.op("dve", lambda: nc.vector.reciprocal(rw[:, 15:16], rw[:, 14:15]), [K("r_w14")], [K("r_w15")])
                            yield
                            tt("dve", rw[:, 16:17], rw[:, 15:16], rw[:, 3:4], ALU.mult, [K("r_w15"), K("r_w3")], [K("r_w16")])
                            yield
                            tt("dve", rw[:, 17:18], rw[:, 16:17], rw[:, 13:14], ALU.mult, [K("r_w16"), K("r_w13")], [K("r_w17")])
                            yield
                            cp("dve", gat[:, tg, 0:2], rw[:, 16:18], [K("r_w16"), K("r_w17")], [("gat", tg)])
                            yield
                            ts("dve", rM[:, 0, :], rml[:], rm8[:, 0:1], None, ALU.is_equal, None, [K("r_ml"), K("r_m8")], [K("r_M0")])
                            yield
                            ts("dve", rM[:, 1, :], rml[:], rm8[:, 1:2], None, ALU.is_equal, None, [K("r_ml"), K("r_m8")], [K("r_M1")])
                            yield
                            tt("dve", rM[:, 2, :], rM[:, 0, :], rM[:, 1, :], ALU.add, [K("r_M0"), K("r_M1")], [K("r_M2")])
                            yield
                            pI, pIk = PS.next()
                            mm(pI[:, 0:NE], tril_f[:, 0:128], rM[:, 2, :], True, True, ["tril_f", K("r_M2")], [pIk])
                            mm(pI[:, NE:2 * NE], ones_f[:], rM[:, 2, :], True, True, ["ones_f", K("r_M2")], [pIk])
                            pIs[t] = (pI, pIk)
                            yield

                        def rt_B(t):
                            tg = i * 4 + t
                            rM, rrk, rsl = rMT[t], rrkT[t], rslT[t]
                            K = lambda n: (n, t)
                            pI, pIk = pIs[t]
                            tt("dve", rrk[:, 0, :], pI[:, 0:NE], rbt[:, t, :], ALU.add, [pIk, ("r_bt", t)], [K("r_rk0")])
                            yield
                            tt("dve", rrk[:, 0, :], rrk[:, 0, :], rM[:, 2, :], ALU.subtract, [K("r_rk0"), K("r_M2")], [K("r_rk0")])
                            yield
                            ts("dve", rrk[:, 1, :], rrk[:, 0, :], float(CAP), None, ALU.is_lt, None, [K("r_rk0")], [K("r_rk1")])
                            yield
                            tt("dve", rrk[:, 2, :], rrk[:, 0, :], ecap[:], ALU.add, [K("r_rk0"), "ecap"], [K("r_rk2")])
                            yield
                            for k_ in range(2):
                                tt("dve", rrk[:, 3, :], rrk[:, 2, :], rM[:, k_, :], ALU.mult, [K("r_rk2"), K("r_M%d" % k_)], [K("r_rk3")])
                                yield
                                S.op("dve", lambda k_=k_: nc.vector.reduce_sum(rsl[:, k_:k_ + 1], rrk[:, 3, :], axis=AX.X),
                                     [K("r_rk3")], [K("r_sl")])
                                yield
                                tt("dve", rrk[:, 4, :], rrk[:, 1, :], rM[:, k_, :], ALU.mult, [K("r_rk1"), K("r_M%d" % k_)], [K("r_rk4")])
                                yield
                                S.op("dve", lambda k_=k_: nc.vector.reduce_sum(rsl[:, 2 + k_:3 + k_], rrk[:, 4, :], axis=AX.X),
                                     [K("r_rk4")], [K("r_sl")])
                                yield
                            ts("dve", rsl[:, 4:6], rsl[:, 0:2], trash[:, 0:1], None, ALU.subtract, None, [K("r_sl"), "trash"], [K("r_sl")])
                            yield
                            tt("dve", rsl[:, 4:6], rsl[:, 4:6], rsl[:, 2:4], ALU.mult, [K("r_sl")], [K("r_sl")])
                            yield
                            ts("dve", rsl[:, 4:6], rsl[:, 4:6], trash[:, 0:1], None, ALU.add, None, [K("r_sl"), "trash"], [K("r_sl")])
                            yield
                            ts("dve", rsl[:, 6:8], rsl[:, 0:2], -float(NE * CAP), None, ALU.add, None, [K("r_sl")], [K("r_sl")])
                            yield
                            tt("dve", rsl[:, 6:8], rsl[:, 6:8], rsl[:, 2:4], ALU.mult, [K("r_sl")], [K("r_sl")])
                            yield
                            ts("dve", rsl[:, 6:8], rsl[:, 6:8], float(NE * CAP), None, ALU.add, None, [K("r_sl")], [K("r_sl")])
                            yield
                            si_, sik = sidx.next()
                            cp("dve", si_[:], rsl[:, 4:6], [K("r_sl")], [sik])
                            yield
                            cp("dve", gix[:, tg, :], rsl[:, 6:8], [K("r_sl")], [("gix", tg)])
                            yield
                            for k_ in range(2):
                                S.dma("pool", (lambda k_=k_, si_=si_: nc.gpsimd.indirect_dma_start(
                                    out=Xg[:, :], out_offset=bass.IndirectOffsetOnAxis(ap=si_[:, k_:k_ + 1], axis=0),
                                    in_=h2tok[t][:, :], in_offset=None)),
                                    [("m_h2tok", t), sik], [("Xg", tg, k_)])
                            yield

                        lockstep(rt_A(t) for t in range(4))
                        cp("dve", rbt[:, 0, :], rbase[:], ["r_base"], [("r_bt", 0)])
                        for t in range(1, 4):
                            tt("dve", rbt[:, t, :], rbt[:, t - 1, :], pIs[t - 1][0][:, NE:2 * NE], ALU.add,
                               [("r_bt", t - 1), pIs[t - 1][1]], [("r_bt", t)])
                        tt("dve", rbase[:], rbt[:, 3, :], pIs[3][0][:, NE:2 * NE], ALU.add, [("r_bt", 3), pIs[3][1]], ["r_base"])
                        lockstep(rt_B(t) for t in range(4))

                print('n_instr before MOE', S.n_instr)
                S.barrier()
                if phase_limit < 6:
                    S.stopped = True
                with ExitStack() as st:
                    CS = CAP // 128
                    CB = CAP // 512
                    w1s = [sbuf(st, "e_w1%d" % i, [128, 8, 2 * DEXP], BF16) for i in range(2)]
                    w2s = [sbuf(st, "e_w2%d" % i, [128, 4, D], BF16) for i in range(2)]
                    xtok = [sbuf(st, "e_xtok%d" % i, [128, 4, D], BF16) for i in range(2)]
                    xT = [sbuf(st, "e_xT%d" % i, [128, 8, 512], BF16) for i in range(2)]
                    sg = Ring([sbuf(st, "e_sg%d" % i, [128, 512], BF16) for i in range(3)], "e_sg")
                    actT = [sbuf(st, "e_act%d" % i, [128, 4, 512], BF16) for i in range(2)]
                    ybuf = Ring([sbuf(st, "e_y%d" % i, [128, D], F32) for i in range(3)], "e_y")
                    xc = Ring([sbuf(st, "e_xc%d" % i, [128, D], F32) for i in range(4)], "e_xc")
                    y1r = Ring([sbuf(st, "e_y1%d" % i, [128, D], F32) for i in range(4)], "e_y1")
                    y2r = Ring([sbuf(st, "e_y2%d" % i, [128, D], F32) for i in range(4)], "e_y2")
                    xg_keys = [("Xg", tg_, k_) for tg_ in range(NT) for k_ in range(2)]
                    yg_keys = []

                    def load_w(e):
                        i = e % 2
                        ld(w1s[i][:], Wl["w1"][e].rearrange("(k p) n -> p k n", p=128), [], ["e_w1%d" % i], q="pool")
                        ld(w2s[i][:], Wl["w2"][e].rearrange("(k p) n -> p k n", p=128), [], ["e_w2%d" % i], q="pool")
                        for k in range(4):
                            tt("pool", w2s[i][:, k, :], w2s[i][:, k, :], g2b[:], ALU.mult, ["e_w2%d" % i, "g2b"], ["e_w2%d" % i])

                    nblk = 0

                    def load_xtok(e, blk):
                        j = (e * CB + blk) % 2
                        r0 = e * CAP + blk * 512
                        ld(xtok[j][:], Xg[r0:r0 + 512, :].rearrange("(s p) d -> p s d", p=128), xg_keys, ["e_xtok%d" % j])

                    load_w(0)
                    load_xtok(0, 0)
                    for e in range(NE):
                        if e + 1 < NE:
                            load_w(e + 1)
                        i = e % 2
                        w1, w1k, w2, w2k = w1s[i], "e_w1%d" % i, w2s[i], "e_w2%d" % i
                        for blk in range(CB):
                            j_ = nblk % 2
                            nblk += 1
                            if blk + 1 < CB:
                                load_xtok(e, blk + 1)
                            elif e + 1 < NE:
                                load_xtok(e + 1, 0)
                            xt_, xtk = xtok[j_], "e_xtok%d" % j_
                            xT_, xTk = xT[j_], "e_xT%d" % j_
                            for k in range(8):
                                pT, pTk = PS.next()
                                pTb = pT[:].bitcast(BF16)
                                for s_i in range(4):
                                    tr(pTb[:, s_i * 128:(s_i + 1) * 128], xt_[:, s_i, k * 128:(k + 1) * 128], ident_b[:],
                                       [xtk, "ident_b"], [pTk])
                                cp("dve" if k % 2 else "act", xT_[:, k, :], pTb[:, 0:512], [pTk], [(xTk, k)])
                            at, atk = actT[j_], "e_act%d" % j_
                            for j in range(4):
                                pg, pgk = PS.next()
                                for k in range(8):
                                    mm(pg[:], w1[:, k, j * 128:(j + 1) * 128], xT_[:, k, :], k == 0, k == 7, [w1k, (xTk, k)], [pgk])
                                pu, puk = PS.next()
                                for k in range(8):
                                    mm(pu[:], w1[:, k, DEXP + j * 128: DEXP + (j + 1) * 128], xT_[:, k, :], k == 0, k == 7,
                                       [w1k, (xTk, k)], [puk])
                                s_, sk = sg.next()
                                act(s_[:], pg[:], AF.Silu, [pgk], [sk])
                                tt("dve", at[:, j, :], pu[:], s_[:], ALU.mult, [puk, sk], [(atk, j)])
                            for t in range(4):
                                yb, ybk = ybuf.next()
                                for n in range(2):
                                    py, pyk = PS.next()
                                    for j in range(4):
                                        mm(py[:], at[:, j, t * 128:(t + 1) * 128], w2[:, j, n * 512:(n + 1) * 512], j == 0, j == 3,
                                           [(atk, j), w2k], [pyk])
                                    cp("act" if n else "dve", yb[:, n * 512:(n + 1) * 512], py[:], [pyk], [(ybk, n)])
                                r0 = e * CAP + blk * 512 + t * 128
                                yk = ("Yg", e, blk, t)
                                yg_keys.append(yk)
                                ld(Yg[r0:r0 + 128, :], yb[:], [(ybk, 0), (ybk, 1)], [yk])
                    def emit_cc(j):
                        rg = [[2 * g_, 2 * g_ + 1] for g_ in range(NCORES // 2)]
                        S.cc(lambda: nc.gpsimd.collective_compute(
                            "AllGather", ALU.bypass, replica_groups=rg, ins=[xmid_c[j].opt()], outs=[xg_c[j].opt()]),
                            [("xmid", t_) for t_ in range(4 * j, 4 * j + 4)], ["xg"])

                    for tg in range(NT):
                        x_, xk_ = xc.next()
                        ld(x_[:], x1_d[tg * 128:(tg + 1) * 128, :], ["x1_d"], [xk_])
                        ya, yak = y1r.next()
                        yb2, ybk2 = y2r.next()
                        for (yt_, ytk, k_) in ((ya, yak, 0), (yb2, ybk2, 1)):
                            S.dma("pool", (lambda yt_=yt_, k_=k_: nc.gpsimd.indirect_dma_start(
                                out=yt_[:, :], out_offset=None, in_=Yg[:, :],
                                in_offset=bass.IndirectOffsetOnAxis(ap=gix[:, tg, k_:k_ + 1], axis=0))),
                                yg_keys + ["Yg", ("gix", tg)], [ytk])
                        stt(x_[:], ya[:], gat[:, tg, 0:1], x_[:], ALU.mult, ALU.add, [yak, ("gat", tg), xk_], [xk_])
                        stt(x_[:], yb2[:], gat[:, tg, 1:2], x_[:], ALU.mult, ALU.add, [ybk2, ("gat", tg), xk_], [xk_])
                        if is_last:
                            ld(x_out[tg * 128:(tg + 1) * 128, :], x_[:], [xk_], [("xout", tg)])
                        else:
                            ld(xmid_c[tg // 4][(tg % 4) * 128:(tg % 4 + 1) * 128, :], x_[:], [xk_], [("xmid", tg)])
                            if tg % 4 == 3 and tg >= 7:
                                emit_cc(tg // 4 - 1)
                    if not is_last:
                        emit_cc(NB - 1)
        except _Stop:
            pass
        S.finish("sp")
    return nc, S


def _t5_bucket_np(rel):
    import jax
    import jax.numpy as jnp
    cpu = jax.devices("cpu")[0]
    with jax.default_device(cpu):
        rel = jnp.asarray(rel, dtype=jnp.int32)
        nb = 16
        max_exact = 8
        ret = jnp.where(rel > 0, nb, 0)
        n = jnp.abs(rel)
        nf = jnp.maximum(n, 1).astype(jnp.float32)
        large = max_exact + (jnp.log(nf / max_exact) / math.log(128 / max_exact) * (nb - max_exact)).astype(jnp.int32)
        large = jnp.minimum(large, nb - 1)
        out = ret + jnp.where(n < max_exact, n, large)
        return np.asarray(out)


def _onehot(b):
    oh = np.zeros((32, b.shape[0]), np.float32)
    oh[b, np.arange(b.shape[0])] = 1.0
    return oh


def _core_consts(hf, NLOC):
    shift = NLOC if hf == 0 else -NLOC
    n = np.arange(TVW)
    base = GC0 + 127 - n
    tabs = [base, base - NLOC + shift, base + NLOC + shift]
    oh = np.concatenate([_onehot(_t5_bucket_np(t)) for t in tabs], axis=1)
    cvals = _t5_bucket_np(np.array([-100000, 100000, shift]))
    ohc = np.concatenate([np.repeat(_onehot(cvals[k:k + 1]), 128, axis=1) for k in range(3)], axis=1)
    flags = np.zeros((128, 2), np.float32)
    flags[:, 0] = 1.0 if hf == 1 else 0.0
    flags[:, 1] = 1.0 if hf == 0 else 0.0
    return oh, ohc, flags


def _col(v, k):
    return np.ascontiguousarray(np.asarray(v, np.float32).reshape(k, 128).T)


def make_in_maps(inp, SEQ, layers, x_override=None):
    NLOC = SEQ // 2
    B = inp["x"].shape[0]
    f = lambda a: np.ascontiguousarray(np.asarray(a, np.float32))
    ident = np.eye(128, dtype=np.float32)
    jmat = np.ascontiguousarray(ident[::-1])
    bones = np.zeros((128, 128), np.float32)
    bones[:64, :64] = 1.0
    bones[64:, 64:] = 1.0
    jj, ii = np.meshgrid(np.arange(128), np.arange(128), indexing="ij")
    tril = (jj <= ii).astype(np.float32)
    triu = (jj > ii).astype(np.float32)
    hmask = np.zeros((128, 2), np.float32)
    hmask[:64, 0] = 1.0
    hmask[64:, 1] = 1.0
    ecap = np.tile((np.arange(NE, dtype=np.float32) * CAP)[None, :], (128, 1)).astype(np.float32)
    trash = (NE * CAP + np.arange(128, dtype=np.float32)).reshape(128, 1).astype(np.float32)
    shared = dict(trash=trash, ecap=ecap, hmask=hmask, ident=ident, jmat=jmat, bones=bones, ones=np.ones((128, 128), np.float32),
                  tril=np.concatenate([tril, tril], 1), triu=np.concatenate([triu, triu], 1),
                  rel_bias=f(inp["rel_bias"]))
    for l in layers:
        shared.update({
            "w_ada%d" % l: f(inp["w_ada"][l]), "b_ada%d" % l: f(inp["b_ada"][l]).reshape(1, -1),
            "n1g%d" % l: _col(inp["norm1_g"][l], 8), "n2g%d" % l: _col(inp["norm2_g"][l], 8),
            "w_in%d" % l: f(inp["w_in"][l]), "w_up%d" % l: f(inp["gla_w_up"][l]),
            "b_up%d" % l: _col(np.asarray(inp["gla_b_up"][l]).reshape(-1), 4),
            "glag%d" % l: f(inp["gla_norm_g"][l]).reshape(128, 1),
            "qng%d" % l: np.ascontiguousarray(np.tile(f(inp["diff_qnorm_g"][l]), 2).reshape(128, 1)),
            "kng%d" % l: np.ascontiguousarray(np.tile(f(inp["diff_knorm_g"][l]), 2).reshape(128, 1)),
            "lamv%d" % l: f(inp["diff_lambda"][l]).reshape(1, 256),
            "subg%d" % l: f(inp["diff_subnorm_g"][l]).reshape(128, 1),
            "w_a%d" % l: f(inp["w_branch_a"][l]), "w_b%d" % l: f(inp["w_branch_b"][l]), "w_o%d" % l: f(inp["w_out"][l]),
            "w_r%d" % l: np.ascontiguousarray(np.concatenate([f(inp["w_router_group"][l]), f(inp["w_router_expert"][l])], 1)),
            "b_r%d" % l: np.concatenate([f(inp["b_router_group"][l]), f(inp["b_router_expert"][l])]).reshape(1, 36),
            "n2grow%d" % l: f(inp["norm2_g"][l]).reshape(1, D),
            "w1_%d" % l: f(inp["w_expert_in"][l]), "w2_%d" % l: f(inp["w_expert_out"][l]),
        })
    x = f(inp["x"]) if x_override is None else x_override
    maps = []
    consts = [_core_consts(hf, NLOC) for hf in range(2)]
    for core in range(2 * B):
        b, hf = core // 2, core % 2
        oh, ohc, flags = consts[hf]
        m = dict(shared)
        m["x_loc"] = np.ascontiguousarray(x[b, hf * NLOC:(hf + 1) * NLOC])
        m["x_oth"] = np.ascontiguousarray(x[b, (1 - hf) * NLOC:(2 - hf) * NLOC])
        m["c_col"] = _col(inp["c"][b], 8)
        m["oh"], m["ohc"], m["flags"] = oh, ohc, flags
        maps.append(m)
    return maps


_CACHE = {}


def run_layers(inp, SEQ, layer_groups, dbg=False, phase_limit=99):
    B = inp["x"].shape[0]
    NLOC = SEQ // 2
    x = np.ascontiguousarray(np.asarray(inp["x"], np.float32))
    for grp in layer_groups:
        key = (SEQ, tuple(grp))
        if key not in _CACHE:
            _CACHE[key] = build_program(SEQ, grp, dbg=dbg, phase_limit=phase_limit, NCORES=2 * B)[0]
        nc = _CACHE[key]
        maps = make_in_maps(inp, SEQ, grp, x_override=x)
        res = run_bass_kernel_spmd(nc, maps, core_ids=list(range(2 * B)))
        xn = np.empty_like(x)
        if dbg:
            return res
        for core in range(2 * B):
            b, hf = core // 2, core % 2
            xn[b, hf * NLOC:(hf + 1) * NLOC] = res.results[core]["x_out"]
        x = xn
    return x


def kernel(**inputs):
    SEQ = inputs["x"].shape[1]
    return run_layers(inputs, SEQ, [[0, 1]])
```
